# Optimizing a Trainium2 kernel written in Bass

```python
import math
import jax, jax.numpy as jnp
from jax import lax
import numpy as np

D_MODEL = 2048
BATCH = 1
SEQ = 8192
DEPTH = 2

N_A = max(1, DEPTH // 2)
N_B = DEPTH - N_A
N_META = 16
CONV_WIDTH = 31
HEAD_DIM = 64
N_HEADS = D_MODEL // HEAD_DIM
N_KV_HEADS = max(1, N_HEADS // 8)
GQA_GROUP = N_HEADS // N_KV_HEADS
WINDOW = 128
BLOCK = 128
ROT_DIM = HEAD_DIM // 4
ROPE_THETA = 500000.0
N_GROUPS = 4
EXPERTS_PER_GROUP = 8
N_EXPERTS = N_GROUPS * EXPERTS_PER_GROUP
TOP_K = 2
EXPERT_FF = D_MODEL // 8
ALPHA = (2.0 * DEPTH) ** 0.25
BETA = (8.0 * DEPTH) ** -0.25
LN_EPS = 1e-5

kernel_name = "yoco_conformer_swa_sinks_hier_moe"


def layer_norm(x, g, b):
    xf = x.astype(jnp.float32)
    mu = jnp.mean(xf, axis=-1, keepdims=True)
    var = jnp.mean(jnp.square(xf - mu), axis=-1, keepdims=True)
    y = (xf - mu) * lax.rsqrt(var + LN_EPS) * g.astype(jnp.float32) + b.astype(jnp.float32)
    return y.astype(x.dtype)


def rope_partial(x, pos):
    half = ROT_DIM // 2
    inv_freq = ROPE_THETA ** (-jnp.arange(0, ROT_DIM, 2, dtype=jnp.float32) / ROT_DIM)
    ang = pos.astype(jnp.float32)[:, None] * inv_freq[None, :]
    cos = jnp.cos(ang)[None, :, None, :].astype(x.dtype)
    sin = jnp.sin(ang)[None, :, None, :].astype(x.dtype)
    x1 = x[..., :half]
    x2 = x[..., half:ROT_DIM]
    return jnp.concatenate([x1 * cos - x2 * sin, x2 * cos + x1 * sin, x[..., ROT_DIM:]], axis=-1)


def conformer_conv(h, w_in, b_in, w_dw, b_dw, ln_g, ln_b, w_out, b_out):
    d = h.shape[-1]
    u = h @ w_in + b_in
    y = u[..., :d] * jax.nn.sigmoid(u[..., d:])
    y = lax.conv_general_dilated(
        y, w_dw[:, None, :], window_strides=(1,), padding=[(CONV_WIDTH - 1, 0)],
        dimension_numbers=("NWC", "WIO", "NWC"), feature_group_count=d) + b_dw
    y = layer_norm(y, ln_g, ln_b)
    y = jax.nn.silu(y)
    return y @ w_out + b_out


def swa_with_sinks(q, k, v, sinks):
    B, L = q.shape[0], q.shape[1]
    lead = (-N_META) % BLOCK
    Lp = L + lead
    nb = Lp // BLOCK
    pad = ((0, 0), (lead, 0), (0, 0), (0, 0))
    qb = jnp.pad(q, pad).reshape(B, nb, BLOCK, N_KV_HEADS, GQA_GROUP, HEAD_DIM)
    kc = jnp.pad(k, pad).reshape(B, nb, BLOCK, N_KV_HEADS, HEAD_DIM)
    vc = jnp.pad(v, pad).reshape(B, nb, BLOCK, N_KV_HEADS, HEAD_DIM)
    zero_blk = jnp.zeros_like(kc[:, :1])
    kb = jnp.concatenate([jnp.concatenate([zero_blk, kc[:, :-1]], axis=1), kc], axis=2)
    vb = jnp.concatenate([jnp.concatenate([zero_blk, vc[:, :-1]], axis=1), vc], axis=2)
    k_meta = k[:, :N_META]
    v_meta = v[:, :N_META]

    base = jnp.arange(nb)[:, None] * BLOCK - lead
    qpos = base + jnp.arange(BLOCK)[None, :]
    kpos = base + jnp.arange(-BLOCK, BLOCK)[None, :]
    band_mask = ((kpos[:, None, :] <= qpos[:, :, None])
                 & (qpos[:, :, None] - kpos[:, None, :] < WINDOW)
                 & (kpos[:, None, :] >= N_META))
    meta_mask = jnp.arange(N_META)[None, None, :] <= qpos[:, :, None]

    scale = 1.0 / math.sqrt(HEAD_DIM)
    s_band = jnp.einsum("bnqhgd,bnkhd->bnhgqk", qb, kb).astype(jnp.float32) * scale
    s_meta = jnp.einsum("bnqhgd,bmhd->bnhgqm", qb, k_meta).astype(jnp.float32) * scale
    s_band = jnp.where(band_mask[None, :, None, None], s_band, -jnp.inf)
    s_meta = jnp.where(meta_mask[None, :, None, None], s_meta, -jnp.inf)
    sink = jnp.broadcast_to(sinks.astype(jnp.float32).reshape(1, 1, N_KV_HEADS, GQA_GROUP, 1, 1),
                            s_band.shape[:-1] + (1,))
    p = jax.nn.softmax(jnp.concatenate([s_band, s_meta, sink], axis=-1), axis=-1)
    p_band = p[..., :2 * BLOCK].astype(v.dtype)
    p_meta = p[..., 2 * BLOCK:2 * BLOCK + N_META].astype(v.dtype)
    o = (jnp.einsum("bnhgqk,bnkhd->bnqhgd", p_band, vb)
         + jnp.einsum("bnhgqm,bmhd->bnqhgd", p_meta, v_meta))
    return o.reshape(B, Lp, N_HEADS, HEAD_DIM)[:, lead:]


def hier_moe(h, wg, bg, we, be, w1, w3, w2):
    B, L, D = h.shape
    xt = h.reshape(B * L, D)
    g_prob = jax.nn.softmax((xt @ wg + bg).astype(jnp.float32), axis=-1)
    g_w, g_idx = lax.top_k(g_prob, 1)
    e_logits = (xt @ we + be).astype(jnp.float32).reshape(-1, N_GROUPS, EXPERTS_PER_GROUP)
    e_sel = jnp.take_along_axis(e_logits, g_idx[:, :, None], axis=1)[:, 0]
    e_w, e_idx = lax.top_k(jax.nn.softmax(e_sel, axis=-1), TOP_K)
    e_w = e_w / jnp.sum(e_w, axis=-1, keepdims=True)
    flat = g_idx * EXPERTS_PER_GROUP + e_idx
    gate = jnp.sum(jax.nn.one_hot(flat, N_EXPERTS, dtype=jnp.float32) * (g_w * e_w)[..., None], axis=1)
    hid = (jax.nn.silu(jnp.einsum("td,edf->tef", xt, w1))
           * jnp.einsum("td,edf->tef", xt, w3) * gate[..., None].astype(xt.dtype))
    return jnp.einsum("tef,efd->td", hid, w2).reshape(B, L, D)


def setup_inputs(seed: int = 0) -> dict:
    key = jax.random.key(seed)
    ks = iter(jax.random.split(key, 40))
    D = D_MODEL
    kv_w = N_KV_HEADS * HEAD_DIM
    qw = N_HEADS * HEAD_DIM

    def nrm(shape, scale):
        return jax.random.normal(next(ks), shape, jnp.float32) * scale

    def gain(shape):
        return 1.0 + nrm(shape, 0.02)

    return {
        "x": nrm((BATCH, SEQ, D), 1.0),
        "meta_tokens": nrm((N_META, D), 1.0),
        "conv_w_in": nrm((N_A, D, 2 * D), D ** -0.5),
        "conv_b_in": nrm((N_A, 2 * D), 0.02),
        "conv_w_dw": nrm((N_A, CONV_WIDTH, D), CONV_WIDTH ** -0.5),
        "conv_b_dw": nrm((N_A, D), 0.02),
        "conv_ln_g": gain((N_A, D)),
        "conv_ln_b": nrm((N_A, D), 0.02),
        "conv_w_out": nrm((N_A, D, D), BETA * D ** -0.5),
        "conv_b_out": nrm((N_A, D), 0.02),
        "w_k": nrm((D, kv_w), D ** -0.5),
        "b_k": nrm((kv_w,), 0.02),
        "w_v": nrm((D, kv_w), BETA * D ** -0.5),
        "b_v": nrm((kv_w,), 0.02),
        "w_q": nrm((N_B, D, qw), D ** -0.5),
        "b_q": nrm((N_B, qw), 0.02),
        "w_o": nrm((N_B, qw, D), BETA * qw ** -0.5),
        "b_o": nrm((N_B, D), 0.02),
        "sinks": nrm((N_B, N_HEADS), 1.0),
        "ln_mix_g": gain((DEPTH, D)),
        "ln_mix_b": nrm((DEPTH, D), 0.02),
        "ln_ffn_g": gain((DEPTH, D)),
        "ln_ffn_b": nrm((DEPTH, D), 0.02),
        "router_group_w": nrm((DEPTH, D, N_GROUPS), D ** -0.5),
        "router_group_b": nrm((DEPTH, N_GROUPS), 0.01),
        "router_expert_w": nrm((DEPTH, D, N_EXPERTS), D ** -0.5),
        "router_expert_b": nrm((DEPTH, N_EXPERTS), 0.01),
        "expert_w1": nrm((DEPTH, N_EXPERTS, D, EXPERT_FF), D ** -0.5),
        "expert_w3": nrm((DEPTH, N_EXPERTS, D, EXPERT_FF), D ** -0.5),
        "expert_w2": nrm((DEPTH, N_EXPERTS, EXPERT_FF, D), BETA * EXPERT_FF ** -0.5),
    }


def reference(x, meta_tokens, conv_w_in, conv_b_in, conv_w_dw, conv_b_dw, conv_ln_g, conv_ln_b,
              conv_w_out, conv_b_out, w_k, b_k, w_v, b_v, w_q, b_q, w_o, b_o, sinks,
              ln_mix_g, ln_mix_b, ln_ffn_g, ln_ffn_b, router_group_w, router_group_b,
              router_expert_w, router_expert_b, expert_w1, expert_w3, expert_w2):
    B = x.shape[0]
    meta = jnp.broadcast_to(meta_tokens[None].astype(x.dtype), (B, N_META, D_MODEL))
    h = jnp.concatenate([meta, x], axis=1)
    L = h.shape[1]
    pos = jnp.arange(L)
    k_sh = None
    v_sh = None
    for layer in range(DEPTH):
        if layer < N_A:
            a = layer
            mix = conformer_conv(h, conv_w_in[a], conv_b_in[a], conv_w_dw[a], conv_b_dw[a],
                                 conv_ln_g[a], conv_ln_b[a], conv_w_out[a], conv_b_out[a])
        else:
            bi = layer - N_A
            q = rope_partial((h @ w_q[bi] + b_q[bi]).reshape(B, L, N_HEADS, HEAD_DIM), pos)
            att = swa_with_sinks(q, k_sh, v_sh, sinks[bi])
            mix = att.reshape(B, L, N_HEADS * HEAD_DIM) @ w_o[bi] + b_o[bi]
        h = layer_norm(ALPHA * h + mix, ln_mix_g[layer], ln_mix_b[layer])
        ffn = hier_moe(h, router_group_w[layer], router_group_b[layer], router_expert_w[layer],
                       router_expert_b[layer], expert_w1[layer], expert_w3[layer], expert_w2[layer])
        h = layer_norm(ALPHA * h + ffn, ln_ffn_g[layer], ln_ffn_b[layer])
        if layer == N_A - 1:
            k_sh = rope_partial((h @ w_k + b_k).reshape(B, L, N_KV_HEADS, HEAD_DIM), pos)
            v_sh = (h @ w_v + b_v).reshape(B, L, N_KV_HEADS, HEAD_DIM)
    return h[:, N_META:]
```

```python
import numpy as np
from contextlib import ExitStack
import concourse.bass as bass
import concourse.mybir as mybir
from concourse.bass_utils import run_bass_kernel_spmd

F32 = mybir.dt.float32
BF16 = mybir.dt.bfloat16
AF = mybir.ActivationFunctionType
ALU = mybir.AluOpType
AX = mybir.AxisListType

NCORES = 8
D = 2048
NJ = 16
T = 1200
TE = 176
TO = 1024
ALPHA = float((2.0 * 2) ** 0.25)
EPS = 1e-5
G0, G1, G2 = (0, 176), (176, 688), (688, 1200)
TT_E = [(0, 128), (128, 176)]
TT_O = [(176 + 128 * k, 176 + 128 * (k + 1)) for k in range(8)]
BIG = 1.0e9


class Sem:
    _n = 0

    def __init__(self, h):
        self.h = h
        self.id = Sem._n
        Sem._n += 1


class Buf:
    def __init__(self, name):
        self.name = name
        self.w = {}
        self.rd = {}
        self.dsem = None
        self.dcnt = 0
        self.psum = name.startswith('ps')


class Eng:
    def __init__(self, name, h, sem):
        self.name = name
        self.h = h
        self.sem = sem
        self.cnt = 0
        self.seen = {}


class Sched:
    def __init__(self, nc, es):
        self.nc = nc
        self.es = es
        mk = lambda n: Sem(es.enter_context(nc.semaphore(n)))
        self.pe = Eng('pe', nc.tensor, mk('s_pe'))
        self.act = Eng('act', nc.scalar, mk('s_act'))
        self.dve = Eng('dve', nc.vector, mk('s_dve'))
        self.pool = Eng('pool', nc.gpsimd, mk('s_pool'))
        self.sp = Eng('sp', nc.sync, mk('s_sp'))
        self.engs = [self.pe, self.act, self.dve, self.pool, self.sp]
        self.dsems = []

    def _deps(self, reads, writes, accw):
        deps = []
        for b in reads:
            deps.extend(b.w.values())
            if b.psum:
                deps.extend(b.rd.values())
        for b in writes:
            deps.extend(b.w.values())
            deps.extend(b.rd.values())
        for b in accw:
            deps.extend(b.rd.values())
        return deps

    def _wait(self, eng, deps):
        best = {}
        for (s, v) in deps:
            if best.get(s.id, (None, 0))[1] < v:
                best[s.id] = (s, v)
        for sid, (s, v) in best.items():
            if eng is self.pe and s is self.pe.sem:
                continue
            if eng.seen.get(sid, 0) < v:
                eng.h.wait_ge(s.h, v)
                eng.seen[sid] = v

    def _mark(self, tag, reads, writes, accw):
        for b in reads:
            b.rd[tag[0].id] = tag
        for b in writes:
            b.w = {tag[0].id: tag}
        for b in accw:
            b.w[tag[0].id] = tag

    def op(self, eng, fn, reads=(), writes=(), accw=()):
        self._wait(eng, self._deps(reads, writes, accw))
        ins = fn(eng.h)
        ins.then_inc(eng.sem.h, 1)
        eng.cnt += 1
        self._mark((eng.sem, eng.cnt), reads, writes, accw)

    def group(self, eng, fns, reads=(), writes=(), accw=()):
        self._wait(eng, self._deps(reads, writes, accw))
        ins = None
        for fn in fns:
            ins = fn(eng.h)
        ins.then_inc(eng.sem.h, 1)
        eng.cnt += 1
        self._mark((eng.sem, eng.cnt), reads, writes, accw)

    def dma(self, eng, out, in_, owner, reads=(), writes=(), accw=()):
        if owner.dsem is None:
            owner.dsem = Sem(self.es.enter_context(self.nc.semaphore('d_' + owner.name)))
            self.dsems.append(owner)
        self._wait(eng, self._deps(reads, writes, accw))
        eng.h.dma_start(out=out, in_=in_).then_inc(owner.dsem.h, 16)
        owner.dcnt += 16
        self._mark((owner.dsem, owner.dcnt), reads, writes, accw)

    def barrier(self):
        for e in self.engs:
            for e2 in self.engs:
                if e2 is e or e2.cnt == 0:
                    continue
                if e.seen.get(e2.sem.id, 0) < e2.cnt:
                    e.h.wait_ge(e2.sem.h, e2.cnt)
                    e.seen[e2.sem.id] = e2.cnt
            for o in self.dsems:
                if o.dcnt and e.seen.get(o.dsem.id, 0) < o.dcnt:
                    e.h.wait_ge(o.dsem.h, o.dcnt)
                    e.seen[o.dsem.id] = o.dcnt

    def wait_all(self, eng, bufs):
        deps = []
        for b in bufs:
            deps.extend(b.w.values())
            deps.extend(b.rd.values())
        self._wait(eng, deps)


class Rot:
    def __init__(self, items):
        self.items = items
        self.i = 0

    def get(self):
        it = self.items[self.i % len(self.items)]
        self.i += 1
        return it


VEC_NAMES = ['b1', 'b2', 'bdw', 'clng', 'clnb', 'bout', 'mixg0', 'mixb0', 'ffng0', 'ffnb0',
             'mixg1', 'mixb1', 'ffng1', 'ffnb1', 'bq', 'bo', 'snk']
CF = {}
_o = 0
for _n, _w in [('ident', 128), ('pswap', 128)] + [(v, 16) for v in VEC_NAMES] + \
        [('bk', 2), ('wdw', 16 * 31), ('padmask', 160), ('brb0', 36), ('brb1', 36), ('bvb', 256)]:
    CF[_n] = (_o, _o + _w)
    _o += _w
NCF = _o
CB = {'ones': (0, 128), 'identb': (128, 256), 'm_own': (256, 384), 'm_prev': (384, 512),
      'm_prev0': (512, 640)}
NCB = 640


class _Stop(Exception):
    pass


def build(dbg=()):
    dbg = set(dbg)
    try:
        return _build(dbg)
    except _Stop as st:
        return st.args


def _build(dbg):
    nc = bass.Bass("TRN2", target_bir_lowering=False)

    def din(name, shape):
        return nc.dram_tensor(name, list(shape), F32, kind="ExternalInput").ap()

    xe_d = din("xe", [T, D])
    cf_d = din("cf", [128, NCF])
    cb_d = din("cb", [128, NCB])
    cs_d = din("cs", [128, 2 * T])
    sel_d = din("sel", [64, 32 * 128])
    win_d = din("win", [NJ, 128, 4096])
    wout_d = din("wout", [NJ, 128, 2048])
    w13_d = din("w13", [2, 32, 2, 128, 4096])
    w2_d = din("w2", [2, 32, 128, 4096])
    wr_d = din("wr", [2, 128, 16 * 36])
    wk_d = din("wk", [2, 128, 2048])
    wv_d = din("wv", [128, 4096])
    wq_d = din("wq", [NJ, 128, 2048])
    wo_d = din("wo", [NJ, 128, 2048])
    out_d = nc.dram_tensor("out", [TO, D], F32, kind="ExternalOutput").ap()
    dbg_d = {}

    with ExitStack() as es:
        S = Sched(nc, es)
        PE, ACT, DVE, POOL, SP = S.pe, S.act, S.dve, S.pool, S.sp

        def sb(scope, name, shape, dt=F32):
            return scope.enter_context(nc.sbuf_tensor(name, list(shape), dt))

        hO = sb(es, "hO", [128, NJ, TO])
        hbO = sb(es, "hbO", [128, NJ, TO], BF16)
        hbE = sb(es, "hbE", [128, NJ, TE], BF16)
        wsl = [sb(es, f"wsl{i}", [128, 4096], BF16) for i in range(2)]
        wslB = [Buf(f"wsl{i}") for i in range(2)]
        cf = sb(es, "cf_sb", [128, NCF])
        cb = sb(es, "cb_sb", [128, NCB], BF16)
        st_mean = sb(es, "st_mean", [128, 512])
        st_rstd = sb(es, "st_rstd", [128, 512])
        B_mean, B_rstd = Buf("st_mean"), Buf("st_rstd")
        _st0 = (st_mean, B_mean, st_rstd, B_rstd)
        fscr = Rot([(sb(es, f"fs{i}", [128, 512]), Buf(f"fs{i}")) for i in range(5)])
        bscr = Rot([(sb(es, f"bs{i}", [128, 512], BF16), Buf(f"bs{i}")) for i in range(3)])
        esnk = sb(es, "esnk", [128, 16])
        B_esnk = Buf("esnk")
        B_cf, B_cb = Buf("cf"), Buf("cb")

        PS = [es.enter_context(nc.psum_tensor(f"ps{i}", [128, 512], F32)) for i in range(8)]
        PB = [Buf(f"ps{i}") for i in range(8)]

        def prot(idx):
            return Rot([(PS[i], PB[i]) for i in idx])

        HB = {G0: Buf("hb0"), G1: Buf("hb1"), G2: Buf("hb2")}
        H = {(j, g): Buf(f"h{j}_{g[0]}") for j in range(NJ) for g in (G0, G1, G2)}

        def tg_of(c0):
            return G0 if c0 < 176 else (G1 if c0 < 688 else G2)

        def cfv(name, j=None):
            a, b = CF[name]
            if j is None:
                return cf[:, a:b]
            return cf[:, a + j:a + j + 1]

        def cbv(name):
            a, b = CB[name]
            return cb[:, a:b]

        S.dma(SP, cf[:], cf_d[:, :], B_cf, writes=[B_cf])
        S.dma(POOL, cb[:], cb_d[:, :], B_cb, writes=[B_cb])
        S.op(ACT, lambda e: e.activation(out=esnk[:], in_=cfv('snk'), func=AF.Exp),
             reads=[B_cf], writes=[B_esnk])
        ident = cfv('ident')
        pswap = cfv('pswap')
        ones = cbv('ones')

        if 'stop0' in dbg:
            dump_esnk = True
        def dump(name, ap_sb, bufs, shape, dt=F32):
            d = nc.dram_tensor("o_" + name, list(shape), dt, kind="ExternalOutput").ap()
            dbg_d[name] = d
            ob = Buf("dbg_" + name)
            S.dma(SP, d, ap_sb, ob, reads=bufs)
            S.wait_all(SP, [ob])
            S._wait(SP, [(ob.dsem, ob.dcnt)])
            S.barrier()

        if 'stop0' in dbg:
            dump('esnk', esnk[:], [B_esnk], [128, 16])
            dump('cbd', cb[:], [B_cb], [128, NCB], BF16)
            S.barrier()
            raise _Stop(nc, dbg_d)
        def stream_weights(units, src_fn, width):
            issued = set()

            def load(i):
                if i in issued or i >= len(units):
                    return
                issued.add(i)
                k = i % 2
                S.dma(POOL, wsl[k][:, 0:width], src_fn(units[i]), wslB[k], writes=[wslB[k]])
            return load

        def layernorm(tgs, hcol, hbcol, gname, bname, write_hb=True):
            with ExitStack() as sc:
                uid = len(S.dsems) * 1000 + S.act.cnt
                sts = [(st_mean, B_mean, st_rstd, B_rstd),
                       (sb(sc, f"lnm{uid}", [128, 512]), Buf("lnm"), sb(sc, f"lnr{uid}", [128, 512]), Buf("lnr"))]
                vsc = Rot([(sb(sc, f"lnv{uid}_{i}", [128, 512], BF16), Buf(f"lnv{i}")) for i in range(4)])
                p12 = prot([6, 7, 4, 5])

                def stats(ti):
                    tg = tgs[ti]
                    c0, c1 = tg
                    n = c1 - c0
                    P1, B1 = p12.get()
                    P2, B2 = p12.get()
                    for j in range(NJ):
                        vb, Bvb = vsc.get()
                        S.op(POOL, lambda e: e.tensor_copy(out=vb[:, 0:n], in_=hcol(j, c0, c1)),
                             reads=[H[(j, tg)]], writes=[Bvb])
                        S.op(PE, lambda e: e.matmul(P1[:, 0:n], lhsT=ones, rhs=vb[:, 0:n], start=(j == 0), stop=(j == NJ - 1)),
                             reads=[Bvb, B_cb], writes=[B1] if j == 0 else [], accw=[] if j == 0 else [B1])
                        vq, Bvq = vsc.get()
                        S.op(ACT, lambda e: e.activation(out=vq[:, 0:n], in_=hcol(j, c0, c1), func=AF.Square),
                             reads=[H[(j, tg)]], writes=[Bvq])
                        S.op(PE, lambda e: e.matmul(P2[:, 0:n], lhsT=ones, rhs=vq[:, 0:n], start=(j == 0), stop=(j == NJ - 1)),
                             reads=[Bvq, B_cb], writes=[B2] if j == 0 else [], accw=[] if j == 0 else [B2])
                    ln_stats(P1, B1, P2, B2, n, sts[ti % 2])

                def norm(ti):
                    tg = tgs[ti]
                    c0, c1 = tg
                    n = c1 - c0
                    mean, Bm, rstd, Br = sts[ti % 2]
                    for j in range(NJ):
                        t1, Bt1 = fscr.get()
                        S.op(DVE, lambda e: e.tensor_tensor(out=t1[:, 0:n], in0=hcol(j, c0, c1), in1=mean[:, 0:n], op=ALU.subtract),
                             reads=[H[(j, tg)], Bm], writes=[Bt1])
                        t2, Bt2 = fscr.get()
                        S.op(DVE, lambda e: e.scalar_tensor_tensor(out=t2[:, 0:n], in0=t1[:, 0:n], scalar=cfv(gname, j), in1=rstd[:, 0:n], op0=ALU.mult, op1=ALU.mult),
                             reads=[Bt1, Br, B_cf], writes=[Bt2])
                        S.op(ACT, lambda e: e.activation(out=hcol(j, c0, c1), in_=t2[:, 0:n], func=AF.Identity, bias=cfv(bname, j)),
                             reads=[Bt2, B_cf], writes=[H[(j, tg)]])
                        if write_hb:
                            S.op(POOL, lambda e: e.tensor_copy(out=hbcol(j, c0, c1), in_=hcol(j, c0, c1)),
                                 reads=[H[(j, tg)]], accw=[HB[tg]])

                stats(0)
                for ti in range(len(tgs)):
                    if ti + 1 < len(tgs):
                        stats(ti + 1)
                    norm(ti)
                S.barrier()

        def ln_stats(P1, B1, P2, B2, n, st=None):
            st_mean, B_mean, st_rstd, B_rstd = st if st is not None else _st0
            S.op(DVE, lambda e: e.tensor_scalar(out=st_mean[:, 0:n], in0=P1[:, 0:n], scalar1=1.0 / D, scalar2=None, op0=ALU.mult),
                 reads=[B1], writes=[B_mean])
            t1, Bt1 = fscr.get()
            S.op(DVE, lambda e: e.tensor_tensor(out=t1[:, 0:n], in0=st_mean[:, 0:n], in1=st_mean[:, 0:n], op=ALU.mult),
                 reads=[B_mean], writes=[Bt1])
            t2, Bt2 = fscr.get()
            S.op(DVE, lambda e: e.scalar_tensor_tensor(out=t2[:, 0:n], in0=P2[:, 0:n], scalar=1.0 / D, in1=t1[:, 0:n], op0=ALU.mult, op1=ALU.subtract),
                 reads=[B2, Bt1], writes=[Bt2])
            t3, Bt3 = fscr.get()
            S.op(ACT, lambda e: e.activation(out=t3[:, 0:n], in_=t2[:, 0:n], func=AF.Sqrt, bias=EPS, scale=1.0),
                 reads=[Bt2], writes=[Bt3])
            S.op(DVE, lambda e: e.reciprocal(out=st_rstd[:, 0:n], in_=t3[:, 0:n]),
                 reads=[Bt3], writes=[B_rstd])

        def proj_residual(tgs, wsrc, bias_name, hcol, hbcol):
            units = list(range(NJ))
            load = stream_weights(units, lambda j: wsrc[j], 2048)
            pr = prot([0, 1, 2, 3])
            load(0)
            for j in units:
                load(j + 1)
                w = wsl[j % 2][:, 0:2048].rearrange("p (k m) -> p k m", k=NJ)
                for tg in tgs:
                    c0, c1 = tg
                    n = c1 - c0
                    P, Bp = pr.get()
                    S.group(PE, [lambda e, kc=kc: e.matmul(P[:, 0:n], lhsT=w[:, kc, :], rhs=hbcol(kc, c0, c1), start=(kc == 0), stop=(kc == NJ - 1)) for kc in range(NJ)],
                            reads=[wslB[j % 2], HB[tg]], writes=[Bp])
                    t, Bt = fscr.get()
                    S.op(ACT, lambda e, t=t, P=P: e.activation(out=t[:, 0:n], in_=P[:, 0:n], func=AF.Identity, bias=cfv(bias_name, j)),
                         reads=[Bp, B_cf], writes=[Bt])
                    S.op(DVE, lambda e, t=t: e.scalar_tensor_tensor(out=hcol(j, c0, c1), in0=hcol(j, c0, c1), scalar=ALPHA, in1=t[:, 0:n], op0=ALU.mult, op1=ALU.add),
                         reads=[Bt], writes=[H[(j, tg)]])

        def moe(layer, tgs, tts, hcol, hbcol, scope):
            wrt = sb(scope, f"wrt{layer}", [128, NJ, 36])
            B_wrt = Buf(f"wrt{layer}")
            sel = sb(scope, f"sel_sb{layer}", [64, 32, 128], BF16)
            B_sel = Buf(f"sel{layer}")
            gT = sb(scope, f"gT{layer}", [64, T], BF16)
            B_gT = {tg: Buf(f"gT{layer}_{tg[0]}") for tg in tgs}
            gball = sb(scope, f"gball{layer}", [128, T])
            B_gb = {tg: Buf(f"gb{layer}_{tg[0]}") for tg in tgs}
            hid = sb(scope, f"hid{layer}", [128, 2, 2, T], BF16)
            B_hid = [Buf(f"hid{layer}_0"), Buf(f"hid{layer}_1")]
            w2s = [sb(scope, f"w2s{layer}_{i}", [128, 2, D], BF16) for i in range(2)]
            B_w2 = [Buf(f"w2s{layer}_0"), Buf(f"w2s{layer}_1")]
            lg = sb(scope, f"lg{layer}", [128, 36])
            B_lg = Buf(f"lg{layer}")
            S.dma(SP, wrt[:].rearrange("p k m -> p (k m)"), wr_d[layer], B_wrt, writes=[B_wrt])
            S.dma(POOL, sel[:].rearrange("p e m -> p (e m)"), sel_d[:, :], B_sel, writes=[B_sel])
            brb = cfv('brb%d' % layer)
            units = [(e, fc) for e in range(32) for fc in range(2)]
            load = stream_weights(units, lambda u: w13_d[layer, u[0], u[1]], 4096)
            load(0)
            NT = len(tts)
            pr = prot([6, 7])

            def wt(name, shape, dt=F32):
                return sb(scope, f"g{layer}_{name}", shape, dt), Buf(f"g{layer}_{name}")
            lgall, B_lga = wt("lgall", [128, NT, 36])
            S.op(POOL, lambda e: e.memset(lgall[:].rearrange("p t m -> p (t m)"), 0.0), writes=[B_lga])
            for ti, (c0, c1) in enumerate(tts):
                n = c1 - c0
                tg = tg_of(c0)
                P, Bp = pr.get()
                S.group(PE, [lambda e, kc=kc: e.matmul(P[0:n, 0:36], lhsT=hcol(kc, c0, c1), rhs=wrt[:, kc, :], start=(kc == 0), stop=(kc == NJ - 1)) for kc in range(NJ)],
                        reads=[B_wrt] + [H[(kc, tg)] for kc in range(NJ)], writes=[Bp])
                S.op(DVE, lambda e: e.tensor_tensor(out=lgall[0:n, ti, :], in0=P[0:n, 0:36], in1=brb[0:n, :], op=ALU.add),
                     reads=[Bp, B_cf], accw=[B_lga])
            lg4 = lgall[:, :, 0:4]
            le = lgall[:, :, 4:36]
            gmax, Bgmax = wt("gmax", [128, NT])
            S.op(DVE, lambda e: e.tensor_reduce(out=gmax[:], in_=lg4, axis=AX.X, op=ALU.max), reads=[B_lga], writes=[Bgmax])
            gd, Bgd = wt("gd", [128, NT, 4])
            S.op(DVE, lambda e: e.tensor_tensor(out=gd[:], in0=lg4, in1=gmax[:].unsqueeze(2).broadcast_to([128, NT, 4]), op=ALU.subtract),
                 reads=[B_lga, Bgmax], writes=[Bgd])
            gexp, Bgexp = wt("gexp", [128, NT, 4])
            S.op(ACT, lambda e: e.activation(out=gexp[:], in_=gd[:], func=AF.Exp), reads=[Bgd], writes=[Bgexp])
            gsum, Bgsum = wt("gsum", [128, NT])
            S.op(DVE, lambda e: e.tensor_reduce(out=gsum[:], in_=gexp[:], axis=AX.X, op=ALU.add), reads=[Bgexp], writes=[Bgsum])
            gw, Bgw = wt("gw", [128, NT])
            S.op(DVE, lambda e: e.reciprocal(out=gw[:], in_=gsum[:]), reads=[Bgsum], writes=[Bgw])
            pen, Bpen = wt("pen", [128, NT, 4])
            S.op(DVE, lambda e: e.tensor_scalar(out=pen[:], in0=gd[:], scalar1=0.0, scalar2=None, op0=ALU.is_equal), reads=[Bgd], writes=[Bpen])
            S.op(DVE, lambda e: e.tensor_scalar(out=pen[:], in0=pen[:], scalar1=BIG, scalar2=-BIG, op0=ALU.mult, op1=ALU.add), reads=[Bpen], writes=[Bpen])
            lem, Blem = wt("lem", [128, NT, 32])
            S.op(DVE, lambda e: e.tensor_tensor(out=lem[:].rearrange("p t (g k) -> p t g k", g=4), in0=le.rearrange("p t (g k) -> p t g k", g=4),
                                                in1=pen[:].unsqueeze(3).broadcast_to([128, NT, 4, 8]), op=ALU.add),
                 reads=[B_lga, Bpen], writes=[Blem])
            m1, Bm1 = wt("m1", [128, NT])
            S.op(DVE, lambda e: e.tensor_reduce(out=m1[:], in_=lem[:], axis=AX.X, op=ALU.max), reads=[Blem], writes=[Bm1])
            eq1, Beq1 = wt("eq1", [128, NT, 32])
            S.op(DVE, lambda e: e.tensor_tensor(out=eq1[:], in0=lem[:], in1=m1[:].unsqueeze(2).broadcast_to([128, NT, 32]), op=ALU.is_equal),
                 reads=[Blem, Bm1], writes=[Beq1])
            lem2, Blem2 = wt("lem2", [128, NT, 32])
            S.op(DVE, lambda e: e.scalar_tensor_tensor(out=lem2[:], in0=eq1[:], scalar=-BIG, in1=lem[:], op0=ALU.mult, op1=ALU.add),
                 reads=[Beq1, Blem], writes=[Blem2])
            m2, Bm2 = wt("m2", [128, NT])
            S.op(DVE, lambda e: e.tensor_reduce(out=m2[:], in_=lem2[:], axis=AX.X, op=ALU.max), reads=[Blem2], writes=[Bm2])
            eq2, Beq2 = wt("eq2", [128, NT, 32])
            S.op(DVE, lambda e: e.tensor_tensor(out=eq2[:], in0=lem2[:], in1=m2[:].unsqueeze(2).broadcast_to([128, NT, 32]), op=ALU.is_equal),
                 reads=[Blem2, Bm2], writes=[Beq2])
            dm, Bdm = wt("dm", [128, NT])
            S.op(DVE, lambda e: e.tensor_tensor(out=dm[:], in0=m1[:], in1=m2[:], op=ALU.subtract), reads=[Bm1, Bm2], writes=[Bdm])
            sg, Bsg = wt("sg", [128, NT])
            S.op(ACT, lambda e: e.activation(out=sg[:], in_=dm[:], func=AF.Sigmoid), reads=[Bdm], writes=[Bsg])
            wa, Bwa = wt("wa", [128, NT])
            S.op(DVE, lambda e: e.tensor_tensor(out=wa[:], in0=gw[:], in1=sg[:], op=ALU.mult), reads=[Bgw, Bsg], writes=[Bwa])
            wb, Bwb = wt("wb", [128, NT])
            S.op(DVE, lambda e: e.tensor_tensor(out=wb[:], in0=gw[:], in1=wa[:], op=ALU.subtract), reads=[Bgw, Bwa], writes=[Bwb])
            S.op(DVE, lambda e: e.tensor_tensor(out=eq1[:], in0=eq1[:], in1=wa[:].unsqueeze(2).broadcast_to([128, NT, 32]), op=ALU.mult),
                 reads=[Bwa], writes=[Beq1])
            S.op(DVE, lambda e: e.tensor_tensor(out=eq2[:], in0=eq2[:], in1=wb[:].unsqueeze(2).broadcast_to([128, NT, 32]), op=ALU.mult),
                 reads=[Bwb], writes=[Beq2])
            gate, Bgate = wt("gate", [128, NT, 32])
            S.op(DVE, lambda e: e.tensor_tensor(out=gate[:], in0=eq1[:], in1=eq2[:], op=ALU.add), reads=[Beq1, Beq2], writes=[Bgate])
            if ('gate%d' % layer) in dbg:
                dump(f"gate{layer}", gate[:].rearrange("p t m -> p (t m)"), [Bgate], [128, NT * 32])
            hib, Bhib = wt("hib", [128, NT, 32], BF16)
            S.op(DVE, lambda e: e.tensor_copy(out=hib[:], in_=gate[:]), reads=[Bgate], writes=[Bhib])
            ghl, Bghl = wt("ghl", [128, NT, 64])
            S.op(DVE, lambda e: e.tensor_copy(out=ghl[:, :, 0:32], in_=hib[:]), reads=[Bhib], writes=[Bghl])
            S.op(DVE, lambda e: e.tensor_tensor(out=ghl[:, :, 32:64], in0=gate[:], in1=hib[:], op=ALU.subtract),
                 reads=[Bgate, Bhib], accw=[Bghl])
            for ti, (c0, c1) in enumerate(tts):
                n = c1 - c0
                tg = tg_of(c0)
                PT, Bpt = pr.get()
                S.op(PE, lambda e: e.transpose(PT[0:64, 0:n], ghl[0:n, ti, :], ident[0:n, 0:n]),
                     reads=[Bghl, B_cf], writes=[Bpt])
                S.op(ACT, lambda e: e.copy(gT[:, c0:c1], PT[0:64, 0:n]), reads=[Bpt], accw=[B_gT[tg]])

            pA = prot([0, 1])
            pB = prot([2, 3])
            pG = prot([4])
            pD = prot([5, 6, 7])
            for ui, (e_, fc) in enumerate(units):
                load(ui + 1)
                es_ = e_ % 2
                w13 = wsl[ui % 2][:, 0:4096].rearrange("p (k h m) -> p k h m", k=NJ, h=2)
                if fc == 0:
                    S.dma(POOL, w2s[es_][:].rearrange("p f d -> p (f d)"), w2_d[layer, e_], B_w2[es_], writes=[B_w2[es_]])
                for tg in tgs:
                    c0, c1 = tg
                    n = c1 - c0
                    if fc == 0:
                        PG, Bpg = pG.get()
                        S.op(PE, lambda e: e.matmul(PG[:, 0:n], lhsT=sel[:, e_, :], rhs=gT[:, c0:c1], start=True, stop=True),
                             reads=[B_sel, B_gT[tg]], writes=[Bpg])
                        S.op(ACT, lambda e: e.copy(gball[:, c0:c1], PG[:, 0:n]), reads=[Bpg], writes=[B_gb[tg]])
                    PA, Bpa = pA.get()
                    S.group(PE, [lambda e, kc=kc: e.matmul(PA[:, 0:n], lhsT=w13[:, kc, 0, :], rhs=hbcol(kc, c0, c1), start=(kc == 0), stop=(kc == NJ - 1)) for kc in range(NJ)],
                            reads=[wslB[ui % 2], HB[tg]], writes=[Bpa])
                    PBk, Bpb = pB.get()
                    S.group(PE, [lambda e, kc=kc: e.matmul(PBk[:, 0:n], lhsT=w13[:, kc, 1, :], rhs=hbcol(kc, c0, c1), start=(kc == 0), stop=(kc == NJ - 1)) for kc in range(NJ)],
                            reads=[wslB[ui % 2], HB[tg]], writes=[Bpb])
                    sa, Bsa = fscr.get()
                    S.op(ACT, lambda e: e.activation(out=sa[:, 0:n], in_=PA[:, 0:n], func=AF.Silu), reads=[Bpa], writes=[Bsa])
                    t, Bt = fscr.get()
                    S.op(DVE, lambda e: e.tensor_tensor(out=t[:, 0:n], in0=sa[:, 0:n], in1=gball[:, c0:c1], op=ALU.mult),
                         reads=[Bsa, B_gb[tg]], writes=[Bt])
                    S.op(DVE, lambda e: e.tensor_tensor(out=hid[:, es_, fc, c0:c1], in0=PBk[:, 0:n], in1=t[:, 0:n], op=ALU.mult),
                         reads=[Bpb, Bt], accw=[B_hid[es_]])
                if fc == 1 and es_ == 1:
                    first = (e_ == 1)
                    for j in range(NJ):
                        for tg in tgs:
                            c0, c1 = tg
                            n = c1 - c0
                            PD, Bpd = pD.get()
                            fns = []
                            for k, (ee, ff) in enumerate([(0, 0), (0, 1), (1, 0), (1, 1)]):
                                fns.append(lambda e, ee=ee, ff=ff, k=k: e.matmul(PD[:, 0:n], lhsT=w2s[ee][:, ff, j * 128:(j + 1) * 128], rhs=hid[:, ee, ff, c0:c1], start=(k == 0), stop=(k == 3)))
                            S.group(PE, fns, reads=[B_w2[0], B_w2[1], B_hid[0], B_hid[1]], writes=[Bpd])
                            if first:
                                S.op(DVE, lambda e: e.scalar_tensor_tensor(out=hcol(j, c0, c1), in0=hcol(j, c0, c1), scalar=ALPHA, in1=PD[:, 0:n], op0=ALU.mult, op1=ALU.add),
                                     reads=[Bpd], writes=[H[(j, tg)]])
                            else:
                                S.op(DVE, lambda e: e.tensor_tensor(out=hcol(j, c0, c1), in0=hcol(j, c0, c1), in1=PD[:, 0:n], op=ALU.add),
                                     reads=[Bpd], writes=[H[(j, tg)]])

        with ExitStack() as L0:
            hE = sb(L0, "hE", [128, NJ, TE])

            def hcol(j, c0, c1):
                return hE[:, j, c0:c1] if c1 <= TE else hO[:, j, c0 - TE:c1 - TE]

            def hbcol(j, c0, c1):
                return hbE[:, j, c0:c1] if c1 <= TE else hbO[:, j, c0 - TE:c1 - TE]

            def hcol4(j0, c0, c1):
                return hE[:, j0:j0 + 4, c0:c1] if c1 <= TE else hO[:, j0:j0 + 4, c0 - TE:c1 - TE]

            def hbcol4(j0, c0, c1):
                return hbE[:, j0:j0 + 4, c0:c1] if c1 <= TE else hbO[:, j0:j0 + 4, c0 - TE:c1 - TE]

            with ExitStack() as PA_:
                xs = [sb(PA_, f"xs{i}", [128, D]) for i in range(2)]
                xsB = [Buf(f"xs{i}") for i in range(2)]
                pr = prot([0, 1, 2, 3])
                tts = TT_E + TT_O
                S.dma(SP, xs[0][0:128, :], xe_d[0:128, :], xsB[0], writes=[xsB[0]])
                for ti, (c0, c1) in enumerate(tts):
                    n = c1 - c0
                    if ti + 1 < len(tts):
                        a0, a1 = tts[ti + 1]
                        S.dma(SP, xs[(ti + 1) % 2][0:a1 - a0, :], xe_d[a0:a1, :], xsB[(ti + 1) % 2], writes=[xsB[(ti + 1) % 2]])
                    x_t = xs[ti % 2]
                    tg = tg_of(c0)
                    for jq in range(4):
                        P, Bp = pr.get()
                        Pv = P[:, :].rearrange("p (a b) -> p a b", a=4)
                        S.group(PE, [lambda e, i=i: e.transpose(Pv[:, i, 0:n], x_t[0:n, (jq * 4 + i) * 128:(jq * 4 + i + 1) * 128], ident[0:n, 0:n]) for i in range(4)],
                                reads=[xsB[ti % 2], B_cf], writes=[Bp])
                        S.op(ACT, lambda e: e.copy(hcol4(jq * 4, c0, c1), Pv[:, :, 0:n]),
                             reads=[Bp], writes=[H[(jq * 4 + i, tg)] for i in range(4)] if False else [], accw=[H[(jq * 4 + i, tg)] for i in range(4)])
                        S.op(DVE, lambda e: e.tensor_copy(out=hbcol4(jq * 4, c0, c1), in_=hcol4(jq * 4, c0, c1)),
                             reads=[H[(jq * 4 + i, tg)] for i in range(4)], accw=[HB[tg]])
                S.barrier()
            def stop(flag):
                if flag in dbg:
                    S.barrier()
                    raise _Stop(nc, dbg_d)
            if 'h0' in dbg:
                dump("h0E", hE[:].rearrange("p j t -> p (j t)"), [], [128, NJ * TE])
                dump("h0O", hO[:].rearrange("p j t -> p (j t)"), [], [128, NJ * TO])

            stop('stopA')
            with ExitStack() as PB_:
                z = sb(PB_, "z", [128, NJ, T], BF16)
                Bz = {(j, g): Buf(f"z{j}_{g[0]}") for j in range(NJ) for g in (G0, G1, G2)}
                yb = [sb(PB_, f"yb{i}", [128, T + 30], BF16) for i in range(2)]
                Byb = [Buf("yb0"), Buf("yb1")]
                diag = sb(PB_, "diag", [128, 31, 128], BF16)
                Bdiag = Buf("diag")
                for i in range(2):
                    S.op(DVE, lambda e, i=i: e.memset(yb[i][:, 0:30], 0.0), writes=[Byb[i]])
                units = list(range(NJ))
                load = stream_weights(units, lambda j: win_d[j], 4096)
                pA = prot([0, 1])
                pB = prot([2, 3])
                pC = prot([4, 5])
                load(0)
                pending = []

                def conv(j, tg):
                    c0, c1 = tg
                    n = c1 - c0
                    y = yb[j % 2]
                    PC, Bpc = pC.get()
                    S.group(PE, [lambda e, k=k: e.matmul(PC[:, 0:n], lhsT=diag[:, k, :], rhs=y[:, c0 + k:c0 + k + n], start=(k == 0), stop=(k == 30)) for k in range(31)],
                            reads=[Bdiag, Byb[j % 2]], writes=[Bpc])
                    S.op(ACT, lambda e: e.activation(out=z[:, j, c0:c1], in_=PC[:, 0:n], func=AF.Identity, bias=cfv('bdw', j)),
                         reads=[Bpc, B_cf], writes=[Bz[(j, tg)]])

                for j in units:
                    load(j + 1)
                    w = wsl[j % 2][:, 0:4096].rearrange("p (k h m) -> p k h m", k=NJ, h=2)
                    y = yb[j % 2]
                    for tg in (G0, G1, G2):
                        c0, c1 = tg
                        n = c1 - c0
                        PA, Bpa = pA.get()
                        S.group(PE, [lambda e, kc=kc: e.matmul(PA[:, 0:n], lhsT=w[:, kc, 0, :], rhs=hbcol(kc, c0, c1), start=(kc == 0), stop=(kc == NJ - 1)) for kc in range(NJ)],
                                reads=[wslB[j % 2], HB[tg]], writes=[Bpa])
                        PBk, Bpb = pB.get()
                        S.group(PE, [lambda e, kc=kc: e.matmul(PBk[:, 0:n], lhsT=w[:, kc, 1, :], rhs=hbcol(kc, c0, c1), start=(kc == 0), stop=(kc == NJ - 1)) for kc in range(NJ)],
                                reads=[wslB[j % 2], HB[tg]], writes=[Bpb])
                        if pending:
                            pj, ptg = pending.pop(0)
                            conv(pj, ptg)
                        if tg == G0:
                            a, b = CF['wdw']
                            wd = cf[:, a + j * 31:a + (j + 1) * 31]
                            S.op(DVE, lambda e: e.tensor_tensor(out=diag[:], in0=cbv('identb').unsqueeze(1).broadcast_to([128, 31, 128]),
                                                                in1=wd.unsqueeze(2).broadcast_to([128, 31, 128]), op=ALU.mult),
                                 reads=[B_cb, B_cf], writes=[Bdiag])
                        sg, Bsg = fscr.get()
                        S.op(ACT, lambda e: e.activation(out=sg[:, 0:n], in_=PBk[:, 0:n], func=AF.Sigmoid, bias=cfv('b2', j)),
                             reads=[Bpb, B_cf], writes=[Bsg])
                        if tg == G0:
                            S.op(DVE, lambda e: e.scalar_tensor_tensor(out=y[:, 30 + c0:30 + c1], in0=PA[:, 0:n], scalar=cfv('b1', j), in1=sg[:, 0:n], op0=ALU.add, op1=ALU.mult),
                                 reads=[Bpa, Bsg, B_cf], writes=[Byb[j % 2]])
                            S.op(DVE, lambda e: e.tensor_tensor(out=y[:, 30 + 16:30 + 176], in0=y[:, 30 + 16:30 + 176], in1=cfv('padmask'), op=ALU.mult),
                                 reads=[B_cf], writes=[Byb[j % 2]])
                        else:
                            S.op(DVE, lambda e: e.scalar_tensor_tensor(out=y[:, 30 + c0:30 + c1], in0=PA[:, 0:n], scalar=cfv('b1', j), in1=sg[:, 0:n], op0=ALU.add, op1=ALU.mult),
                                 reads=[Bpa, Bsg, B_cf], accw=[Byb[j % 2]])
                        pending.append((j, tg))
                while pending:
                    pj, ptg = pending.pop(0)
                    conv(pj, ptg)
                if 'z' in dbg:
                    dump("z", z[:].rearrange("p j t -> p (j t)"), list(Bz.values()), [128, NJ * T], BF16)

                p12 = prot([6, 7, 0, 1])
                for tg in (G0, G1, G2):
                    c0, c1 = tg
                    n = c1 - c0
                    P1, B1 = p12.get()
                    P2, B2 = p12.get()
                    S.group(PE, [lambda e, j=j: e.matmul(P1[:, 0:n], lhsT=ones, rhs=z[:, j, c0:c1], start=(j == 0), stop=(j == NJ - 1)) for j in range(NJ)],
                            reads=[B_cb] + [Bz[(j, tg)] for j in range(NJ)], writes=[B1])
                    for j in range(NJ):
                        vq, Bvq = bscr.get()
                        S.op(ACT, lambda e, vq=vq, j=j: e.activation(out=vq[:, 0:n], in_=z[:, j, c0:c1], func=AF.Square),
                             reads=[Bz[(j, tg)]], writes=[Bvq])
                        S.op(PE, lambda e, vq=vq, j=j: e.matmul(P2[:, 0:n], lhsT=ones, rhs=vq[:, 0:n], start=(j == 0), stop=(j == NJ - 1)),
                             reads=[Bvq, B_cb], writes=[B2] if j == 0 else [], accw=[] if j == 0 else [B2])
                    ln_stats(P1, B1, P2, B2, n)
                    for j in range(NJ):
                        t1, Bt1 = fscr.get()
                        S.op(DVE, lambda e, t1=t1, j=j: e.tensor_tensor(out=t1[:, 0:n], in0=z[:, j, c0:c1], in1=st_mean[:, 0:n], op=ALU.subtract),
                             reads=[Bz[(j, tg)], B_mean], writes=[Bt1])
                        t2, Bt2 = fscr.get()
                        S.op(DVE, lambda e, t1=t1, t2=t2, j=j: e.scalar_tensor_tensor(out=t2[:, 0:n], in0=t1[:, 0:n], scalar=cfv('clng', j), in1=st_rstd[:, 0:n], op0=ALU.mult, op1=ALU.mult),
                             reads=[Bt1, B_rstd, B_cf], writes=[Bt2])
                        S.op(ACT, lambda e, t2=t2, j=j: e.activation(out=hbcol(j, c0, c1), in_=t2[:, 0:n], func=AF.Silu, bias=cfv('clnb', j)),
                             reads=[Bt2, B_cf], writes=[HB[tg]] if j == 0 else [], accw=[] if j == 0 else [HB[tg]])
                S.barrier()
            if 'a' in dbg:
                dump("aE", hbE[:].rearrange("p j t -> p (j t)"), list(HB.values()), [128, NJ * TE], BF16)
                dump("aO", hbO[:].rearrange("p j t -> p (j t)"), list(HB.values()), [128, NJ * TO], BF16)

            stop('stopC')
            proj_residual((G0, G1, G2), wout_d, 'bout', hcol, hbcol)
            layernorm((G0, G1, G2), hcol, hbcol, 'mixg0', 'mixb0')
            if 'h1' in dbg:
                S.barrier()
                dump("h1E", hE[:].rearrange("p j t -> p (j t)"), [], [128, NJ * TE])
                dump("h1O", hO[:].rearrange("p j t -> p (j t)"), [], [128, NJ * TO])

            stop('stopD')
            if 'stop1' not in dbg:
                with ExitStack() as PE_:
                    moe(0, (G0, G1, G2), TT_E + TT_O, hcol, hbcol, PE_)
                    S.barrier()
                    if 'pre2' in dbg:
                        dump("pre2E", hE[:].rearrange("p j t -> p (j t)"), [], [128, NJ * TE])
                        dump("pre2O", hO[:].rearrange("p j t -> p (j t)"), [], [128, NJ * TO])
                layernorm((G0, G1, G2), hcol, hbcol, 'ffng0', 'ffnb0')
            S.barrier()
            if 'h2' in dbg:
                dump("h2E", hE[:].rearrange("p j t -> p (j t)"), [], [128, NJ * TE])
                dump("h2O", hO[:].rearrange("p j t -> p (j t)"), [], [128, NJ * TO])

        def hcol(j, c0, c1):
            return hO[:, j, c0 - TE:c1 - TE]

        def hbcol(j, c0, c1):
            return hbE[:, j, c0:c1] if c1 <= TE else hbO[:, j, c0 - TE:c1 - TE]

        if 'stop2' not in dbg:
            with ExitStack() as L1:
                KT = sb(L1, "KT", [128, 2, T], BF16)
                B_KT = Buf("KT")
                V = sb(L1, "V", [128, 10, 256], BF16)
                B_V = Buf("V")
                with ExitStack() as LQ:
                    QT = sb(LQ, "QT", [128, NJ, TO], BF16)
                    B_QT = {g: Buf(f"QT{g[0]}") for g in (G1, G2)}
                    with ExitStack() as LCS:
                        cs = sb(LCS, "cs_sb", [128, 2, T])
                        B_cs = Buf("cs")
                        S.dma(SP, cs[:].rearrange("p a t -> p (a t)"), cs_d[:, :], B_cs, writes=[B_cs])
                        pr = prot([0, 1, 2, 3])
                        pr2 = prot([4, 5])

                        def rope_evac(P, Bp, n, c0, c1, bias_ap, dst_ap, dstB, acc=True):
                            raw, Braw = fscr.get()
                            S.op(ACT, lambda e: e.activation(out=raw[:, 0:n], in_=P[:, 0:n], func=AF.Identity, bias=bias_ap),
                                 reads=[Bp, B_cf], writes=[Braw])
                            P2, Bp2 = pr2.get()
                            S.op(PE, lambda e: e.matmul(P2[:, 0:n], lhsT=pswap, rhs=raw[:, 0:n], start=True, stop=True),
                                 reads=[Braw, B_cf], writes=[Bp2])
                            t1, Bt1 = fscr.get()
                            S.op(DVE, lambda e: e.tensor_tensor(out=t1[:, 0:n], in0=raw[:, 0:n], in1=cs[:, 0, c0:c1], op=ALU.mult),
                                 reads=[Braw, B_cs], writes=[Bt1])
                            t2, Bt2 = fscr.get()
                            S.op(DVE, lambda e: e.tensor_tensor(out=t2[:, 0:n], in0=P2[:, 0:n], in1=cs[:, 1, c0:c1], op=ALU.mult),
                                 reads=[Bp2, B_cs], writes=[Bt2])
                            S.op(DVE, lambda e: e.tensor_tensor(out=dst_ap, in0=t1[:, 0:n], in1=t2[:, 0:n], op=ALU.add),
                                 reads=[Bt1, Bt2], accw=[dstB])

                        units = [0, 1]
                        load = stream_weights(units, lambda g: wk_d[g], 2048)
                        load(0)
                        for gp in units:
                            load(gp + 1)
                            w = wsl[gp % 2][:, 0:2048].rearrange("p (k m) -> p k m", k=NJ)
                            for (c0, c1) in [(0, 16), (48, 176), G1, G2]:
                                n = c1 - c0
                                P, Bp = pr.get()
                                S.group(PE, [lambda e, kc=kc: e.matmul(P[:, 0:n], lhsT=w[:, kc, :], rhs=hbcol(kc, c0, c1), start=(kc == 0), stop=(kc == NJ - 1)) for kc in range(NJ)],
                                        reads=[wslB[gp % 2], HB[tg_of(c0)]], writes=[Bp])
                                a, b = CF['bk']
                                rope_evac(P, Bp, n, c0, c1, cf[:, a + gp:a + gp + 1], KT[:, gp, c0:c1], B_KT)
                        S.dma(POOL, wsl[0][:, 0:4096], wv_d[:, :], wslB[0], writes=[wslB[0]])
                        wv = wsl[0][:, 0:4096].rearrange("p (k m) -> p k m", k=NJ)
                        vts = [(0, 16)] + [(48 + 128 * m, 48 + 128 * (m + 1)) for m in range(9)]
                        for vi, (c0, c1) in enumerate(vts):
                            n = c1 - c0
                            P, Bp = pr.get()
                            S.group(PE, [lambda e, kc=kc: e.matmul(P[0:n, 0:256], lhsT=hbcol(kc, c0, c1), rhs=wv[:, kc, :], start=(kc == 0), stop=(kc == NJ - 1)) for kc in range(NJ)],
                                    reads=[wslB[0], HB[tg_of(c0)]], writes=[Bp])
                            S.op(DVE, lambda e: e.tensor_tensor(out=V[0:n, vi, :], in0=P[0:n, 0:256], in1=cfv('bvb')[0:n, :], op=ALU.add),
                                 reads=[Bp, B_cf], accw=[B_V])
                        units = list(range(NJ))
                        load = stream_weights(units, lambda j: wq_d[j], 2048)
                        load(0)
                        for j in units:
                            load(j + 1)
                            w = wsl[j % 2][:, 0:2048].rearrange("p (k m) -> p k m", k=NJ)
                            for tg in (G1, G2):
                                c0, c1 = tg
                                n = c1 - c0
                                P, Bp = pr.get()
                                S.group(PE, [lambda e, kc=kc: e.matmul(P[:, 0:n], lhsT=w[:, kc, :], rhs=hbcol(kc, c0, c1), start=(kc == 0), stop=(kc == NJ - 1)) for kc in range(NJ)],
                                        reads=[wslB[j % 2], HB[tg]], writes=[Bp])
                                rope_evac(P, Bp, n, c0, c1, cfv('bq', j), QT[:, j, c0 - TE:c1 - TE], B_QT[tg])
                        S.barrier()
                    if 'qkv' in dbg:
                        dump("KT", KT[:].rearrange("p g t -> p (g t)"), [], [128, 2 * T], BF16)
                        dump("V", V[:].rearrange("p g t -> p (g t)"), [], [128, 10 * 256], BF16)
                        dump("QT", QT[:].rearrange("p g t -> p (g t)"), [], [128, NJ * TO], BF16)

                    Eo = Rot([(sb(LQ, f"Eo{i}", [128, 512], BF16), Buf(f"Eo{i}")) for i in range(2)])
                    Ep = Rot([(sb(LQ, f"Ep{i}", [128, 512], BF16), Buf(f"Ep{i}")) for i in range(2)])
                    Em = Rot([(sb(LQ, f"Em{i}", [16, 512], BF16), Buf(f"Em{i}")) for i in range(2)])
                    pS = prot([0, 1, 2, 3, 4, 5])
                    pO = prot([6])
                    pDn = prot([7])
                    B_att = Buf("att")
                    ascr = Rot([(sb(LQ, f"as{i}", [128, 512]), Buf(f"as{i}")) for i in range(6)])
                    for gp in range(2):
                        for half in range(2):
                            r0, r1 = half * 64, half * 64 + 64
                            for n_ in range(8):
                                own = (TE + 128 * n_, TE + 128 * n_ + 128)
                                prv = (48 + 128 * n_, 48 + 128 * n_ + 128)
                                qtg = G1 if n_ < 4 else G2
                                for quad in range(2):
                                    cj = gp * 8 + quad * 4
                                    rhsQ = QT[r0:r1, cj:cj + 4, n_ * 128:(n_ + 1) * 128]
                                    PSo, Bso = pS.get()
                                    PSp, Bsp = pS.get()
                                    PSm, Bsm = pS.get()
                                    S.op(PE, lambda e: e.matmul(PSo[:, :], lhsT=KT[r0:r1, gp, own[0]:own[1]], rhs=rhsQ, start=True, stop=True),
                                         reads=[B_KT, B_QT[qtg]], writes=[Bso])
                                    S.op(PE, lambda e: e.matmul(PSp[:, :], lhsT=KT[r0:r1, gp, prv[0]:prv[1]], rhs=rhsQ, start=True, stop=True),
                                         reads=[B_KT, B_QT[qtg]], writes=[Bsp])
                                    S.op(PE, lambda e: e.matmul(PSm[0:16, :], lhsT=KT[r0:r1, gp, 0:16], rhs=rhsQ, start=True, stop=True),
                                         reads=[B_KT, B_QT[qtg]], writes=[Bsm])
                                    eo, Beo = Eo.get()
                                    ep, Bep = Ep.get()
                                    em, Bem = Em.get()
                                    S.op(ACT, lambda e: e.activation(out=eo[:, :], in_=PSo[:, :], func=AF.Exp, scale=0.125), reads=[Bso], writes=[Beo])
                                    S.op(ACT, lambda e: e.activation(out=ep[:, :], in_=PSp[:, :], func=AF.Exp, scale=0.125), reads=[Bsp], writes=[Bep])
                                    S.op(ACT, lambda e: e.activation(out=em[:, :], in_=PSm[0:16, :], func=AF.Exp, scale=0.125), reads=[Bsm], writes=[Bem])
                                    eo3 = eo[:, :].rearrange("p (a b) -> p a b", a=4)
                                    ep3 = ep[:, :].rearrange("p (a b) -> p a b", a=4)
                                    S.op(DVE, lambda e: e.tensor_tensor(out=eo3, in0=eo3, in1=cbv('m_own').unsqueeze(1).broadcast_to([128, 4, 128]), op=ALU.mult),
                                         reads=[B_cb], writes=[Beo])
                                    mp = cbv('m_prev0') if n_ == 0 else cbv('m_prev')
                                    S.op(DVE, lambda e: e.tensor_tensor(out=ep3, in0=ep3, in1=mp.unsqueeze(1).broadcast_to([128, 4, 128]), op=ALU.mult),
                                         reads=[B_cb], writes=[Bep])
                                    PO, Bpo = pO.get()
                                    PDn, Bpdn = pDn.get()
                                    vo, vp = n_ + 2, n_ + 1
                                    S.group(PE, [
                                        lambda e: e.matmul(PO[:, :], lhsT=V[:, vo, gp * 128:(gp + 1) * 128], rhs=eo[:, :], start=True, stop=False),
                                        lambda e: e.matmul(PO[:, :], lhsT=V[:, vp, gp * 128:(gp + 1) * 128], rhs=ep[:, :], start=False, stop=False),
                                        lambda e: e.matmul(PO[:, :], lhsT=V[0:16, 0, gp * 128:(gp + 1) * 128], rhs=em[:, :], start=False, stop=True),
                                    ], reads=[B_V, Beo, Bep, Bem], writes=[Bpo])
                                    S.group(PE, [
                                        lambda e: e.matmul(PDn[:, :], lhsT=ones, rhs=eo[:, :], start=True, stop=False),
                                        lambda e: e.matmul(PDn[:, :], lhsT=ones, rhs=ep[:, :], start=False, stop=False),
                                        lambda e: e.matmul(PDn[:, :], lhsT=ones[0:16, :], rhs=em[:, :], start=False, stop=True),
                                    ], reads=[B_cb, Beo, Bep, Bem], writes=[Bpdn])
                                    d1, Bd1 = ascr.get()
                                    d13 = d1[r0:r1, :].rearrange("p (a b) -> p a b", a=4)
                                    S.op(DVE, lambda e: e.tensor_tensor(out=d13, in0=PDn[r0:r1, :].rearrange("p (a b) -> p a b", a=4),
                                                                        in1=esnk[r0:r1, cj:cj + 4].unsqueeze(2).broadcast_to([64, 4, 128]), op=ALU.add),
                                         reads=[Bpdn, B_esnk], writes=[Bd1])
                                    po, Bpos = ascr.get()
                                    S.op(ACT, lambda e: e.copy(po[r0:r1, :], PO[r0:r1, :]), reads=[Bpo], writes=[Bpos])
                                    S.op(ACT, lambda e: e.activation(out=d1[r0:r1, :], in_=d1[r0:r1, :], func=AF.Ln), reads=[Bd1], writes=[Bd1])
                                    S.op(ACT, lambda e: e.activation(out=d1[r0:r1, :], in_=d1[r0:r1, :], func=AF.Exp, scale=-1.0), reads=[Bd1], writes=[Bd1])
                                    S.op(POOL, lambda e: e.tensor_tensor(out=hbO[r0:r1, cj:cj + 4, n_ * 128:(n_ + 1) * 128],
                                                                         in0=po[r0:r1, :].rearrange("p (a b) -> p a b", a=4),
                                                                         in1=d1[r0:r1, :].rearrange("p (a b) -> p a b", a=4), op=ALU.mult),
                                         reads=[Bpos, Bd1], accw=[HB[qtg]])
                    S.barrier()
            if 'att' in dbg:
                dump("attT", hbO[:].rearrange("p j t -> p (j t)"), [], [128, NJ * TO], BF16)
            proj_residual((G1, G2), wo_d, 'bo', hcol, hbcol)
            layernorm((G1, G2), hcol, hbcol, 'mixg1', 'mixb1')
            if 'h3' in dbg:
                S.barrier()
                dump("h3O", hO[:].rearrange("p j t -> p (j t)"), [], [128, NJ * TO])
            if 'stop3' not in dbg:
                with ExitStack() as PM_:
                    moe(1, (G1, G2), TT_O, hcol, hbcol, PM_)
                    S.barrier()
                layernorm((G1, G2), hcol, hbcol, 'ffng1', 'ffnb1', write_hb=False)
            S.barrier()

        with ExitStack() as LO:
            ost = [sb(LO, f"ost{i}", [128, D]) for i in range(2)]
            Bost = [Buf("ost0"), Buf("ost1")]
            pr = prot([0, 1, 2, 3])
            for k in range(8):
                o = ost[k % 2]
                tg = G1 if k < 4 else G2
                for jq in range(4):
                    P, Bp = pr.get()
                    S.group(PE, [lambda e, i=i: e.transpose(P[:, i * 128:(i + 1) * 128], hO[:, jq * 4 + i, k * 128:(k + 1) * 128], ident) for i in range(4)],
                            reads=[B_cf] + [H[(jq * 4 + i, tg)] for i in range(4)], writes=[Bp])
                    eng = ACT if jq % 2 == 0 else DVE
                    if eng is ACT:
                        S.op(ACT, lambda e: e.copy(o[:, jq * 512:(jq + 1) * 512], P[:, :]), reads=[Bp], writes=[Bost[k % 2]] if jq == 0 else [], accw=[] if jq == 0 else [Bost[k % 2]])
                    else:
                        S.op(DVE, lambda e: e.tensor_copy(out=o[:, jq * 512:(jq + 1) * 512], in_=P[:, :]), reads=[Bp], accw=[Bost[k % 2]])
                S.dma(SP, out_d[k * 128:(k + 1) * 128, :], o[:, :], Bost[k % 2], reads=[Bost[k % 2]])
            S.wait_all(SP, Bost)
            S.barrier()
    return nc, dbg_d


def _fm_vec(v):
    return np.ascontiguousarray(np.asarray(v, np.float32).reshape(NJ, 128).T)


def _head_perm():
    idx = []
    for jp in range(NJ):
        gp, i = jp // 8, jp % 8
        for hd in (16 * gp + i, 16 * gp + 8 + i):
            idx.extend(range(hd * 64, hd * 64 + 64))
    return np.array(idx)


def _w_fm(w, ncols_chunk):
    K, C = w.shape
    nch = C // ncols_chunk
    a = w.reshape(NJ, 128, nch, ncols_chunk).transpose(2, 1, 0, 3)
    return np.ascontiguousarray(a).reshape(nch, 128, NJ * ncols_chunk)


def prepare(inputs):
    f = lambda k: np.asarray(inputs[k], np.float32)
    x = f('x')[0]
    meta = f('meta_tokens')
    h0 = np.concatenate([meta, x], 0)
    shared = {}
    w_in = f('conv_w_in')[0]
    wv_ = w_in[:, :D].reshape(NJ, 128, NJ, 128)
    wg_ = w_in[:, D:].reshape(NJ, 128, NJ, 128)
    win = np.stack([wv_, wg_], 3)
    shared['win'] = np.ascontiguousarray(win.transpose(2, 1, 0, 3, 4)).reshape(NJ, 128, 4096)
    shared['wout'] = _w_fm(f('conv_w_out')[0], 128)
    w1 = f('expert_w1')
    w3 = f('expert_w3')
    a1 = w1.reshape(2, 32, NJ, 128, 2, 128)
    a3 = w3.reshape(2, 32, NJ, 128, 2, 128)
    w13 = np.stack([a1, a3], 5)
    shared['w13'] = np.ascontiguousarray(w13.transpose(0, 1, 4, 3, 2, 5, 6)).reshape(2, 32, 2, 128, 4096)
    del w13, a1, a3
    w2 = f('expert_w2').reshape(2, 32, 2, 128, D)
    shared['w2'] = np.ascontiguousarray(w2.transpose(0, 1, 3, 2, 4)).reshape(2, 32, 128, 4096)
    wr = np.concatenate([f('router_group_w'), f('router_expert_w')], -1)
    shared['wr'] = np.ascontiguousarray(wr.reshape(2, NJ, 128, 36).transpose(0, 2, 1, 3)).reshape(2, 128, NJ * 36)
    shared['wk'] = _w_fm(f('w_k'), 128)
    shared['wv'] = _w_fm(f('w_v'), 256)[0]
    perm = _head_perm()
    shared['wq'] = _w_fm(f('w_q')[0][:, perm], 128)
    shared['wo'] = _w_fm(f('w_o')[0][perm, :], 128)
    cfp = np.zeros((128, NCF), np.float32)

    def put(name, arr):
        a, b = CF[name]
        cfp[:, a:b] = arr
    put('ident', np.eye(128, dtype=np.float32))
    ps = np.zeros((128, 128), np.float32)
    for m in range(128):
        d = m % 64
        if d < 8:
            ps[m + 8, m] = 1.0
        elif d < 16:
            ps[m - 8, m] = 1.0
    put('pswap', ps)
    b_in = f('conv_b_in')[0]
    put('b1', _fm_vec(b_in[:D]))
    put('b2', _fm_vec(b_in[D:]))
    put('bdw', _fm_vec(f('conv_b_dw')[0]))
    put('clng', _fm_vec(f('conv_ln_g')[0]))
    put('clnb', _fm_vec(f('conv_ln_b')[0]))
    put('bout', _fm_vec(f('conv_b_out')[0]))
    for l in range(2):
        put('mixg%d' % l, _fm_vec(f('ln_mix_g')[l]))
        put('mixb%d' % l, _fm_vec(f('ln_mix_b')[l]))
        put('ffng%d' % l, _fm_vec(f('ln_ffn_g')[l]))
        put('ffnb%d' % l, _fm_vec(f('ln_ffn_b')[l]))
    put('bq', _fm_vec(f('b_q')[0][perm]))
    put('bo', _fm_vec(f('b_o')[0]))
    put('snk', _fm_vec(np.repeat(f('sinks')[0], 64)[perm]))
    put('bk', np.ascontiguousarray(f('b_k').reshape(2, 128).T))
    wdw = f('conv_w_dw')[0]
    put('wdw', np.ascontiguousarray(wdw.reshape(31, NJ, 128).transpose(2, 1, 0)).reshape(128, NJ * 31))
    for l in range(2):
        br = np.concatenate([f('router_group_b')[l], f('router_expert_b')[l]])
        put('brb%d' % l, np.broadcast_to(br[None, :], (128, 36)))
    put('bvb', np.broadcast_to(f('b_v')[None, :], (128, 256)))
    kk = np.arange(128)[:, None]
    qq = np.arange(128)[None, :]
    m_own = (kk <= qq).astype(np.float32)
    m_prev = (kk > qq).astype(np.float32)
    sel = np.zeros((64, 32, 128), np.float32)
    for e in range(32):
        sel[e, e, :] = 1.0
        sel[32 + e, e, :] = 1.0
    shared['sel'] = sel.reshape(64, 32 * 128)
    inv_freq = (np.float32(500000.0) ** (-np.arange(0, 16, 2, dtype=np.float32) / np.float32(16))).astype(np.float32)
    in_maps = []
    for c in range(NCORES):
        own0 = 16 + 1024 * c
        pos = np.concatenate([np.arange(16), np.arange(own0 - 160, own0 + 1024)])
        valid = pos >= 0
        xe = np.zeros((T, D), np.float32)
        xe[valid] = h0[pos[valid]]
        cfc = cfp.copy()
        a, b = CF['padmask']
        cfc[:, a:b] = valid[16:176].astype(np.float32)[None, :]
        cbc = np.zeros((128, NCB), np.float32)
        cbc[:, 0:128] = 1.0
        cbc[:, 128:256] = np.eye(128, dtype=np.float32)
        cbc[:, 256:384] = m_own
        cbc[:, 384:512] = m_prev
        cbc[:, 512:640] = m_prev if c > 0 else 0.0
        ang = np.clip(pos, 0, None).astype(np.float32)[:, None] * inv_freq[None, :]
        cosv = np.cos(ang).astype(np.float32)
        sinv = np.sin(ang).astype(np.float32)
        cst = np.zeros((128, 2, T), np.float32)
        cst[:, 0, :] = 1.0
        for p in range(128):
            d = p % 64
            if d < 16:
                cst[p, 0, :] = cosv[:, d % 8]
                cst[p, 1, :] = -sinv[:, d % 8] if d < 8 else sinv[:, d % 8]
        m = dict(shared)
        m['xe'] = xe
        m['cf'] = cfc
        m['cb'] = cbc
        m['cs'] = cst.reshape(128, 2 * T)
        in_maps.append(m)
    return in_maps


_CACHE = {}


def kernel(**inputs):
    in_maps = prepare(inputs)
    if 'nc' not in _CACHE:
        _CACHE['nc'] = build()[0]
    nc = _CACHE['nc']
    res = run_bass_kernel_spmd(nc, in_maps, core_ids=list(range(NCORES)))
    out = np.concatenate([np.asarray(r["out"], np.float32) for r in res.results], 0)
    return out.reshape(1, NCORES * TO, D)
```

```python
import numpy as np
from contextlib import ExitStack
import concourse.bass as bass
import concourse.mybir as mybir
from concourse.bass_utils import run_bass_kernel_spmd

F32 = mybir.dt.float32
BF16 = mybir.dt.bfloat16
AF = mybir.ActivationFunctionType
ALU = mybir.AluOpType
AX = mybir.AxisListType

NCORES = 8
D = 2048
NJ = 16
T = 1200
TE = 176
TO = 1024
ALPHA = float((2.0 * 2) ** 0.25)
EPS = 1e-5
G0, G1, G2 = (0, 176), (176, 688), (688, 1200)
TT_E = [(0, 128), (128, 176)]
TT_O = [(176 + 128 * k, 176 + 128 * (k + 1)) for k in range(8)]
BIG = 1.0e9


class Sem:
    _n = 0

    def __init__(self, h):
        self.h = h
        self.id = Sem._n
        Sem._n += 1


class Buf:
    def __init__(self, name):
        self.name = name
        self.w = {}
        self.wf = {}
        self.rd = {}
        self.dsem = None
        self.dcnt = 0
        self.psum = name.startswith('ps')


class Eng:
    def __init__(self, name, h, sem):
        self.name = name
        self.h = h
        self.sem = sem
        self.cnt = 0
        self.seen = {}


class Sched:
    def __init__(self, nc, es):
        self.nc = nc
        self.es = es
        mk = lambda n: Sem(es.enter_context(nc.semaphore(n)))
        self.pe = Eng('pe', nc.tensor, mk('s_pe'))
        self.act = Eng('act', nc.scalar, mk('s_act'))
        self.dve = Eng('dve', nc.vector, mk('s_dve'))
        self.pool = Eng('pool', nc.gpsimd, mk('s_pool'))
        self.sp = Eng('sp', nc.sync, mk('s_sp'))
        self.engs = [self.pe, self.act, self.dve, self.pool, self.sp]
        self.dsems = []

    def _deps(self, reads, writes, accw):
        deps = []
        for b in reads:
            deps.extend(b.w.values())
            if b.psum:
                deps.extend(b.rd.values())
        for b in writes:
            deps.extend(b.w.values())
            deps.extend(b.rd.values())
        for b in accw:
            deps.extend(b.rd.values())
            deps.extend(b.wf.values())
        return deps

    def _wait(self, eng, deps):
        best = {}
        for (s, v) in deps:
            if best.get(s.id, (None, 0))[1] < v:
                best[s.id] = (s, v)
        for sid, (s, v) in best.items():
            if eng is self.pe and s is self.pe.sem:
                continue
            if eng.seen.get(sid, 0) < v:
                eng.h.wait_ge(s.h, v)
                eng.seen[sid] = v

    def _mark(self, tag, reads, writes, accw):
        for b in reads:
            b.rd[tag[0].id] = tag
        for b in writes:
            b.w = {tag[0].id: tag}
            b.wf = {tag[0].id: tag}
        for b in accw:
            b.w[tag[0].id] = tag

    def op(self, eng, fn, reads=(), writes=(), accw=()):
        self._wait(eng, self._deps(reads, writes, accw))
        ins = fn(eng.h)
        ins.then_inc(eng.sem.h, 1)
        eng.cnt += 1
        self._mark((eng.sem, eng.cnt), reads, writes, accw)

    def group(self, eng, fns, reads=(), writes=(), accw=()):
        self._wait(eng, self._deps(reads, writes, accw))
        ins = None
        for fn in fns:
            ins = fn(eng.h)
        ins.then_inc(eng.sem.h, 1)
        eng.cnt += 1
        self._mark((eng.sem, eng.cnt), reads, writes, accw)

    def dma(self, eng, out, in_, owner, reads=(), writes=(), accw=()):
        if owner.dsem is None:
            owner.dsem = Sem(self.es.enter_context(self.nc.semaphore('d_' + owner.name)))
            self.dsems.append(owner)
        self._wait(eng, self._deps(reads, writes, accw))
        eng.h.dma_start(out=out, in_=in_).then_inc(owner.dsem.h, 16)
        owner.dcnt += 16
        self._mark((owner.dsem, owner.dcnt), reads, writes, accw)

    def barrier(self):
        for e in self.engs:
            for e2 in self.engs:
                if e2 is e or e2.cnt == 0:
                    continue
                if e.seen.get(e2.sem.id, 0) < e2.cnt:
                    e.h.wait_ge(e2.sem.h, e2.cnt)
                    e.seen[e2.sem.id] = e2.cnt
            for o in self.dsems:
                if o.dcnt and e.seen.get(o.dsem.id, 0) < o.dcnt:
                    e.h.wait_ge(o.dsem.h, o.dcnt)
                    e.seen[o.dsem.id] = o.dcnt

    def wait_all(self, eng, bufs):
        deps = []
        for b in bufs:
            deps.extend(b.w.values())
            deps.extend(b.rd.values())
        self._wait(eng, deps)


class Rot:
    def __init__(self, items):
        self.items = items
        self.i = 0

    def get(self):
        it = self.items[self.i % len(self.items)]
        self.i += 1
        return it


VEC_NAMES = ['b1', 'b2', 'bdw', 'clng', 'clnb', 'bout', 'mixg0', 'mixb0', 'ffng0', 'ffnb0',
             'mixg1', 'mixb1', 'ffng1', 'ffnb1', 'bq', 'bo', 'snk']
CF = {}
_o = 0
for _n, _w in [('ident', 128), ('pswap', 128), ('onesf', 128)] + [(v, 16) for v in VEC_NAMES] + \
        [('bk', 2), ('wdw', 16 * 31), ('padmask', 160), ('brb0', 36), ('brb1', 36), ('bvb', 256)]:
    CF[_n] = (_o, _o + _w)
    _o += _w
NCF = _o
CB = {'ones': (0, 128), 'identb': (128, 256), 'm_own': (256, 384), 'm_prev': (384, 512),
      'm_prev0': (512, 640)}
NCB = 640


class _Stop(Exception):
    pass


def build(dbg=()):
    dbg = set(dbg)
    try:
        return _build(dbg)
    except _Stop as st:
        return st.args


def _build(dbg):
    nc = bass.Bass("TRN2", target_bir_lowering=False)

    def din(name, shape):
        return nc.dram_tensor(name, list(shape), F32, kind="ExternalInput").ap()

    xe_d = din("xe", [T, D])
    cf_d = din("cf", [128, NCF])
    cb_d = din("cb", [128, NCB])
    cs_d = din("cs", [128, 2 * T])
    sel_d = din("sel", [64, 32 * 128])
    win_d = din("win", [NJ, 128, 4096])
    wout_d = din("wout", [NJ, 128, 2048])
    w13_d = din("w13", [2, 32, 2, 128, 4096])
    w2_d = din("w2", [2, 32, 128, 4096])
    wr_d = din("wr", [2, 128, 16 * 36])
    wk_d = din("wk", [2, 128, 2048])
    wv_d = din("wv", [128, 4096])
    wq_d = din("wq", [NJ, 128, 2048])
    wo_d = din("wo", [NJ, 128, 2048])
    out_d = nc.dram_tensor("out", [TO, D], F32, kind="ExternalOutput").ap()
    dbg_d = {}

    with ExitStack() as es:
        S = Sched(nc, es)
        PE, ACT, DVE, POOL, SP = S.pe, S.act, S.dve, S.pool, S.sp

        def sb(scope, name, shape, dt=F32):
            return scope.enter_context(nc.sbuf_tensor(name, list(shape), dt))

        hO = sb(es, "hO", [128, NJ, TO])
        hbO = sb(es, "hbO", [128, NJ, TO], BF16)
        hbE = sb(es, "hbE", [128, NJ, TE], BF16)
        wsl = [sb(es, f"wsl{i}", [128, 4096], BF16) for i in range(2)]
        wslB = [Buf(f"wsl{i}") for i in range(2)]
        cf = sb(es, "cf_sb", [128, NCF])
        cb = sb(es, "cb_sb", [128, NCB], BF16)
        st_mean = sb(es, "st_mean", [128, 512])
        st_rstd = sb(es, "st_rstd", [128, 512])
        B_mean, B_rstd = Buf("st_mean"), Buf("st_rstd")
        _st0 = (st_mean, B_mean, st_rstd, B_rstd)
        fscr = Rot([(sb(es, f"fs{i}", [128, 512]), Buf(f"fs{i}")) for i in range(5)])
        bscr = Rot([(sb(es, f"bs{i}", [128, 512], BF16), Buf(f"bs{i}")) for i in range(3)])
        esnk = sb(es, "esnk", [128, 16])
        B_esnk = Buf("esnk")
        B_cf, B_cb = Buf("cf"), Buf("cb")

        PS = [es.enter_context(nc.psum_tensor(f"ps{i}", [128, 512], F32)) for i in range(8)]
        PB = [Buf(f"ps{i}") for i in range(8)]

        def prot(idx):
            return Rot([(PS[i], PB[i]) for i in idx])

        HB = {G0: Buf("hb0"), G1: Buf("hb1"), G2: Buf("hb2")}
        H = {(j, g): Buf(f"h{j}_{g[0]}") for j in range(NJ) for g in (G0, G1, G2)}

        def tg_of(c0):
            return G0 if c0 < 176 else (G1 if c0 < 688 else G2)

        def cfv(name, j=None):
            a, b = CF[name]
            if j is None:
                return cf[:, a:b]
            return cf[:, a + j:a + j + 1]

        def cbv(name):
            a, b = CB[name]
            return cb[:, a:b]

        S.dma(SP, cf[:], cf_d[:, :], B_cf, writes=[B_cf])
        S.dma(POOL, cb[:], cb_d[:, :], B_cb, writes=[B_cb])
        S.op(ACT, lambda e: e.activation(out=esnk[:], in_=cfv('snk'), func=AF.Exp),
             reads=[B_cf], writes=[B_esnk])
        ident = cfv('ident')
        pswap = cfv('pswap')
        ones = cbv('ones')

        if 'stop0' in dbg:
            dump_esnk = True
        def dump(name, ap_sb, bufs, shape, dt=F32):
            d = nc.dram_tensor("o_" + name, list(shape), dt, kind="ExternalOutput").ap()
            dbg_d[name] = d
            ob = Buf("dbg_" + name)
            S.dma(SP, d, ap_sb, ob, reads=bufs)
            S.wait_all(SP, [ob])
            S._wait(SP, [(ob.dsem, ob.dcnt)])
            S.barrier()

        if 'stop0' in dbg:
            dump('esnk', esnk[:], [B_esnk], [128, 16])
            dump('cbd', cb[:], [B_cb], [128, NCB], BF16)
            S.barrier()
            raise _Stop(nc, dbg_d)
        def stream_weights(units, src_fn, width):
            issued = set()

            def load(i):
                if i in issued or i >= len(units):
                    return
                issued.add(i)
                k = i % 2
                S.dma(POOL, wsl[k][:, 0:width], src_fn(units[i]), wslB[k], writes=[wslB[k]])
            return load

        def layernorm(tgs, hcol, hbcol, gname, bname, write_hb=True):
            with ExitStack() as sc:
                uid = len(S.dsems) * 1000 + S.act.cnt
                sts = [(st_mean, B_mean, st_rstd, B_rstd),
                       (sb(sc, f"lnm{uid}", [128, 512]), Buf("lnm"), sb(sc, f"lnr{uid}", [128, 512]), Buf("lnr"))]
                vsc = Rot([(sb(sc, f"lnv{uid}_{i}", [128, 512], BF16), Buf(f"lnv{i}")) for i in range(4)])
                p12 = prot([6, 7, 4, 5])

                def stats(ti):
                    tg = tgs[ti]
                    c0, c1 = tg
                    n = c1 - c0
                    P1, B1 = p12.get()
                    P2, B2 = p12.get()
                    for j in range(NJ):
                        if True:
                            vb, Bvb = vsc.get()
                            S.op(ACT, lambda e: e.activation(out=vb[:, 0:n], in_=hcol(j, c0, c1), func=AF.Copy),
                                 reads=[H[(j, tg)]], writes=[Bvb])
                            S.op(PE, lambda e: e.matmul(P1[:, 0:n], lhsT=ones, rhs=vb[:, 0:n], start=(j == 0), stop=(j == NJ - 1)),
                                 reads=[Bvb, B_cb], writes=[B1] if j == 0 else [], accw=[] if j == 0 else [B1])
                        else:
                            S.op(PE, lambda e: e.matmul(P1[:, 0:n], lhsT=cfv('onesf'), rhs=hcol(j, c0, c1), start=(j == 0), stop=(j == NJ - 1)),
                                 reads=[H[(j, tg)], B_cf], writes=[B1] if j == 0 else [], accw=[] if j == 0 else [B1])
                        vq, Bvq = vsc.get()
                        S.op(ACT, lambda e: e.activation(out=vq[:, 0:n], in_=hcol(j, c0, c1), func=AF.Square),
                             reads=[H[(j, tg)]], writes=[Bvq])
                        S.op(PE, lambda e: e.matmul(P2[:, 0:n], lhsT=ones, rhs=vq[:, 0:n], start=(j == 0), stop=(j == NJ - 1)),
                             reads=[Bvq, B_cb], writes=[B2] if j == 0 else [], accw=[] if j == 0 else [B2])
                    ln_stats(P1, B1, P2, B2, n, sts[ti % 2])

                def norm(ti):
                    tg = tgs[ti]
                    c0, c1 = tg
                    n = c1 - c0
                    mean, Bm, rstd, Br = sts[ti % 2]
                    for j in range(NJ):
                        t1, Bt1 = fscr.get()
                        S.op(DVE, lambda e: e.tensor_tensor(out=t1[:, 0:n], in0=hcol(j, c0, c1), in1=mean[:, 0:n], op=ALU.subtract),
                             reads=[H[(j, tg)], Bm], writes=[Bt1])
                        t2, Bt2 = fscr.get()
                        S.op(DVE, lambda e: e.scalar_tensor_tensor(out=t2[:, 0:n], in0=t1[:, 0:n], scalar=cfv(gname, j), in1=rstd[:, 0:n], op0=ALU.mult, op1=ALU.mult),
                             reads=[Bt1, Br, B_cf], writes=[Bt2])
                        S.op(ACT, lambda e: e.activation(out=hcol(j, c0, c1), in_=t2[:, 0:n], func=AF.Identity, bias=cfv(bname, j)),
                             reads=[Bt2, B_cf], writes=[H[(j, tg)]])
                        if write_hb:
                            S.op(DVE, lambda e: e.tensor_scalar(out=hbcol(j, c0, c1), in0=t2[:, 0:n], scalar1=cfv(bname, j), scalar2=None, op0=ALU.add),
                                 reads=[Bt2, B_cf], accw=[HB[tg]])

                stats(0)
                for ti in range(len(tgs)):
                    if ti + 1 < len(tgs):
                        stats(ti + 1)
                    norm(ti)
                S.barrier()

        def ln_stats(P1, B1, P2, B2, n, st=None):
            st_mean, B_mean, st_rstd, B_rstd = st if st is not None else _st0
            S.op(DVE, lambda e: e.tensor_scalar(out=st_mean[:, 0:n], in0=P1[:, 0:n], scalar1=1.0 / D, scalar2=None, op0=ALU.mult),
                 reads=[B1], writes=[B_mean])
            t1, Bt1 = fscr.get()
            S.op(DVE, lambda e: e.tensor_tensor(out=t1[:, 0:n], in0=st_mean[:, 0:n], in1=st_mean[:, 0:n], op=ALU.mult),
                 reads=[B_mean], writes=[Bt1])
            t2, Bt2 = fscr.get()
            S.op(DVE, lambda e: e.scalar_tensor_tensor(out=t2[:, 0:n], in0=P2[:, 0:n], scalar=1.0 / D, in1=t1[:, 0:n], op0=ALU.mult, op1=ALU.subtract),
                 reads=[B2, Bt1], writes=[Bt2])
            t3, Bt3 = fscr.get()
            S.op(ACT, lambda e: e.activation(out=t3[:, 0:n], in_=t2[:, 0:n], func=AF.Sqrt, bias=EPS, scale=1.0),
                 reads=[Bt2], writes=[Bt3])
            S.op(DVE, lambda e: e.reciprocal(out=st_rstd[:, 0:n], in_=t3[:, 0:n]),
                 reads=[Bt3], writes=[B_rstd])

        def proj_residual(tgs, wsrc, bias_name, hcol, hbcol):
            units = list(range(NJ))
            load = stream_weights(units, lambda j: wsrc[j], 2048)
            pr = prot([0, 1, 2, 3])
            load(0)
            for j in units:
                load(j + 1)
                w = wsl[j % 2][:, 0:2048].rearrange("p (k m) -> p k m", k=NJ)
                for tg in tgs:
                    c0, c1 = tg
                    n = c1 - c0
                    P, Bp = pr.get()
                    S.group(PE, [lambda e, kc=kc: e.matmul(P[:, 0:n], lhsT=w[:, kc, :], rhs=hbcol(kc, c0, c1), start=(kc == 0), stop=(kc == NJ - 1)) for kc in range(NJ)],
                            reads=[wslB[j % 2], HB[tg]], writes=[Bp])
                    t, Bt = fscr.get()
                    S.op(ACT, lambda e, t=t, P=P: e.activation(out=t[:, 0:n], in_=P[:, 0:n], func=AF.Identity, bias=cfv(bias_name, j)),
                         reads=[Bp, B_cf], writes=[Bt])
                    S.op(DVE, lambda e, t=t: e.scalar_tensor_tensor(out=hcol(j, c0, c1), in0=hcol(j, c0, c1), scalar=ALPHA, in1=t[:, 0:n], op0=ALU.mult, op1=ALU.add),
                         reads=[Bt], writes=[H[(j, tg)]])

        def moe(layer, tgs, tts, hcol, hbcol, scope):
            wrt = sb(scope, f"wrt{layer}", [128, NJ, 36])
            B_wrt = Buf(f"wrt{layer}")
            sel = sb(scope, f"sel_sb{layer}", [64, 32, 128], BF16)
            B_sel = Buf(f"sel{layer}")
            gT = sb(scope, f"gT{layer}", [64, T], BF16)
            B_gT = {tg: Buf(f"gT{layer}_{tg[0]}") for tg in tgs}
            gball = sb(scope, f"gball{layer}", [128, T])
            B_gb = {tg: Buf(f"gb{layer}_{tg[0]}") for tg in tgs}
            hid = sb(scope, f"hid{layer}", [128, 2, 2, T], BF16)
            B_hid = [Buf(f"hid{layer}_0"), Buf(f"hid{layer}_1")]
            w2s = [sb(scope, f"w2s{layer}_{i}", [128, 2, D], BF16) for i in range(2)]
            B_w2 = [Buf(f"w2s{layer}_0"), Buf(f"w2s{layer}_1")]
            lg = sb(scope, f"lg{layer}", [128, 36])
            B_lg = Buf(f"lg{layer}")
            S.dma(SP, wrt[:].rearrange("p k m -> p (k m)"), wr_d[layer], B_wrt, writes=[B_wrt])
            S.dma(POOL, sel[:].rearrange("p e m -> p (e m)"), sel_d[:, :], B_sel, writes=[B_sel])
            brb = cfv('brb%d' % layer)
            units = [(e, fc) for e in range(32) for fc in range(2)]
            load = stream_weights(units, lambda u: w13_d[layer, u[0], u[1]], 4096)
            load(0)
            NT = len(tts)
            pr = prot([6, 7])

            def wt(name, shape, dt=F32):
                return sb(scope, f"g{layer}_{name}", shape, dt), Buf(f"g{layer}_{name}")
            lgall, B_lga = wt("lgall", [128, NT, 36])
            S.op(POOL, lambda e: e.memset(lgall[:].rearrange("p t m -> p (t m)"), 0.0), writes=[B_lga])
            for ti, (c0, c1) in enumerate(tts):
                n = c1 - c0
                tg = tg_of(c0)
                P, Bp = pr.get()
                S.group(PE, [lambda e, kc=kc: e.matmul(P[0:n, 0:36], lhsT=hcol(kc, c0, c1), rhs=wrt[:, kc, :], start=(kc == 0), stop=(kc == NJ - 1)) for kc in range(NJ)],
                        reads=[B_wrt] + [H[(kc, tg)] for kc in range(NJ)], writes=[Bp])
                S.op(DVE, lambda e: e.tensor_tensor(out=lgall[0:n, ti, :], in0=P[0:n, 0:36], in1=brb[0:n, :], op=ALU.add),
                     reads=[Bp, B_cf], accw=[B_lga])
            lg4 = lgall[:, :, 0:4]
            le = lgall[:, :, 4:36]
            gmax, Bgmax = wt("gmax", [128, NT])
            S.op(DVE, lambda e: e.tensor_reduce(out=gmax[:], in_=lg4, axis=AX.X, op=ALU.max), reads=[B_lga], writes=[Bgmax])
            gd, Bgd = wt("gd", [128, NT, 4])
            S.op(DVE, lambda e: e.tensor_tensor(out=gd[:], in0=lg4, in1=gmax[:].unsqueeze(2).broadcast_to([128, NT, 4]), op=ALU.subtract),
                 reads=[B_lga, Bgmax], writes=[Bgd])
            gexp, Bgexp = wt("gexp", [128, NT, 4])
            S.op(ACT, lambda e: e.activation(out=gexp[:], in_=gd[:], func=AF.Exp), reads=[Bgd], writes=[Bgexp])
            gsum, Bgsum = wt("gsum", [128, NT])
            S.op(DVE, lambda e: e.tensor_reduce(out=gsum[:], in_=gexp[:], axis=AX.X, op=ALU.add), reads=[Bgexp], writes=[Bgsum])
            gw, Bgw = wt("gw", [128, NT])
            S.op(DVE, lambda e: e.reciprocal(out=gw[:], in_=gsum[:]), reads=[Bgsum], writes=[Bgw])
            pen, Bpen = wt("pen", [128, NT, 4])
            S.op(DVE, lambda e: e.tensor_scalar(out=pen[:], in0=gd[:], scalar1=0.0, scalar2=None, op0=ALU.is_equal), reads=[Bgd], writes=[Bpen])
            S.op(DVE, lambda e: e.tensor_scalar(out=pen[:], in0=pen[:], scalar1=BIG, scalar2=-BIG, op0=ALU.mult, op1=ALU.add), reads=[Bpen], writes=[Bpen])
            lem, Blem = wt("lem", [128, NT, 32])
            S.op(DVE, lambda e: e.tensor_tensor(out=lem[:].rearrange("p t (g k) -> p t g k", g=4), in0=le.rearrange("p t (g k) -> p t g k", g=4),
                                                in1=pen[:].unsqueeze(3).broadcast_to([128, NT, 4, 8]), op=ALU.add),
                 reads=[B_lga, Bpen], writes=[Blem])
            m1, Bm1 = wt("m1", [128, NT])
            S.op(DVE, lambda e: e.tensor_reduce(out=m1[:], in_=lem[:], axis=AX.X, op=ALU.max), reads=[Blem], writes=[Bm1])
            eq1, Beq1 = wt("eq1", [128, NT, 32])
            S.op(DVE, lambda e: e.tensor_tensor(out=eq1[:], in0=lem[:], in1=m1[:].unsqueeze(2).broadcast_to([128, NT, 32]), op=ALU.is_equal),
                 reads=[Blem, Bm1], writes=[Beq1])
            lem2, Blem2 = wt("lem2", [128, NT, 32])
            S.op(DVE, lambda e: e.scalar_tensor_tensor(out=lem2[:], in0=eq1[:], scalar=-BIG, in1=lem[:], op0=ALU.mult, op1=ALU.add),
                 reads=[Beq1, Blem], writes=[Blem2])
            m2, Bm2 = wt("m2", [128, NT])
            S.op(DVE, lambda e: e.tensor_reduce(out=m2[:], in_=lem2[:], axis=AX.X, op=ALU.max), reads=[Blem2], writes=[Bm2])
            eq2, Beq2 = wt("eq2", [128, NT, 32])
            S.op(DVE, lambda e: e.tensor_tensor(out=eq2[:], in0=lem2[:], in1=m2[:].unsqueeze(2).broadcast_to([128, NT, 32]), op=ALU.is_equal),
                 reads=[Blem2, Bm2], writes=[Beq2])
            dm, Bdm = wt("dm", [128, NT])
            S.op(DVE, lambda e: e.tensor_tensor(out=dm[:], in0=m1[:], in1=m2[:], op=ALU.subtract), reads=[Bm1, Bm2], writes=[Bdm])
            sg, Bsg = wt("sg", [128, NT])
            S.op(ACT, lambda e: e.activation(out=sg[:], in_=dm[:], func=AF.Sigmoid), reads=[Bdm], writes=[Bsg])
            wa, Bwa = wt("wa", [128, NT])
            S.op(DVE, lambda e: e.tensor_tensor(out=wa[:], in0=gw[:], in1=sg[:], op=ALU.mult), reads=[Bgw, Bsg], writes=[Bwa])
            wb, Bwb = wt("wb", [128, NT])
            S.op(DVE, lambda e: e.tensor_tensor(out=wb[:], in0=gw[:], in1=wa[:], op=ALU.subtract), reads=[Bgw, Bwa], writes=[Bwb])
            S.op(DVE, lambda e: e.tensor_tensor(out=eq1[:], in0=eq1[:], in1=wa[:].unsqueeze(2).broadcast_to([128, NT, 32]), op=ALU.mult),
                 reads=[Bwa], writes=[Beq1])
            S.op(DVE, lambda e: e.tensor_tensor(out=eq2[:], in0=eq2[:], in1=wb[:].unsqueeze(2).broadcast_to([128, NT, 32]), op=ALU.mult),
                 reads=[Bwb], writes=[Beq2])
            gate, Bgate = lem, Blem
            S.op(DVE, lambda e: e.tensor_tensor(out=gate[:], in0=eq1[:], in1=eq2[:], op=ALU.add), reads=[Beq1, Beq2], writes=[Bgate])
            if ('gate%d' % layer) in dbg:
                dump(f"gate{layer}", gate[:].rearrange("p t m -> p (t m)"), [Bgate], [128, NT * 32])
            hib, Bhib = wt("hib", [128, NT, 32], BF16)
            S.op(DVE, lambda e: e.tensor_copy(out=hib[:], in_=gate[:]), reads=[Bgate], writes=[Bhib])
            ghl, Bghl = wt("ghl", [128, NT, 64])
            S.op(DVE, lambda e: e.tensor_copy(out=ghl[:, :, 0:32], in_=hib[:]), reads=[Bhib], writes=[Bghl])
            S.op(DVE, lambda e: e.tensor_tensor(out=ghl[:, :, 32:64], in0=gate[:], in1=hib[:], op=ALU.subtract),
                 reads=[Bgate, Bhib], accw=[Bghl])
            for ti, (c0, c1) in enumerate(tts):
                n = c1 - c0
                tg = tg_of(c0)
                PT, Bpt = pr.get()
                S.op(PE, lambda e: e.transpose(PT[0:64, 0:n], ghl[0:n, ti, :], ident[0:n, 0:n]),
                     reads=[Bghl, B_cf], writes=[Bpt])
                S.op(ACT, lambda e: e.copy(gT[:, c0:c1], PT[0:64, 0:n]), reads=[Bpt], accw=[B_gT[tg]])

            pA = prot([0, 1])
            pB = prot([2, 3])
            pG = prot([4])
            pD = prot([5, 6, 7])
            for ui, (e_, fc) in enumerate(units):
                load(ui + 1)
                es_ = e_ % 2
                w13 = wsl[ui % 2][:, 0:4096].rearrange("p (k h m) -> p k h m", k=NJ, h=2)
                if fc == 0:
                    S.dma(POOL, w2s[es_][:].rearrange("p f d -> p (f d)"), w2_d[layer, e_], B_w2[es_], writes=[B_w2[es_]])
                for tg in tgs:
                    c0, c1 = tg
                    n = c1 - c0
                    if fc == 0:
                        PG, Bpg = pG.get()
                        S.op(PE, lambda e: e.matmul(PG[:, 0:n], lhsT=sel[:, e_, :], rhs=gT[:, c0:c1], start=True, stop=True),
                             reads=[B_sel, B_gT[tg]], writes=[Bpg])
                        S.op(ACT, lambda e: e.copy(gball[:, c0:c1], PG[:, 0:n]), reads=[Bpg], writes=[B_gb[tg]])
                    PA, Bpa = pA.get()
                    S.group(PE, [lambda e, kc=kc: e.matmul(PA[:, 0:n], lhsT=w13[:, kc, 0, :], rhs=hbcol(kc, c0, c1), start=(kc == 0), stop=(kc == NJ - 1)) for kc in range(NJ)],
                            reads=[wslB[ui % 2], HB[tg]], writes=[Bpa])
                    PBk, Bpb = pB.get()
                    S.group(PE, [lambda e, kc=kc: e.matmul(PBk[:, 0:n], lhsT=w13[:, kc, 1, :], rhs=hbcol(kc, c0, c1), start=(kc == 0), stop=(kc == NJ - 1)) for kc in range(NJ)],
                            reads=[wslB[ui % 2], HB[tg]], writes=[Bpb])
                    sa, Bsa = fscr.get()
                    S.op(ACT, lambda e: e.activation(out=sa[:, 0:n], in_=PA[:, 0:n], func=AF.Silu), reads=[Bpa], writes=[Bsa])
                    t, Bt = fscr.get()
                    S.op(DVE, lambda e: e.tensor_tensor(out=t[:, 0:n], in0=sa[:, 0:n], in1=gball[:, c0:c1], op=ALU.mult),
                         reads=[Bsa, B_gb[tg]], writes=[Bt])
                    S.op(DVE, lambda e: e.tensor_tensor(out=hid[:, es_, fc, c0:c1], in0=PBk[:, 0:n], in1=t[:, 0:n], op=ALU.mult),
                         reads=[Bpb, Bt], accw=[B_hid[es_]])
                if fc == 1 and es_ == 1:
                    first = (e_ == 1)
                    for j in range(NJ):
                        for tg in tgs:
                            c0, c1 = tg
                            n = c1 - c0
                            PD, Bpd = pD.get()
                            fns = []
                            for k, (ee, ff) in enumerate([(0, 0), (0, 1), (1, 0), (1, 1)]):
                                fns.append(lambda e, ee=ee, ff=ff, k=k: e.matmul(PD[:, 0:n], lhsT=w2s[ee][:, ff, j * 128:(j + 1) * 128], rhs=hid[:, ee, ff, c0:c1], start=(k == 0), stop=(k == 3)))
                            S.group(PE, fns, reads=[B_w2[0], B_w2[1], B_hid[0], B_hid[1]], writes=[Bpd])
                            if first:
                                S.op(DVE, lambda e: e.scalar_tensor_tensor(out=hcol(j, c0, c1), in0=hcol(j, c0, c1), scalar=ALPHA, in1=PD[:, 0:n], op0=ALU.mult, op1=ALU.add),
                                     reads=[Bpd], writes=[H[(j, tg)]])
                            else:
                                S.op(DVE, lambda e: e.tensor_tensor(out=hcol(j, c0, c1), in0=hcol(j, c0, c1), in1=PD[:, 0:n], op=ALU.add),
                                     reads=[Bpd], writes=[H[(j, tg)]])

        with ExitStack() as L0:
            hE = sb(L0, "hE", [128, NJ, TE])

            def hcol(j, c0, c1):
                return hE[:, j, c0:c1] if c1 <= TE else hO[:, j, c0 - TE:c1 - TE]

            def hbcol(j, c0, c1):
                return hbE[:, j, c0:c1] if c1 <= TE else hbO[:, j, c0 - TE:c1 - TE]

            def hcol4(j0, c0, c1):
                return hE[:, j0:j0 + 4, c0:c1] if c1 <= TE else hO[:, j0:j0 + 4, c0 - TE:c1 - TE]

            def hbcol4(j0, c0, c1):
                return hbE[:, j0:j0 + 4, c0:c1] if c1 <= TE else hbO[:, j0:j0 + 4, c0 - TE:c1 - TE]

            with ExitStack() as PA_:
                xs = [sb(PA_, f"xs{i}", [128, D]) for i in range(2)]
                xsB = [Buf(f"xs{i}") for i in range(2)]
                pr = prot([0, 1, 2, 3])
                tts = TT_E + TT_O
                S.dma(SP, xs[0][0:128, :], xe_d[0:128, :], xsB[0], writes=[xsB[0]])
                for ti, (c0, c1) in enumerate(tts):
                    n = c1 - c0
                    if ti + 1 < len(tts):
                        a0, a1 = tts[ti + 1]
                        S.dma(SP, xs[(ti + 1) % 2][0:a1 - a0, :], xe_d[a0:a1, :], xsB[(ti + 1) % 2], writes=[xsB[(ti + 1) % 2]])
                    x_t = xs[ti % 2]
                    tg = tg_of(c0)
                    for jq in range(4):
                        P, Bp = pr.get()
                        Pv = P[:, :].rearrange("p (a b) -> p a b", a=4)
                        S.group(PE, [lambda e, i=i: e.transpose(Pv[:, i, 0:n], x_t[0:n, (jq * 4 + i) * 128:(jq * 4 + i + 1) * 128], ident[0:n, 0:n]) for i in range(4)],
                                reads=[xsB[ti % 2], B_cf], writes=[Bp])
                        S.op(ACT, lambda e: e.copy(hcol4(jq * 4, c0, c1), Pv[:, :, 0:n]),
                             reads=[Bp], writes=[H[(jq * 4 + i, tg)] for i in range(4)] if False else [], accw=[H[(jq * 4 + i, tg)] for i in range(4)])
                        S.op(DVE, lambda e: e.tensor_copy(out=hbcol4(jq * 4, c0, c1), in_=hcol4(jq * 4, c0, c1)),
                             reads=[H[(jq * 4 + i, tg)] for i in range(4)], accw=[HB[tg]])
                S.barrier()
            def stop(flag):
                if flag in dbg:
                    S.barrier()
                    raise _Stop(nc, dbg_d)
            if 'h0' in dbg:
                dump("h0E", hE[:].rearrange("p j t -> p (j t)"), [], [128, NJ * TE])
                dump("h0O", hO[:].rearrange("p j t -> p (j t)"), [], [128, NJ * TO])

            stop('stopA')
            with ExitStack() as PB_:
                z = sb(PB_, "z", [128, NJ, T], BF16)
                Bz = {(j, g): Buf(f"z{j}_{g[0]}") for j in range(NJ) for g in (G0, G1, G2)}
                yb = [sb(PB_, f"yb{i}", [128, T + 30], BF16) for i in range(2)]
                Byb = [Buf("yb0"), Buf("yb1")]
                diag = sb(PB_, "diag", [128, 31, 128], BF16)
                Bdiag = Buf("diag")
                for i in range(2):
                    S.op(DVE, lambda e, i=i: e.memset(yb[i][:, 0:30], 0.0), writes=[Byb[i]])
                units = list(range(NJ))
                load = stream_weights(units, lambda j: win_d[j], 4096)
                pA = prot([0, 1])
                pB = prot([2, 3])
                pC = prot([4, 5])
                load(0)
                pending = []

                def conv(j, tg):
                    c0, c1 = tg
                    n = c1 - c0
                    y = yb[j % 2]
                    PC, Bpc = pC.get()
                    S.group(PE, [lambda e, k=k: e.matmul(PC[:, 0:n], lhsT=diag[:, k, :], rhs=y[:, c0 + k:c0 + k + n], start=(k == 0), stop=(k == 30)) for k in range(31)],
                            reads=[Bdiag, Byb[j % 2]], writes=[Bpc])
                    S.op(ACT, lambda e: e.activation(out=z[:, j, c0:c1], in_=PC[:, 0:n], func=AF.Identity, bias=cfv('bdw', j)),
                         reads=[Bpc, B_cf], writes=[Bz[(j, tg)]])

                for j in units:
                    load(j + 1)
                    w = wsl[j % 2][:, 0:4096].rearrange("p (k h m) -> p k h m", k=NJ, h=2)
                    y = yb[j % 2]
                    for tg in (G0, G1, G2):
                        c0, c1 = tg
                        n = c1 - c0
                        PA, Bpa = pA.get()
                        S.group(PE, [lambda e, kc=kc: e.matmul(PA[:, 0:n], lhsT=w[:, kc, 0, :], rhs=hbcol(kc, c0, c1), start=(kc == 0), stop=(kc == NJ - 1)) for kc in range(NJ)],
                                reads=[wslB[j % 2], HB[tg]], writes=[Bpa])
                        PBk, Bpb = pB.get()
                        S.group(PE, [lambda e, kc=kc: e.matmul(PBk[:, 0:n], lhsT=w[:, kc, 1, :], rhs=hbcol(kc, c0, c1), start=(kc == 0), stop=(kc == NJ - 1)) for kc in range(NJ)],
                                reads=[wslB[j % 2], HB[tg]], writes=[Bpb])
                        if pending:
                            pj, ptg = pending.pop(0)
                            conv(pj, ptg)
                        if tg == G0:
                            a, b = CF['wdw']
                            wd = cf[:, a + j * 31:a + (j + 1) * 31]
                            S.op(DVE, lambda e: e.tensor_tensor(out=diag[:], in0=cbv('identb').unsqueeze(1).broadcast_to([128, 31, 128]),
                                                                in1=wd.unsqueeze(2).broadcast_to([128, 31, 128]), op=ALU.mult),
                                 reads=[B_cb, B_cf], writes=[Bdiag])
                        sg, Bsg = fscr.get()
                        S.op(ACT, lambda e: e.activation(out=sg[:, 0:n], in_=PBk[:, 0:n], func=AF.Sigmoid, bias=cfv('b2', j)),
                             reads=[Bpb, B_cf], writes=[Bsg])
                        if tg == G0:
                            S.op(DVE, lambda e: e.scalar_tensor_tensor(out=y[:, 30 + c0:30 + c1], in0=PA[:, 0:n], scalar=cfv('b1', j), in1=sg[:, 0:n], op0=ALU.add, op1=ALU.mult),
                                 reads=[Bpa, Bsg, B_cf], writes=[Byb[j % 2]])
                            S.op(DVE, lambda e: e.tensor_tensor(out=y[:, 30 + 16:30 + 176], in0=y[:, 30 + 16:30 + 176], in1=cfv('padmask'), op=ALU.mult),
                                 reads=[B_cf], writes=[Byb[j % 2]])
                        else:
                            S.op(DVE, lambda e: e.scalar_tensor_tensor(out=y[:, 30 + c0:30 + c1], in0=PA[:, 0:n], scalar=cfv('b1', j), in1=sg[:, 0:n], op0=ALU.add, op1=ALU.mult),
                                 reads=[Bpa, Bsg, B_cf], accw=[Byb[j % 2]])
                        pending.append((j, tg))
                while pending:
                    pj, ptg = pending.pop(0)
                    conv(pj, ptg)
                if 'z' in dbg:
                    dump("z", z[:].rearrange("p j t -> p (j t)"), list(Bz.values()), [128, NJ * T], BF16)

                p12 = prot([6, 7, 0, 1])
                for tg in (G0, G1, G2):
                    c0, c1 = tg
                    n = c1 - c0
                    P1, B1 = p12.get()
                    P2, B2 = p12.get()
                    S.group(PE, [lambda e, j=j: e.matmul(P1[:, 0:n], lhsT=ones, rhs=z[:, j, c0:c1], start=(j == 0), stop=(j == NJ - 1)) for j in range(NJ)],
                            reads=[B_cb] + [Bz[(j, tg)] for j in range(NJ)], writes=[B1])
                    for j in range(NJ):
                        vq, Bvq = bscr.get()
                        S.op(ACT, lambda e, vq=vq, j=j: e.activation(out=vq[:, 0:n], in_=z[:, j, c0:c1], func=AF.Square),
                             reads=[Bz[(j, tg)]], writes=[Bvq])
                        S.op(PE, lambda e, vq=vq, j=j: e.matmul(P2[:, 0:n], lhsT=ones, rhs=vq[:, 0:n], start=(j == 0), stop=(j == NJ - 1)),
                             reads=[Bvq, B_cb], writes=[B2] if j == 0 else [], accw=[] if j == 0 else [B2])
                    ln_stats(P1, B1, P2, B2, n)
                    for j in range(NJ):
                        t1, Bt1 = fscr.get()
                        S.op(DVE, lambda e, t1=t1, j=j: e.tensor_tensor(out=t1[:, 0:n], in0=z[:, j, c0:c1], in1=st_mean[:, 0:n], op=ALU.subtract),
                             reads=[Bz[(j, tg)], B_mean], writes=[Bt1])
                        t2, Bt2 = fscr.get()
                        S.op(DVE, lambda e, t1=t1, t2=t2, j=j: e.scalar_tensor_tensor(out=t2[:, 0:n], in0=t1[:, 0:n], scalar=cfv('clng', j), in1=st_rstd[:, 0:n], op0=ALU.mult, op1=ALU.mult),
                             reads=[Bt1, B_rstd, B_cf], writes=[Bt2])
                        S.op(ACT, lambda e, t2=t2, j=j: e.activation(out=hbcol(j, c0, c1), in_=t2[:, 0:n], func=AF.Silu, bias=cfv('clnb', j)),
                             reads=[Bt2, B_cf], writes=[HB[tg]] if j == 0 else [], accw=[] if j == 0 else [HB[tg]])
                S.barrier()
            if 'a' in dbg:
                dump("aE", hbE[:].rearrange("p j t -> p (j t)"), list(HB.values()), [128, NJ * TE], BF16)
                dump("aO", hbO[:].rearrange("p j t -> p (j t)"), list(HB.values()), [128, NJ * TO], BF16)

            stop('stopC')
            proj_residual((G0, G1, G2), wout_d, 'bout', hcol, hbcol)
            layernorm((G0, G1, G2), hcol, hbcol, 'mixg0', 'mixb0')
            if 'h1' in dbg:
                S.barrier()
                dump("h1E", hE[:].rearrange("p j t -> p (j t)"), [], [128, NJ * TE])
                dump("h1O", hO[:].rearrange("p j t -> p (j t)"), [], [128, NJ * TO])

            stop('stopD')
            if 'stop1' not in dbg:
                with ExitStack() as PE_:
                    moe(0, (G0, G1, G2), TT_E + TT_O, hcol, hbcol, PE_)
                    S.barrier()
                    if 'pre2' in dbg:
                        dump("pre2E", hE[:].rearrange("p j t -> p (j t)"), [], [128, NJ * TE])
                        dump("pre2O", hO[:].rearrange("p j t -> p (j t)"), [], [128, NJ * TO])
                layernorm((G0, G1, G2), hcol, hbcol, 'ffng0', 'ffnb0')
            S.barrier()
            if 'h2' in dbg:
                dump("h2E", hE[:].rearrange("p j t -> p (j t)"), [], [128, NJ * TE])
                dump("h2O", hO[:].rearrange("p j t -> p (j t)"), [], [128, NJ * TO])

        def hcol(j, c0, c1):
            return hO[:, j, c0 - TE:c1 - TE]

        def hbcol(j, c0, c1):
            return hbE[:, j, c0:c1] if c1 <= TE else hbO[:, j, c0 - TE:c1 - TE]

        if 'stop2' not in dbg:
            with ExitStack() as L1:
                KT = sb(L1, "KT", [128, 2, T], BF16)
                B_KT = Buf("KT")
                V = sb(L1, "V", [128, 10, 256], BF16)
                B_V = Buf("V")
                with ExitStack() as LQ:
                    QT = sb(LQ, "QT", [128, NJ, TO], BF16)
                    B_QT = {g: Buf(f"QT{g[0]}") for g in (G1, G2)}
                    with ExitStack() as LCS:
                        cs = sb(LCS, "cs_sb", [128, 2, T])
                        B_cs = Buf("cs")
                        S.dma(SP, cs[:].rearrange("p a t -> p (a t)"), cs_d[:, :], B_cs, writes=[B_cs])
                        pr = prot([0, 1, 2, 3])
                        pr2 = prot([4, 5])

                        def rope_evac(P, Bp, n, c0, c1, bias_ap, dst_ap, dstB, acc=True):
                            raw, Braw = fscr.get()
                            S.op(ACT, lambda e: e.activation(out=raw[:, 0:n], in_=P[:, 0:n], func=AF.Identity, bias=bias_ap),
                                 reads=[Bp, B_cf], writes=[Braw])
                            P2, Bp2 = pr2.get()
                            S.op(PE, lambda e: e.matmul(P2[:, 0:n], lhsT=pswap, rhs=raw[:, 0:n], start=True, stop=True),
                                 reads=[Braw, B_cf], writes=[Bp2])
                            t1, Bt1 = fscr.get()
                            S.op(DVE, lambda e: e.tensor_tensor(out=t1[:, 0:n], in0=raw[:, 0:n], in1=cs[:, 0, c0:c1], op=ALU.mult),
                                 reads=[Braw, B_cs], writes=[Bt1])
                            t2, Bt2 = fscr.get()
                            S.op(DVE, lambda e: e.tensor_tensor(out=t2[:, 0:n], in0=P2[:, 0:n], in1=cs[:, 1, c0:c1], op=ALU.mult),
                                 reads=[Bp2, B_cs], writes=[Bt2])
                            S.op(DVE, lambda e: e.tensor_tensor(out=dst_ap, in0=t1[:, 0:n], in1=t2[:, 0:n], op=ALU.add),
                                 reads=[Bt1, Bt2], accw=[dstB])

                        units = [0, 1]
                        load = stream_weights(units, lambda g: wk_d[g], 2048)
                        load(0)
                        for gp in units:
                            load(gp + 1)
                            w = wsl[gp % 2][:, 0:2048].rearrange("p (k m) -> p k m", k=NJ)
                            for (c0, c1) in [(0, 16), (48, 176), G1, G2]:
                                n = c1 - c0
                                P, Bp = pr.get()
                                S.group(PE, [lambda e, kc=kc: e.matmul(P[:, 0:n], lhsT=w[:, kc, :], rhs=hbcol(kc, c0, c1), start=(kc == 0), stop=(kc == NJ - 1)) for kc in range(NJ)],
                                        reads=[wslB[gp % 2], HB[tg_of(c0)]], writes=[Bp])
                                a, b = CF['bk']
                                rope_evac(P, Bp, n, c0, c1, cf[:, a + gp:a + gp + 1], KT[:, gp, c0:c1], B_KT)
                        S.dma(POOL, wsl[0][:, 0:4096], wv_d[:, :], wslB[0], writes=[wslB[0]])
                        wv = wsl[0][:, 0:4096].rearrange("p (k m) -> p k m", k=NJ)
                        vts = [(0, 16)] + [(48 + 128 * m, 48 + 128 * (m + 1)) for m in range(9)]
                        for vi, (c0, c1) in enumerate(vts):
                            n = c1 - c0
                            P, Bp = pr.get()
                            S.group(PE, [lambda e, kc=kc: e.matmul(P[0:n, 0:256], lhsT=hbcol(kc, c0, c1), rhs=wv[:, kc, :], start=(kc == 0), stop=(kc == NJ - 1)) for kc in range(NJ)],
                                    reads=[wslB[0], HB[tg_of(c0)]], writes=[Bp])
                            S.op(DVE, lambda e: e.tensor_tensor(out=V[0:n, vi, :], in0=P[0:n, 0:256], in1=cfv('bvb')[0:n, :], op=ALU.add),
                                 reads=[Bp, B_cf], accw=[B_V])
                        units = list(range(NJ))
                        load = stream_weights(units, lambda j: wq_d[j], 2048)
                        load(0)
                        for j in units:
                            load(j + 1)
                            w = wsl[j % 2][:, 0:2048].rearrange("p (k m) -> p k m", k=NJ)
                            for tg in (G1, G2):
                                c0, c1 = tg
                                n = c1 - c0
                                P, Bp = pr.get()
                                S.group(PE, [lambda e, kc=kc: e.matmul(P[:, 0:n], lhsT=w[:, kc, :], rhs=hbcol(kc, c0, c1), start=(kc == 0), stop=(kc == NJ - 1)) for kc in range(NJ)],
                                        reads=[wslB[j % 2], HB[tg]], writes=[Bp])
                                rope_evac(P, Bp, n, c0, c1, cfv('bq', j), QT[:, j, c0 - TE:c1 - TE], B_QT[tg])
                        S.barrier()
                    if 'qkv' in dbg:
                        dump("KT", KT[:].rearrange("p g t -> p (g t)"), [], [128, 2 * T], BF16)
                        dump("V", V[:].rearrange("p g t -> p (g t)"), [], [128, 10 * 256], BF16)
                        dump("QT", QT[:].rearrange("p g t -> p (g t)"), [], [128, NJ * TO], BF16)

                    Eo = Rot([(sb(LQ, f"Eo{i}", [128, 512], BF16), Buf(f"Eo{i}")) for i in range(3)])
                    Ep = Rot([(sb(LQ, f"Ep{i}", [128, 512], BF16), Buf(f"Ep{i}")) for i in range(3)])
                    Em = Rot([(sb(LQ, f"Em{i}", [16, 512], BF16), Buf(f"Em{i}")) for i in range(3)])
                    pS = prot([0, 1, 2, 3, 4, 5])
                    pO = prot([6])
                    pDn = prot([7])
                    ascr = Rot([(sb(LQ, f"as{i}", [128, 512]), Buf(f"as{i}")) for i in range(6)])
                    its = [(gp, half, n_, quad) for gp in range(2) for half in range(2) for n_ in range(8) for quad in range(2)]
                    stS = {}
                    stE = {}

                    def geom(it):
                        gp, half, n_, quad = it
                        r0, r1 = half * 64, half * 64 + 64
                        own = (TE + 128 * n_, TE + 128 * n_ + 128)
                        prv = (48 + 128 * n_, 48 + 128 * n_ + 128)
                        qtg = G1 if n_ < 4 else G2
                        cj = gp * 8 + quad * 4
                        return gp, half, n_, quad, r0, r1, own, prv, qtg, cj

                    def stage_S(k):
                        gp, half, n_, quad, r0, r1, own, prv, qtg, cj = geom(its[k])
                        rhsQ = QT[r0:r1, cj:cj + 4, n_ * 128:(n_ + 1) * 128]
                        PSo, Bso = pS.get()
                        PSp, Bsp = pS.get()
                        PSm, Bsm = pS.get()
                        S.op(PE, lambda e: e.matmul(PSo[:, :], lhsT=KT[r0:r1, gp, own[0]:own[1]], rhs=rhsQ, start=True, stop=True),
                             reads=[B_KT, B_QT[qtg]], writes=[Bso])
                        S.op(PE, lambda e: e.matmul(PSp[:, :], lhsT=KT[r0:r1, gp, prv[0]:prv[1]], rhs=rhsQ, start=True, stop=True),
                             reads=[B_KT, B_QT[qtg]], writes=[Bsp])
                        S.op(PE, lambda e: e.matmul(PSm[0:16, :], lhsT=KT[r0:r1, gp, 0:16], rhs=rhsQ, start=True, stop=True),
                             reads=[B_KT, B_QT[qtg]], writes=[Bsm])
                        stS[k] = (PSo, Bso, PSp, Bsp, PSm, Bsm)

                    def stage_X(k):
                        gp, half, n_, quad, r0, r1, own, prv, qtg, cj = geom(its[k])
                        PSo, Bso, PSp, Bsp, PSm, Bsm = stS.pop(k)
                        eo, Beo = Eo.get()
                        ep, Bep = Ep.get()
                        em, Bem = Em.get()
                        S.op(ACT, lambda e: e.activation(out=eo[:, :], in_=PSo[:, :], func=AF.Exp, scale=0.125), reads=[Bso], writes=[Beo])
                        S.op(ACT, lambda e: e.activation(out=ep[:, :], in_=PSp[:, :], func=AF.Exp, scale=0.125), reads=[Bsp], writes=[Bep])
                        S.op(ACT, lambda e: e.activation(out=em[:, :], in_=PSm[0:16, :], func=AF.Exp, scale=0.125), reads=[Bsm], writes=[Bem])
                        eo3 = eo[:, :].rearrange("p (a b) -> p a b", a=4)
                        ep3 = ep[:, :].rearrange("p (a b) -> p a b", a=4)
                        S.op(DVE, lambda e: e.tensor_tensor(out=eo3, in0=eo3, in1=cbv('m_own').unsqueeze(1).broadcast_to([128, 4, 128]), op=ALU.mult),
                             reads=[B_cb], writes=[Beo])
                        mp = cbv('m_prev0') if n_ == 0 else cbv('m_prev')
                        S.op(DVE, lambda e: e.tensor_tensor(out=ep3, in0=ep3, in1=mp.unsqueeze(1).broadcast_to([128, 4, 128]), op=ALU.mult),
                             reads=[B_cb], writes=[Bep])
                        stE[k] = (eo, Beo, ep, Bep, em, Bem)

                    def stage_V(k):
                        gp, half, n_, quad, r0, r1, own, prv, qtg, cj = geom(its[k])
                        eo, Beo, ep, Bep, em, Bem = stE.pop(k)
                        PO, Bpo = pO.get()
                        PDn, Bpdn = pDn.get()
                        vo, vp = n_ + 2, n_ + 1
                        S.group(PE, [
                            lambda e: e.matmul(PO[:, :], lhsT=V[:, vo, gp * 128:(gp + 1) * 128], rhs=eo[:, :], start=True, stop=False),
                            lambda e: e.matmul(PO[:, :], lhsT=V[:, vp, gp * 128:(gp + 1) * 128], rhs=ep[:, :], start=False, stop=False),
                            lambda e: e.matmul(PO[:, :], lhsT=V[0:16, 0, gp * 128:(gp + 1) * 128], rhs=em[:, :], start=False, stop=True),
                        ], reads=[B_V, Beo, Bep, Bem], writes=[Bpo])
                        S.group(PE, [
                            lambda e: e.matmul(PDn[:, :], lhsT=ones, rhs=eo[:, :], start=True, stop=False),
                            lambda e: e.matmul(PDn[:, :], lhsT=ones, rhs=ep[:, :], start=False, stop=False),
                            lambda e: e.matmul(PDn[:, :], lhsT=ones[0:16, :], rhs=em[:, :], start=False, stop=True),
                        ], reads=[B_cb, Beo, Bep, Bem], writes=[Bpdn])
                        d1, Bd1 = ascr.get()
                        d13 = d1[r0:r1, :].rearrange("p (a b) -> p a b", a=4)
                        S.op(DVE, lambda e: e.tensor_tensor(out=d13, in0=PDn[r0:r1, :].rearrange("p (a b) -> p a b", a=4),
                                                            in1=esnk[r0:r1, cj:cj + 4].unsqueeze(2).broadcast_to([64, 4, 128]), op=ALU.add),
                             reads=[Bpdn, B_esnk], writes=[Bd1])
                        po, Bpos = ascr.get()
                        S.op(ACT, lambda e: e.copy(po[r0:r1, :], PO[r0:r1, :]), reads=[Bpo], writes=[Bpos])
                        S.op(ACT, lambda e: e.activation(out=d1[r0:r1, :], in_=d1[r0:r1, :], func=AF.Ln), reads=[Bd1], writes=[Bd1])
                        S.op(ACT, lambda e: e.activation(out=d1[r0:r1, :], in_=d1[r0:r1, :], func=AF.Exp, scale=-1.0), reads=[Bd1], writes=[Bd1])
                        S.op(DVE, lambda e: e.tensor_tensor(out=hbO[r0:r1, cj:cj + 4, n_ * 128:(n_ + 1) * 128],
                                                            in0=po[r0:r1, :].rearrange("p (a b) -> p a b", a=4),
                                                            in1=d1[r0:r1, :].rearrange("p (a b) -> p a b", a=4), op=ALU.mult),
                             reads=[Bpos, Bd1], accw=[HB[qtg]])

                    stage_S(0)
                    stage_X(0)
                    stage_S(1)
                    stage_X(1)
                    for k in range(len(its)):
                        if k + 2 < len(its):
                            stage_S(k + 2)
                            stage_X(k + 2)
                        stage_V(k)
                    S.barrier()
            if 'att' in dbg:
                dump("attT", hbO[:].rearrange("p j t -> p (j t)"), [], [128, NJ * TO], BF16)
            proj_residual((G1, G2), wo_d, 'bo', hcol, hbcol)
            layernorm((G1, G2), hcol, hbcol, 'mixg1', 'mixb1')
            if 'h3' in dbg:
                S.barrier()
                dump("h3O", hO[:].rearrange("p j t -> p (j t)"), [], [128, NJ * TO])
            if 'stop3' not in dbg:
                with ExitStack() as PM_:
                    moe(1, (G1, G2), TT_O, hcol, hbcol, PM_)
                    S.barrier()
                layernorm((G1, G2), hcol, hbcol, 'ffng1', 'ffnb1', write_hb=False)
            S.barrier()

        with ExitStack() as LO:
            ost = [sb(LO, f"ost{i}", [128, D]) for i in range(2)]
            Bost = [Buf("ost0"), Buf("ost1")]
            pr = prot([0, 1, 2, 3])
            for k in range(8):
                o = ost[k % 2]
                tg = G1 if k < 4 else G2
                for jq in range(4):
                    P, Bp = pr.get()
                    S.group(PE, [lambda e, i=i: e.transpose(P[:, i * 128:(i + 1) * 128], hO[:, jq * 4 + i, k * 128:(k + 1) * 128], ident) for i in range(4)],
                            reads=[B_cf] + [H[(jq * 4 + i, tg)] for i in range(4)], writes=[Bp])
                    eng = ACT if jq % 2 == 0 else DVE
                    if eng is ACT:
                        S.op(ACT, lambda e: e.copy(o[:, jq * 512:(jq + 1) * 512], P[:, :]), reads=[Bp], writes=[Bost[k % 2]] if jq == 0 else [], accw=[] if jq == 0 else [Bost[k % 2]])
                    else:
                        S.op(DVE, lambda e: e.tensor_copy(out=o[:, jq * 512:(jq + 1) * 512], in_=P[:, :]), reads=[Bp], accw=[Bost[k % 2]])
                S.dma(SP, out_d[k * 128:(k + 1) * 128, :], o[:, :], Bost[k % 2], reads=[Bost[k % 2]])
            S.wait_all(SP, Bost)
            S.barrier()
    return nc, dbg_d


def _fm_vec(v):
    return np.ascontiguousarray(np.asarray(v, np.float32).reshape(NJ, 128).T)


def _head_perm():
    idx = []
    for jp in range(NJ):
        gp, i = jp // 8, jp % 8
        for hd in (16 * gp + i, 16 * gp + 8 + i):
            idx.extend(range(hd * 64, hd * 64 + 64))
    return np.array(idx)


def _w_fm(w, ncols_chunk):
    K, C = w.shape
    nch = C // ncols_chunk
    a = w.reshape(NJ, 128, nch, ncols_chunk).transpose(2, 1, 0, 3)
    return np.ascontiguousarray(a).reshape(nch, 128, NJ * ncols_chunk)


def prepare(inputs):
    f = lambda k: np.asarray(inputs[k], np.float32)
    x = f('x')[0]
    meta = f('meta_tokens')
    h0 = np.concatenate([meta, x], 0)
    shared = {}
    w_in = f('conv_w_in')[0]
    wv_ = w_in[:, :D].reshape(NJ, 128, NJ, 128)
    wg_ = w_in[:, D:].reshape(NJ, 128, NJ, 128)
    win = np.stack([wv_, wg_], 3)
    shared['win'] = np.ascontiguousarray(win.transpose(2, 1, 0, 3, 4)).reshape(NJ, 128, 4096)
    shared['wout'] = _w_fm(f('conv_w_out')[0], 128)
    w1 = f('expert_w1')
    w3 = f('expert_w3')
    a1 = w1.reshape(2, 32, NJ, 128, 2, 128)
    a3 = w3.reshape(2, 32, NJ, 128, 2, 128)
    w13 = np.stack([a1, a3], 5)
    shared['w13'] = np.ascontiguousarray(w13.transpose(0, 1, 4, 3, 2, 5, 6)).reshape(2, 32, 2, 128, 4096)
    del w13, a1, a3
    w2 = f('expert_w2').reshape(2, 32, 2, 128, D)
    shared['w2'] = np.ascontiguousarray(w2.transpose(0, 1, 3, 2, 4)).reshape(2, 32, 128, 4096)
    wr = np.concatenate([f('router_group_w'), f('router_expert_w')], -1)
    shared['wr'] = np.ascontiguousarray(wr.reshape(2, NJ, 128, 36).transpose(0, 2, 1, 3)).reshape(2, 128, NJ * 36)
    shared['wk'] = _w_fm(f('w_k'), 128)
    shared['wv'] = _w_fm(f('w_v'), 256)[0]
    perm = _head_perm()
    shared['wq'] = _w_fm(f('w_q')[0][:, perm], 128)
    shared['wo'] = _w_fm(f('w_o')[0][perm, :], 128)
    cfp = np.zeros((128, NCF), np.float32)

    def put(name, arr):
        a, b = CF[name]
        cfp[:, a:b] = arr
    put('ident', np.eye(128, dtype=np.float32))
    put('onesf', np.ones((128, 128), np.float32))
    ps = np.zeros((128, 128), np.float32)
    for m in range(128):
        d = m % 64
        if d < 8:
            ps[m + 8, m] = 1.0
        elif d < 16:
            ps[m - 8, m] = 1.0
    put('pswap', ps)
    b_in = f('conv_b_in')[0]
    put('b1', _fm_vec(b_in[:D]))
    put('b2', _fm_vec(b_in[D:]))
    put('bdw', _fm_vec(f('conv_b_dw')[0]))
    put('clng', _fm_vec(f('conv_ln_g')[0]))
    put('clnb', _fm_vec(f('conv_ln_b')[0]))
    put('bout', _fm_vec(f('conv_b_out')[0]))
    for l in range(2):
        put('mixg%d' % l, _fm_vec(f('ln_mix_g')[l]))
        put('mixb%d' % l, _fm_vec(f('ln_mix_b')[l]))
        put('ffng%d' % l, _fm_vec(f('ln_ffn_g')[l]))
        put('ffnb%d' % l, _fm_vec(f('ln_ffn_b')[l]))
    put('bq', _fm_vec(f('b_q')[0][perm]))
    put('bo', _fm_vec(f('b_o')[0]))
    put('snk', _fm_vec(np.repeat(f('sinks')[0], 64)[perm]))
    put('bk', np.ascontiguousarray(f('b_k').reshape(2, 128).T))
    wdw = f('conv_w_dw')[0]
    put('wdw', np.ascontiguousarray(wdw.reshape(31, NJ, 128).transpose(2, 1, 0)).reshape(128, NJ * 31))
    for l in range(2):
        br = np.concatenate([f('router_group_b')[l], f('router_expert_b')[l]])
        put('brb%d' % l, np.broadcast_to(br[None, :], (128, 36)))
    put('bvb', np.broadcast_to(f('b_v')[None, :], (128, 256)))
    kk = np.arange(128)[:, None]
    qq = np.arange(128)[None, :]
    m_own = (kk <= qq).astype(np.float32)
    m_prev = (kk > qq).astype(np.float32)
    sel = np.zeros((64, 32, 128), np.float32)
    for e in range(32):
        sel[e, e, :] = 1.0
        sel[32 + e, e, :] = 1.0
    shared['sel'] = sel.reshape(64, 32 * 128)
    inv_freq = (np.float32(500000.0) ** (-np.arange(0, 16, 2, dtype=np.float32) / np.float32(16))).astype(np.float32)
    in_maps = []
    for c in range(NCORES):
        own0 = 16 + 1024 * c
        pos = np.concatenate([np.arange(16), np.arange(own0 - 160, own0 + 1024)])
        valid = pos >= 0
        xe = np.zeros((T, D), np.float32)
        xe[valid] = h0[pos[valid]]
        cfc = cfp.copy()
        a, b = CF['padmask']
        cfc[:, a:b] = valid[16:176].astype(np.float32)[None, :]
        cbc = np.zeros((128, NCB), np.float32)
        cbc[:, 0:128] = 1.0
        cbc[:, 128:256] = np.eye(128, dtype=np.float32)
        cbc[:, 256:384] = m_own
        cbc[:, 384:512] = m_prev
        cbc[:, 512:640] = m_prev if c > 0 else 0.0
        ang = np.clip(pos, 0, None).astype(np.float32)[:, None] * inv_freq[None, :]
        cosv = np.cos(ang).astype(np.float32)
        sinv = np.sin(ang).astype(np.float32)
        cst = np.zeros((128, 2, T), np.float32)
        cst[:, 0, :] = 1.0
        for p in range(128):
            d = p % 64
            if d < 16:
                cst[p, 0, :] = cosv[:, d % 8]
                cst[p, 1, :] = -sinv[:, d % 8] if d < 8 else sinv[:, d % 8]
        m = dict(shared)
        m['xe'] = xe
        m['cf'] = cfc
        m['cb'] = cbc
        m['cs'] = cst.reshape(128, 2 * T)
        in_maps.append(m)
    return in_maps


_CACHE = {}


def kernel(**inputs):
    in_maps = prepare(inputs)
    if 'nc' not in _CACHE:
        _CACHE['nc'] = build()[0]
    nc = _CACHE['nc']
    res = run_bass_kernel_spmd(nc, in_maps, core_ids=list(range(NCORES)))
    out = np.concatenate([np.asarray(r["out"], np.float32) for r in res.results], 0)
    return out.reshape(1, NCORES * TO, D)
```

```python
import numpy as np
from contextlib import ExitStack
import concourse.bass as bass
import concourse.mybir as mybir
from concourse.bass_utils import run_bass_kernel_spmd

F32 = mybir.dt.float32
BF16 = mybir.dt.bfloat16
AF = mybir.ActivationFunctionType
ALU = mybir.AluOpType
AX = mybir.AxisListType

NCORES = 8
D = 2048
NJ = 16
T = 1200
TE = 176
TO = 1024
ALPHA = float((2.0 * 2) ** 0.25)
EPS = 1e-5
G0, G1, G2 = (0, 176), (176, 688), (688, 1200)
TT_E = [(0, 128), (128, 176)]
TT_O = [(176 + 128 * k, 176 + 128 * (k + 1)) for k in range(8)]
BIG = 1.0e9


class Sem:
    _n = 0

    def __init__(self, h):
        self.h = h
        self.id = Sem._n
        Sem._n += 1


class Buf:
    def __init__(self, name):
        self.name = name
        self.w = {}
        self.wf = {}
        self.rd = {}
        self.dsem = None
        self.dcnt = 0
        self.psum = name.startswith('ps')


class Eng:
    def __init__(self, name, h, sem):
        self.name = name
        self.h = h
        self.sem = sem
        self.cnt = 0
        self.seen = {}


class Sched:
    def __init__(self, nc, es):
        self.nc = nc
        self.es = es
        mk = lambda n: Sem(es.enter_context(nc.semaphore(n)))
        self.pe = Eng('pe', nc.tensor, mk('s_pe'))
        self.act = Eng('act', nc.scalar, mk('s_act'))
        self.dve = Eng('dve', nc.vector, mk('s_dve'))
        self.pool = Eng('pool', nc.gpsimd, mk('s_pool'))
        self.sp = Eng('sp', nc.sync, mk('s_sp'))
        self.engs = [self.pe, self.act, self.dve, self.pool, self.sp]
        self.dsems = []

    def _deps(self, reads, writes, accw):
        deps = []
        for b in reads:
            deps.extend(b.w.values())
            if b.psum:
                deps.extend(b.rd.values())
        for b in writes:
            deps.extend(b.w.values())
            deps.extend(b.rd.values())
        for b in accw:
            deps.extend(b.rd.values())
            deps.extend(b.wf.values())
        return deps

    def _wait(self, eng, deps):
        best = {}
        for (s, v) in deps:
            if best.get(s.id, (None, 0))[1] < v:
                best[s.id] = (s, v)
        for sid, (s, v) in best.items():
            if eng is self.pe and s is self.pe.sem:
                continue
            if eng.seen.get(sid, 0) < v:
                eng.h.wait_ge(s.h, v)
                eng.seen[sid] = v

    def _mark(self, tag, reads, writes, accw):
        for b in reads:
            b.rd[tag[0].id] = tag
        for b in writes:
            b.w = {tag[0].id: tag}
            b.wf = {tag[0].id: tag}
        for b in accw:
            b.w[tag[0].id] = tag

    def op(self, eng, fn, reads=(), writes=(), accw=()):
        self._wait(eng, self._deps(reads, writes, accw))
        ins = fn(eng.h)
        ins.then_inc(eng.sem.h, 1)
        eng.cnt += 1
        self._mark((eng.sem, eng.cnt), reads, writes, accw)

    def group(self, eng, fns, reads=(), writes=(), accw=()):
        self._wait(eng, self._deps(reads, writes, accw))
        ins = None
        for fn in fns:
            ins = fn(eng.h)
        ins.then_inc(eng.sem.h, 1)
        eng.cnt += 1
        self._mark((eng.sem, eng.cnt), reads, writes, accw)

    def dma(self, eng, out, in_, owner, reads=(), writes=(), accw=()):
        if owner.dsem is None:
            owner.dsem = Sem(self.es.enter_context(self.nc.semaphore('d_' + owner.name)))
            self.dsems.append(owner)
        self._wait(eng, self._deps(reads, writes, accw))
        eng.h.dma_start(out=out, in_=in_).then_inc(owner.dsem.h, 16)
        owner.dcnt += 16
        self._mark((owner.dsem, owner.dcnt), reads, writes, accw)

    def barrier(self):
        for e in self.engs:
            for e2 in self.engs:
                if e2 is e or e2.cnt == 0:
                    continue
                if e.seen.get(e2.sem.id, 0) < e2.cnt:
                    e.h.wait_ge(e2.sem.h, e2.cnt)
                    e.seen[e2.sem.id] = e2.cnt
            for o in self.dsems:
                if o.dcnt and e.seen.get(o.dsem.id, 0) < o.dcnt:
                    e.h.wait_ge(o.dsem.h, o.dcnt)
                    e.seen[o.dsem.id] = o.dcnt

    def wait_all(self, eng, bufs):
        deps = []
        for b in bufs:
            deps.extend(b.w.values())
            deps.extend(b.rd.values())
        self._wait(eng, deps)


class Rot:
    def __init__(self, items):
        self.items = items
        self.i = 0

    def get(self):
        it = self.items[self.i % len(self.items)]
        self.i += 1
        return it


VEC_NAMES = ['b1', 'b2', 'bdw', 'clng', 'clnb', 'bout', 'mixg0', 'mixb0', 'ffng0', 'ffnb0',
             'mixg1', 'mixb1', 'ffng1', 'ffnb1', 'bq', 'bo', 'snk']
CF = {}
_o = 0
for _n, _w in [('ident', 128), ('pswap', 128), ('onesf', 128)] + [(v, 16) for v in VEC_NAMES] + \
        [('bk', 2), ('wdw', 16 * 31), ('padmask', 160), ('brb0', 36), ('brb1', 36), ('bvb', 256)]:
    CF[_n] = (_o, _o + _w)
    _o += _w
NCF = _o
CB = {'ones': (0, 128), 'identb': (128, 256), 'm_own': (256, 384), 'm_prev': (384, 512),
      'm_prev0': (512, 640)}
NCB = 640


class _Stop(Exception):
    pass


def build(dbg=()):
    dbg = set(dbg)
    try:
        return _build(dbg)
    except _Stop as st:
        return st.args


def _build(dbg):
    _CACHE_OUT_DONE = []
    nc = bass.Bass("TRN2", target_bir_lowering=False)

    def din(name, shape):
        return nc.dram_tensor(name, list(shape), F32, kind="ExternalInput").ap()

    xe_d = din("xe", [T, D])
    cf_d = din("cf", [128, NCF])
    cb_d = din("cb", [128, NCB])
    cs_d = din("cs", [128, 2 * T])
    sel_d = din("sel", [64, 32 * 128])
    win_d = din("win", [NJ, 128, 4096])
    wout_d = din("wout", [NJ, 128, 2048])
    w13_d = din("w13", [2, 32, 2, 128, 4096])
    w2_d = din("w2", [2, 32, 128, 4096])
    wr_d = din("wr", [2, 128, 16 * 36])
    wk_d = din("wk", [2, 128, 2048])
    wv_d = din("wv", [128, 4096])
    wq_d = din("wq", [NJ, 128, 2048])
    wo_d = din("wo", [NJ, 128, 2048])
    out_d = nc.dram_tensor("out", [TO, D], F32, kind="ExternalOutput").ap()
    dbg_d = {}

    with ExitStack() as es:
        S = Sched(nc, es)
        PE, ACT, DVE, POOL, SP = S.pe, S.act, S.dve, S.pool, S.sp

        def sb(scope, name, shape, dt=F32):
            return scope.enter_context(nc.sbuf_tensor(name, list(shape), dt))

        hO = sb(es, "hO", [128, NJ, TO])
        hbO = sb(es, "hbO", [128, NJ, TO], BF16)
        hbE = sb(es, "hbE", [128, NJ, TE], BF16)
        wsl = [sb(es, f"wsl{i}", [128, 4096], BF16) for i in range(2)]
        wslB = [Buf(f"wsl{i}") for i in range(2)]
        cf = sb(es, "cf_sb", [128, NCF])
        cb = sb(es, "cb_sb", [128, NCB], BF16)
        st_mean = sb(es, "st_mean", [128, 512])
        st_rstd = sb(es, "st_rstd", [128, 512])
        B_mean, B_rstd = Buf("st_mean"), Buf("st_rstd")
        _st0 = (st_mean, B_mean, st_rstd, B_rstd)
        fscr = Rot([(sb(es, f"fs{i}", [128, 512]), Buf(f"fs{i}")) for i in range(5)])
        bscr = Rot([(sb(es, f"bs{i}", [128, 512], BF16), Buf(f"bs{i}")) for i in range(3)])
        esnk = sb(es, "esnk", [128, 16])
        B_esnk = Buf("esnk")
        B_cf, B_cb = Buf("cf"), Buf("cb")

        PS = [es.enter_context(nc.psum_tensor(f"ps{i}", [128, 512], F32)) for i in range(8)]
        PB = [Buf(f"ps{i}") for i in range(8)]

        def prot(idx):
            return Rot([(PS[i], PB[i]) for i in idx])

        HB = {G0: Buf("hb0"), G1: Buf("hb1"), G2: Buf("hb2")}
        H = {(j, g): Buf(f"h{j}_{g[0]}") for j in range(NJ) for g in (G0, G1, G2)}

        def tg_of(c0):
            return G0 if c0 < 176 else (G1 if c0 < 688 else G2)

        def cfv(name, j=None):
            a, b = CF[name]
            if j is None:
                return cf[:, a:b]
            return cf[:, a + j:a + j + 1]

        def cbv(name):
            a, b = CB[name]
            return cb[:, a:b]

        S.dma(SP, cf[:], cf_d[:, :], B_cf, writes=[B_cf])
        S.dma(POOL, cb[:], cb_d[:, :], B_cb, writes=[B_cb])
        S.op(ACT, lambda e: e.activation(out=esnk[:], in_=cfv('snk'), func=AF.Exp),
             reads=[B_cf], writes=[B_esnk])
        ident = cfv('ident')
        pswap = cfv('pswap')
        ones = cbv('ones')

        if 'stop0' in dbg:
            dump_esnk = True
        def dump(name, ap_sb, bufs, shape, dt=F32):
            d = nc.dram_tensor("o_" + name, list(shape), dt, kind="ExternalOutput").ap()
            dbg_d[name] = d
            ob = Buf("dbg_" + name)
            S.dma(SP, d, ap_sb, ob, reads=bufs)
            S.wait_all(SP, [ob])
            S._wait(SP, [(ob.dsem, ob.dcnt)])
            S.barrier()

        if 'stop0' in dbg:
            dump('esnk', esnk[:], [B_esnk], [128, 16])
            dump('cbd', cb[:], [B_cb], [128, NCB], BF16)
            S.barrier()
            raise _Stop(nc, dbg_d)
        def stream_weights(units, src_fn, width):
            issued = set()

            def load(i):
                if i in issued or i >= len(units):
                    return
                issued.add(i)
                k = i % 2
                S.dma(POOL, wsl[k][:, 0:width], src_fn(units[i]), wslB[k], writes=[wslB[k]])
            return load

        def layernorm(tgs, hcol, hbcol, gname, bname, write_hb=True, after=None):
            with ExitStack() as sc:
                uid = len(S.dsems) * 1000 + S.act.cnt
                sts = [(st_mean, B_mean, st_rstd, B_rstd),
                       (sb(sc, f"lnm{uid}", [128, 512]), Buf("lnm"), sb(sc, f"lnr{uid}", [128, 512]), Buf("lnr"))]
                vsc = Rot([(sb(sc, f"lnv{uid}_{i}", [128, 512], BF16), Buf(f"lnv{i}")) for i in range(4)])
                p12 = prot([6, 7, 4, 5])

                def stats(ti):
                    tg = tgs[ti]
                    c0, c1 = tg
                    n = c1 - c0
                    P1, B1 = p12.get()
                    P2, B2 = p12.get()
                    for j in range(NJ):
                        if True:
                            vb, Bvb = vsc.get()
                            S.op(ACT, lambda e: e.activation(out=vb[:, 0:n], in_=hcol(j, c0, c1), func=AF.Copy),
                                 reads=[H[(j, tg)]], writes=[Bvb])
                            S.op(PE, lambda e: e.matmul(P1[:, 0:n], lhsT=ones, rhs=vb[:, 0:n], start=(j == 0), stop=(j == NJ - 1)),
                                 reads=[Bvb, B_cb], writes=[B1] if j == 0 else [], accw=[] if j == 0 else [B1])
                        else:
                            S.op(PE, lambda e: e.matmul(P1[:, 0:n], lhsT=cfv('onesf'), rhs=hcol(j, c0, c1), start=(j == 0), stop=(j == NJ - 1)),
                                 reads=[H[(j, tg)], B_cf], writes=[B1] if j == 0 else [], accw=[] if j == 0 else [B1])
                        vq, Bvq = vsc.get()
                        S.op(ACT, lambda e: e.activation(out=vq[:, 0:n], in_=hcol(j, c0, c1), func=AF.Square),
                             reads=[H[(j, tg)]], writes=[Bvq])
                        S.op(PE, lambda e: e.matmul(P2[:, 0:n], lhsT=ones, rhs=vq[:, 0:n], start=(j == 0), stop=(j == NJ - 1)),
                             reads=[Bvq, B_cb], writes=[B2] if j == 0 else [], accw=[] if j == 0 else [B2])
                    ln_stats(P1, B1, P2, B2, n, sts[ti % 2])

                nsc = Rot([(sb(sc, f"lns{uid}_{i}", [128, 512]), Buf(f"lns{i}")) for i in range(8)])

                def norm(ti):
                    tg = tgs[ti]
                    c0, c1 = tg
                    n = c1 - c0
                    mean, Bm, rstd, Br = sts[ti % 2]
                    for jb in range(0, NJ, 4):
                        js = list(range(jb, jb + 4))
                        t1s = {}
                        t2s = {}
                        for j in js:
                            t1s[j] = nsc.get()
                            t1, Bt1 = t1s[j]
                            S.op(DVE, lambda e: e.tensor_tensor(out=t1[:, 0:n], in0=hcol(j, c0, c1), in1=mean[:, 0:n], op=ALU.subtract),
                                 reads=[H[(j, tg)], Bm], writes=[Bt1])
                        for j in js:
                            t1, Bt1 = t1s[j]
                            t2s[j] = nsc.get()
                            t2, Bt2 = t2s[j]
                            S.op(DVE, lambda e: e.scalar_tensor_tensor(out=t2[:, 0:n], in0=t1[:, 0:n], scalar=cfv(gname, j), in1=rstd[:, 0:n], op0=ALU.mult, op1=ALU.mult),
                                 reads=[Bt1, Br, B_cf], writes=[Bt2])
                        for j in js:
                            t2, Bt2 = t2s[j]
                            S.op(ACT, lambda e: e.activation(out=hcol(j, c0, c1), in_=t2[:, 0:n], func=AF.Identity, bias=cfv(bname, j)),
                                 reads=[Bt2, B_cf], writes=[H[(j, tg)]])
                        if write_hb:
                            for j in js:
                                t2, Bt2 = t2s[j]
                                if j % 8 < 5:
                                    S.op(ACT, lambda e: e.activation(out=hbcol(j, c0, c1), in_=t2[:, 0:n], func=AF.Identity, bias=cfv(bname, j)),
                                         reads=[Bt2, B_cf], accw=[HB[tg]])
                                else:
                                    S.op(DVE, lambda e: e.tensor_scalar(out=hbcol(j, c0, c1), in0=t2[:, 0:n], scalar1=cfv(bname, j), scalar2=None, op0=ALU.add),
                                         reads=[Bt2, B_cf], accw=[HB[tg]])
                    if after is not None:
                        after(ti)

                stats(0)
                for ti in range(len(tgs)):
                    if ti + 1 < len(tgs):
                        stats(ti + 1)
                    norm(ti)
                S.barrier()

        def ln_stats(P1, B1, P2, B2, n, st=None):
            st_mean, B_mean, st_rstd, B_rstd = st if st is not None else _st0
            S.op(DVE, lambda e: e.tensor_scalar(out=st_mean[:, 0:n], in0=P1[:, 0:n], scalar1=1.0 / D, scalar2=None, op0=ALU.mult),
                 reads=[B1], writes=[B_mean])
            t1, Bt1 = fscr.get()
            S.op(DVE, lambda e: e.tensor_tensor(out=t1[:, 0:n], in0=st_mean[:, 0:n], in1=st_mean[:, 0:n], op=ALU.mult),
                 reads=[B_mean], writes=[Bt1])
            t2, Bt2 = fscr.get()
            S.op(DVE, lambda e: e.scalar_tensor_tensor(out=t2[:, 0:n], in0=P2[:, 0:n], scalar=1.0 / D, in1=t1[:, 0:n], op0=ALU.mult, op1=ALU.subtract),
                 reads=[B2, Bt1], writes=[Bt2])
            t3, Bt3 = fscr.get()
            S.op(ACT, lambda e: e.activation(out=t3[:, 0:n], in_=t2[:, 0:n], func=AF.Sqrt, bias=EPS, scale=1.0),
                 reads=[Bt2], writes=[Bt3])
            S.op(DVE, lambda e: e.reciprocal(out=st_rstd[:, 0:n], in_=t3[:, 0:n]),
                 reads=[Bt3], writes=[B_rstd])

        def proj_residual(tgs, wsrc, bias_name, hcol, hbcol):
            units = list(range(NJ))
            load = stream_weights(units, lambda j: wsrc[j], 2048)
            pr = prot([0, 1, 2, 3])
            load(0)
            for j in units:
                load(j + 1)
                w = wsl[j % 2][:, 0:2048].rearrange("p (k m) -> p k m", k=NJ)
                for tg in tgs:
                    c0, c1 = tg
                    n = c1 - c0
                    P, Bp = pr.get()
                    S.group(PE, [lambda e, kc=kc: e.matmul(P[:, 0:n], lhsT=w[:, kc, :], rhs=hbcol(kc, c0, c1), start=(kc == 0), stop=(kc == NJ - 1)) for kc in range(NJ)],
                            reads=[wslB[j % 2], HB[tg]], writes=[Bp])
                    t, Bt = fscr.get()
                    S.op(ACT, lambda e, t=t, P=P: e.activation(out=t[:, 0:n], in_=P[:, 0:n], func=AF.Identity, bias=cfv(bias_name, j)),
                         reads=[Bp, B_cf], writes=[Bt])
                    S.op(DVE, lambda e, t=t: e.scalar_tensor_tensor(out=hcol(j, c0, c1), in0=hcol(j, c0, c1), scalar=ALPHA, in1=t[:, 0:n], op0=ALU.mult, op1=ALU.add),
                         reads=[Bt], writes=[H[(j, tg)]])

        def moe(layer, tgs, tts, hcol, hbcol, scope):
            wrt = sb(scope, f"wrt{layer}", [128, NJ, 36])
            B_wrt = Buf(f"wrt{layer}")
            sel = sb(scope, f"sel_sb{layer}", [64, 32, 128], BF16)
            B_sel = Buf(f"sel{layer}")
            gT = sb(scope, f"gT{layer}", [64, T], BF16)
            B_gT = {tg: Buf(f"gT{layer}_{tg[0]}") for tg in tgs}
            gball = sb(scope, f"gball{layer}", [128, T])
            B_gb = {tg: Buf(f"gb{layer}_{tg[0]}") for tg in tgs}
            hid = sb(scope, f"hid{layer}", [128, 2, 2, T], BF16)
            B_hid = [Buf(f"hid{layer}_0"), Buf(f"hid{layer}_1")]
            w2s = [sb(scope, f"w2s{layer}_{i}", [128, 2, D], BF16) for i in range(2)]
            B_w2 = [Buf(f"w2s{layer}_0"), Buf(f"w2s{layer}_1")]
            lg = sb(scope, f"lg{layer}", [128, 36])
            B_lg = Buf(f"lg{layer}")
            S.dma(SP, wrt[:].rearrange("p k m -> p (k m)"), wr_d[layer], B_wrt, writes=[B_wrt])
            S.dma(POOL, sel[:].rearrange("p e m -> p (e m)"), sel_d[:, :], B_sel, writes=[B_sel])
            brb = cfv('brb%d' % layer)
            units = [(e, fc) for e in range(32) for fc in range(2)]
            load = stream_weights(units, lambda u: w13_d[layer, u[0], u[1]], 4096)
            load(0)
            NT = len(tts)
            pr = prot([6, 7])

            def wt(name, shape, dt=F32):
                return sb(scope, f"g{layer}_{name}", shape, dt), Buf(f"g{layer}_{name}")
            lgall, B_lga = wt("lgall", [128, NT, 36])
            S.op(POOL, lambda e: e.memset(lgall[:].rearrange("p t m -> p (t m)"), 0.0), writes=[B_lga])
            for ti, (c0, c1) in enumerate(tts):
                n = c1 - c0
                tg = tg_of(c0)
                P, Bp = pr.get()
                S.group(PE, [lambda e, kc=kc: e.matmul(P[0:n, 0:36], lhsT=hcol(kc, c0, c1), rhs=wrt[:, kc, :], start=(kc == 0), stop=(kc == NJ - 1)) for kc in range(NJ)],
                        reads=[B_wrt] + [H[(kc, tg)] for kc in range(NJ)], writes=[Bp])
                S.op(DVE, lambda e: e.tensor_tensor(out=lgall[0:n, ti, :], in0=P[0:n, 0:36], in1=brb[0:n, :], op=ALU.add),
                     reads=[Bp, B_cf], accw=[B_lga])
            lg4 = lgall[:, :, 0:4]
            le = lgall[:, :, 4:36]
            gmax, Bgmax = wt("gmax", [128, NT])
            S.op(DVE, lambda e: e.tensor_reduce(out=gmax[:], in_=lg4, axis=AX.X, op=ALU.max), reads=[B_lga], writes=[Bgmax])
            gd, Bgd = wt("gd", [128, NT, 4])
            S.op(DVE, lambda e: e.tensor_tensor(out=gd[:], in0=lg4, in1=gmax[:].unsqueeze(2).broadcast_to([128, NT, 4]), op=ALU.subtract),
                 reads=[B_lga, Bgmax], writes=[Bgd])
            gexp, Bgexp = wt("gexp", [128, NT, 4])
            S.op(ACT, lambda e: e.activation(out=gexp[:], in_=gd[:], func=AF.Exp), reads=[Bgd], writes=[Bgexp])
            gsum, Bgsum = wt("gsum", [128, NT])
            S.op(DVE, lambda e: e.tensor_reduce(out=gsum[:], in_=gexp[:], axis=AX.X, op=ALU.add), reads=[Bgexp], writes=[Bgsum])
            gw, Bgw = wt("gw", [128, NT])
            S.op(DVE, lambda e: e.reciprocal(out=gw[:], in_=gsum[:]), reads=[Bgsum], writes=[Bgw])
            pen, Bpen = wt("pen", [128, NT, 4])
            S.op(DVE, lambda e: e.tensor_scalar(out=pen[:], in0=gd[:], scalar1=0.0, scalar2=None, op0=ALU.is_equal), reads=[Bgd], writes=[Bpen])
            S.op(DVE, lambda e: e.tensor_scalar(out=pen[:], in0=pen[:], scalar1=BIG, scalar2=-BIG, op0=ALU.mult, op1=ALU.add), reads=[Bpen], writes=[Bpen])
            lem, Blem = wt("lem", [128, NT, 32])
            S.op(DVE, lambda e: e.tensor_tensor(out=lem[:].rearrange("p t (g k) -> p t g k", g=4), in0=le.rearrange("p t (g k) -> p t g k", g=4),
                                                in1=pen[:].unsqueeze(3).broadcast_to([128, NT, 4, 8]), op=ALU.add),
                 reads=[B_lga, Bpen], writes=[Blem])
            m1, Bm1 = wt("m1", [128, NT])
            S.op(DVE, lambda e: e.tensor_reduce(out=m1[:], in_=lem[:], axis=AX.X, op=ALU.max), reads=[Blem], writes=[Bm1])
            eq1, Beq1 = wt("eq1", [128, NT, 32])
            S.op(DVE, lambda e: e.tensor_tensor(out=eq1[:], in0=lem[:], in1=m1[:].unsqueeze(2).broadcast_to([128, NT, 32]), op=ALU.is_equal),
                 reads=[Blem, Bm1], writes=[Beq1])
            lem2, Blem2 = wt("lem2", [128, NT, 32])
            S.op(DVE, lambda e: e.scalar_tensor_tensor(out=lem2[:], in0=eq1[:], scalar=-BIG, in1=lem[:], op0=ALU.mult, op1=ALU.add),
                 reads=[Beq1, Blem], writes=[Blem2])
            m2, Bm2 = wt("m2", [128, NT])
            S.op(DVE, lambda e: e.tensor_reduce(out=m2[:], in_=lem2[:], axis=AX.X, op=ALU.max), reads=[Blem2], writes=[Bm2])
            eq2, Beq2 = wt("eq2", [128, NT, 32])
            S.op(DVE, lambda e: e.tensor_tensor(out=eq2[:], in0=lem2[:], in1=m2[:].unsqueeze(2).broadcast_to([128, NT, 32]), op=ALU.is_equal),
                 reads=[Blem2, Bm2], writes=[Beq2])
            dm, Bdm = wt("dm", [128, NT])
            S.op(DVE, lambda e: e.tensor_tensor(out=dm[:], in0=m1[:], in1=m2[:], op=ALU.subtract), reads=[Bm1, Bm2], writes=[Bdm])
            sg, Bsg = wt("sg", [128, NT])
            S.op(ACT, lambda e: e.activation(out=sg[:], in_=dm[:], func=AF.Sigmoid), reads=[Bdm], writes=[Bsg])
            wa, Bwa = wt("wa", [128, NT])
            S.op(DVE, lambda e: e.tensor_tensor(out=wa[:], in0=gw[:], in1=sg[:], op=ALU.mult), reads=[Bgw, Bsg], writes=[Bwa])
            wb, Bwb = wt("wb", [128, NT])
            S.op(DVE, lambda e: e.tensor_tensor(out=wb[:], in0=gw[:], in1=wa[:], op=ALU.subtract), reads=[Bgw, Bwa], writes=[Bwb])
            S.op(DVE, lambda e: e.tensor_tensor(out=eq1[:], in0=eq1[:], in1=wa[:].unsqueeze(2).broadcast_to([128, NT, 32]), op=ALU.mult),
                 reads=[Bwa], writes=[Beq1])
            S.op(DVE, lambda e: e.tensor_tensor(out=eq2[:], in0=eq2[:], in1=wb[:].unsqueeze(2).broadcast_to([128, NT, 32]), op=ALU.mult),
                 reads=[Bwb], writes=[Beq2])
            gate, Bgate = lem, Blem
            S.op(DVE, lambda e: e.tensor_tensor(out=gate[:], in0=eq1[:], in1=eq2[:], op=ALU.add), reads=[Beq1, Beq2], writes=[Bgate])
            if ('gate%d' % layer) in dbg:
                dump(f"gate{layer}", gate[:].rearrange("p t m -> p (t m)"), [Bgate], [128, NT * 32])
            hib, Bhib = wt("hib", [128, NT, 32], BF16)
            S.op(DVE, lambda e: e.tensor_copy(out=hib[:], in_=gate[:]), reads=[Bgate], writes=[Bhib])
            ghl, Bghl = wt("ghl", [128, NT, 64])
            S.op(DVE, lambda e: e.tensor_copy(out=ghl[:, :, 0:32], in_=hib[:]), reads=[Bhib], writes=[Bghl])
            S.op(DVE, lambda e: e.tensor_tensor(out=ghl[:, :, 32:64], in0=gate[:], in1=hib[:], op=ALU.subtract),
                 reads=[Bgate, Bhib], accw=[Bghl])
            for ti, (c0, c1) in enumerate(tts):
                n = c1 - c0
                tg = tg_of(c0)
                PT, Bpt = pr.get()
                S.op(PE, lambda e: e.transpose(PT[0:64, 0:n], ghl[0:n, ti, :], ident[0:n, 0:n]),
                     reads=[Bghl, B_cf], writes=[Bpt])
                S.op(ACT, lambda e: e.copy(gT[:, c0:c1], PT[0:64, 0:n]), reads=[Bpt], accw=[B_gT[tg]])

            pA = prot([0, 1])
            pB = prot([2, 3])
            pG = prot([4])
            pD = prot([5, 6, 7])
            for ui, (e_, fc) in enumerate(units):
                load(ui + 1)
                es_ = e_ % 2
                w13 = wsl[ui % 2][:, 0:4096].rearrange("p (k h m) -> p k h m", k=NJ, h=2)
                if fc == 0:
                    S.dma(POOL, w2s[es_][:].rearrange("p f d -> p (f d)"), w2_d[layer, e_], B_w2[es_], writes=[B_w2[es_]])
                for tg in tgs:
                    c0, c1 = tg
                    n = c1 - c0
                    if fc == 0:
                        PG, Bpg = pG.get()
                        S.op(PE, lambda e: e.matmul(PG[:, 0:n], lhsT=sel[:, e_, :], rhs=gT[:, c0:c1], start=True, stop=True),
                             reads=[B_sel, B_gT[tg]], writes=[Bpg])
                        S.op(ACT, lambda e: e.copy(gball[:, c0:c1], PG[:, 0:n]), reads=[Bpg], writes=[B_gb[tg]])
                    PA, Bpa = pA.get()
                    S.group(PE, [lambda e, kc=kc: e.matmul(PA[:, 0:n], lhsT=w13[:, kc, 0, :], rhs=hbcol(kc, c0, c1), start=(kc == 0), stop=(kc == NJ - 1)) for kc in range(NJ)],
                            reads=[wslB[ui % 2], HB[tg]], writes=[Bpa])
                    PBk, Bpb = pB.get()
                    S.group(PE, [lambda e, kc=kc: e.matmul(PBk[:, 0:n], lhsT=w13[:, kc, 1, :], rhs=hbcol(kc, c0, c1), start=(kc == 0), stop=(kc == NJ - 1)) for kc in range(NJ)],
                            reads=[wslB[ui % 2], HB[tg]], writes=[Bpb])
                    sa, Bsa = fscr.get()
                    S.op(ACT, lambda e: e.activation(out=sa[:, 0:n], in_=PA[:, 0:n], func=AF.Silu), reads=[Bpa], writes=[Bsa])
                    t, Bt = fscr.get()
                    S.op(DVE, lambda e: e.tensor_tensor(out=t[:, 0:n], in0=sa[:, 0:n], in1=gball[:, c0:c1], op=ALU.mult),
                         reads=[Bsa, B_gb[tg]], writes=[Bt])
                    S.op(DVE, lambda e: e.tensor_tensor(out=hid[:, es_, fc, c0:c1], in0=PBk[:, 0:n], in1=t[:, 0:n], op=ALU.mult),
                         reads=[Bpb, Bt], accw=[B_hid[es_]])
                if fc == 1 and es_ == 1:
                    first = (e_ == 1)
                    for j in range(NJ):
                        for tg in tgs:
                            c0, c1 = tg
                            n = c1 - c0
                            PD, Bpd = pD.get()
                            fns = []
                            for k, (ee, ff) in enumerate([(0, 0), (0, 1), (1, 0), (1, 1)]):
                                fns.append(lambda e, ee=ee, ff=ff, k=k: e.matmul(PD[:, 0:n], lhsT=w2s[ee][:, ff, j * 128:(j + 1) * 128], rhs=hid[:, ee, ff, c0:c1], start=(k == 0), stop=(k == 3)))
                            S.group(PE, fns, reads=[B_w2[0], B_w2[1], B_hid[0], B_hid[1]], writes=[Bpd])
                            if first:
                                S.op(DVE, lambda e: e.scalar_tensor_tensor(out=hcol(j, c0, c1), in0=hcol(j, c0, c1), scalar=ALPHA, in1=PD[:, 0:n], op0=ALU.mult, op1=ALU.add),
                                     reads=[Bpd], writes=[H[(j, tg)]])
                            else:
                                S.op(DVE, lambda e: e.tensor_tensor(out=hcol(j, c0, c1), in0=hcol(j, c0, c1), in1=PD[:, 0:n], op=ALU.add),
                                     reads=[Bpd], writes=[H[(j, tg)]])

        with ExitStack() as L0:
            hE = sb(L0, "hE", [128, NJ, TE])

            def hcol(j, c0, c1):
                return hE[:, j, c0:c1] if c1 <= TE else hO[:, j, c0 - TE:c1 - TE]

            def hbcol(j, c0, c1):
                return hbE[:, j, c0:c1] if c1 <= TE else hbO[:, j, c0 - TE:c1 - TE]

            def hcol4(j0, c0, c1):
                return hE[:, j0:j0 + 4, c0:c1] if c1 <= TE else hO[:, j0:j0 + 4, c0 - TE:c1 - TE]

            def hbcol4(j0, c0, c1):
                return hbE[:, j0:j0 + 4, c0:c1] if c1 <= TE else hbO[:, j0:j0 + 4, c0 - TE:c1 - TE]

            with ExitStack() as PA_:
                xs = [sb(PA_, f"xs{i}", [128, D]) for i in range(2)]
                xsB = [Buf(f"xs{i}") for i in range(2)]
                pr = prot([0, 1, 2, 3])
                tts = TT_E + TT_O
                S.dma(SP, xs[0][0:128, :], xe_d[0:128, :], xsB[0], writes=[xsB[0]])
                for ti, (c0, c1) in enumerate(tts):
                    n = c1 - c0
                    if ti + 1 < len(tts):
                        a0, a1 = tts[ti + 1]
                        S.dma(SP, xs[(ti + 1) % 2][0:a1 - a0, :], xe_d[a0:a1, :], xsB[(ti + 1) % 2], writes=[xsB[(ti + 1) % 2]])
                    x_t = xs[ti % 2]
                    tg = tg_of(c0)
                    for jq in range(4):
                        P, Bp = pr.get()
                        Pv = P[:, :].rearrange("p (a b) -> p a b", a=4)
                        S.group(PE, [lambda e, i=i: e.transpose(Pv[:, i, 0:n], x_t[0:n, (jq * 4 + i) * 128:(jq * 4 + i + 1) * 128], ident[0:n, 0:n]) for i in range(4)],
                                reads=[xsB[ti % 2], B_cf], writes=[Bp])
                        S.op(ACT, lambda e: e.copy(hcol4(jq * 4, c0, c1), Pv[:, :, 0:n]),
                             reads=[Bp], writes=[H[(jq * 4 + i, tg)] for i in range(4)] if False else [], accw=[H[(jq * 4 + i, tg)] for i in range(4)])
                        S.op(DVE, lambda e: e.tensor_copy(out=hbcol4(jq * 4, c0, c1), in_=hcol4(jq * 4, c0, c1)),
                             reads=[H[(jq * 4 + i, tg)] for i in range(4)], accw=[HB[tg]])
                S.barrier()
            def stop(flag):
                if flag in dbg:
                    S.barrier()
                    raise _Stop(nc, dbg_d)
            if 'h0' in dbg:
                dump("h0E", hE[:].rearrange("p j t -> p (j t)"), [], [128, NJ * TE])
                dump("h0O", hO[:].rearrange("p j t -> p (j t)"), [], [128, NJ * TO])

            stop('stopA')
            with ExitStack() as PB_:
                z = sb(PB_, "z", [128, NJ, T], BF16)
                Bz = {(j, g): Buf(f"z{j}_{g[0]}") for j in range(NJ) for g in (G0, G1, G2)}
                yb = [sb(PB_, f"yb{i}", [128, T + 30], BF16) for i in range(2)]
                Byb = [Buf("yb0"), Buf("yb1")]
                diag = sb(PB_, "diag", [128, 31, 128], BF16)
                Bdiag = Buf("diag")
                for i in range(2):
                    S.op(DVE, lambda e, i=i: e.memset(yb[i][:, 0:30], 0.0), writes=[Byb[i]])
                units = list(range(NJ))
                load = stream_weights(units, lambda j: win_d[j], 4096)
                pA = prot([0, 1])
                pB = prot([2, 3])
                pC = prot([4, 5])
                load(0)
                pending = []

                def conv(j, tg):
                    c0, c1 = tg
                    n = c1 - c0
                    y = yb[j % 2]
                    PC, Bpc = pC.get()
                    S.group(PE, [lambda e, k=k: e.matmul(PC[:, 0:n], lhsT=diag[:, k, :], rhs=y[:, c0 + k:c0 + k + n], start=(k == 0), stop=(k == 30)) for k in range(31)],
                            reads=[Bdiag, Byb[j % 2]], writes=[Bpc])
                    S.op(ACT, lambda e: e.activation(out=z[:, j, c0:c1], in_=PC[:, 0:n], func=AF.Identity, bias=cfv('bdw', j)),
                         reads=[Bpc, B_cf], writes=[Bz[(j, tg)]])

                for j in units:
                    load(j + 1)
                    w = wsl[j % 2][:, 0:4096].rearrange("p (k h m) -> p k h m", k=NJ, h=2)
                    y = yb[j % 2]
                    for tg in (G0, G1, G2):
                        c0, c1 = tg
                        n = c1 - c0
                        PA, Bpa = pA.get()
                        S.group(PE, [lambda e, kc=kc: e.matmul(PA[:, 0:n], lhsT=w[:, kc, 0, :], rhs=hbcol(kc, c0, c1), start=(kc == 0), stop=(kc == NJ - 1)) for kc in range(NJ)],
                                reads=[wslB[j % 2], HB[tg]], writes=[Bpa])
                        PBk, Bpb = pB.get()
                        S.group(PE, [lambda e, kc=kc: e.matmul(PBk[:, 0:n], lhsT=w[:, kc, 1, :], rhs=hbcol(kc, c0, c1), start=(kc == 0), stop=(kc == NJ - 1)) for kc in range(NJ)],
                                reads=[wslB[j % 2], HB[tg]], writes=[Bpb])
                        if pending:
                            pj, ptg = pending.pop(0)
                            conv(pj, ptg)
                        if tg == G0:
                            a, b = CF['wdw']
                            wd = cf[:, a + j * 31:a + (j + 1) * 31]
                            S.op(DVE, lambda e: e.tensor_tensor(out=diag[:], in0=cbv('identb').unsqueeze(1).broadcast_to([128, 31, 128]),
                                                                in1=wd.unsqueeze(2).broadcast_to([128, 31, 128]), op=ALU.mult),
                                 reads=[B_cb, B_cf], writes=[Bdiag])
                        sg, Bsg = fscr.get()
                        S.op(ACT, lambda e: e.activation(out=sg[:, 0:n], in_=PBk[:, 0:n], func=AF.Sigmoid, bias=cfv('b2', j)),
                             reads=[Bpb, B_cf], writes=[Bsg])
                        if tg == G0:
                            S.op(DVE, lambda e: e.scalar_tensor_tensor(out=y[:, 30 + c0:30 + c1], in0=PA[:, 0:n], scalar=cfv('b1', j), in1=sg[:, 0:n], op0=ALU.add, op1=ALU.mult),
                                 reads=[Bpa, Bsg, B_cf], writes=[Byb[j % 2]])
                            S.op(DVE, lambda e: e.tensor_tensor(out=y[:, 30 + 16:30 + 176], in0=y[:, 30 + 16:30 + 176], in1=cfv('padmask'), op=ALU.mult),
                                 reads=[B_cf], writes=[Byb[j % 2]])
                        else:
                            S.op(DVE, lambda e: e.scalar_tensor_tensor(out=y[:, 30 + c0:30 + c1], in0=PA[:, 0:n], scalar=cfv('b1', j), in1=sg[:, 0:n], op0=ALU.add, op1=ALU.mult),
                                 reads=[Bpa, Bsg, B_cf], accw=[Byb[j % 2]])
                        pending.append((j, tg))
                while pending:
                    pj, ptg = pending.pop(0)
                    conv(pj, ptg)
                if 'z' in dbg:
                    dump("z", z[:].rearrange("p j t -> p (j t)"), list(Bz.values()), [128, NJ * T], BF16)

                p12 = prot([6, 7, 0, 1])
                for tg in (G0, G1, G2):
                    c0, c1 = tg
                    n = c1 - c0
                    P1, B1 = p12.get()
                    P2, B2 = p12.get()
                    S.group(PE, [lambda e, j=j: e.matmul(P1[:, 0:n], lhsT=ones, rhs=z[:, j, c0:c1], start=(j == 0), stop=(j == NJ - 1)) for j in range(NJ)],
                            reads=[B_cb] + [Bz[(j, tg)] for j in range(NJ)], writes=[B1])
                    for j in range(NJ):
                        vq, Bvq = bscr.get()
                        S.op(ACT, lambda e, vq=vq, j=j: e.activation(out=vq[:, 0:n], in_=z[:, j, c0:c1], func=AF.Square),
                             reads=[Bz[(j, tg)]], writes=[Bvq])
                        S.op(PE, lambda e, vq=vq, j=j: e.matmul(P2[:, 0:n], lhsT=ones, rhs=vq[:, 0:n], start=(j == 0), stop=(j == NJ - 1)),
                             reads=[Bvq, B_cb], writes=[B2] if j == 0 else [], accw=[] if j == 0 else [B2])
                    ln_stats(P1, B1, P2, B2, n)
                    for j in range(NJ):
                        t1, Bt1 = fscr.get()
                        S.op(DVE, lambda e, t1=t1, j=j: e.tensor_tensor(out=t1[:, 0:n], in0=z[:, j, c0:c1], in1=st_mean[:, 0:n], op=ALU.subtract),
                             reads=[Bz[(j, tg)], B_mean], writes=[Bt1])
                        t2, Bt2 = fscr.get()
                        S.op(DVE, lambda e, t1=t1, t2=t2, j=j: e.scalar_tensor_tensor(out=t2[:, 0:n], in0=t1[:, 0:n], scalar=cfv('clng', j), in1=st_rstd[:, 0:n], op0=ALU.mult, op1=ALU.mult),
                             reads=[Bt1, B_rstd, B_cf], writes=[Bt2])
                        S.op(ACT, lambda e, t2=t2, j=j: e.activation(out=hbcol(j, c0, c1), in_=t2[:, 0:n], func=AF.Silu, bias=cfv('clnb', j)),
                             reads=[Bt2, B_cf], writes=[HB[tg]] if j == 0 else [], accw=[] if j == 0 else [HB[tg]])
                S.barrier()
            if 'a' in dbg:
                dump("aE", hbE[:].rearrange("p j t -> p (j t)"), list(HB.values()), [128, NJ * TE], BF16)
                dump("aO", hbO[:].rearrange("p j t -> p (j t)"), list(HB.values()), [128, NJ * TO], BF16)

            stop('stopC')
            proj_residual((G0, G1, G2), wout_d, 'bout', hcol, hbcol)
            layernorm((G0, G1, G2), hcol, hbcol, 'mixg0', 'mixb0')
            if 'h1' in dbg:
                S.barrier()
                dump("h1E", hE[:].rearrange("p j t -> p (j t)"), [], [128, NJ * TE])
                dump("h1O", hO[:].rearrange("p j t -> p (j t)"), [], [128, NJ * TO])

            stop('stopD')
            if 'stop1' not in dbg:
                with ExitStack() as PE_:
                    moe(0, (G0, G1, G2), TT_E + TT_O, hcol, hbcol, PE_)
                    S.barrier()
                    if 'pre2' in dbg:
                        dump("pre2E", hE[:].rearrange("p j t -> p (j t)"), [], [128, NJ * TE])
                        dump("pre2O", hO[:].rearrange("p j t -> p (j t)"), [], [128, NJ * TO])
                layernorm((G0, G1, G2), hcol, hbcol, 'ffng0', 'ffnb0')
            S.barrier()
            if 'h2' in dbg:
                dump("h2E", hE[:].rearrange("p j t -> p (j t)"), [], [128, NJ * TE])
                dump("h2O", hO[:].rearrange("p j t -> p (j t)"), [], [128, NJ * TO])

        def hcol(j, c0, c1):
            return hO[:, j, c0 - TE:c1 - TE]

        def hbcol(j, c0, c1):
            return hbE[:, j, c0:c1] if c1 <= TE else hbO[:, j, c0 - TE:c1 - TE]

        if 'stop2' not in dbg:
            with ExitStack() as L1:
                KT = sb(L1, "KT", [128, 2, T], BF16)
                B_KT = Buf("KT")
                V = sb(L1, "V", [128, 10, 256], BF16)
                B_V = Buf("V")
                with ExitStack() as LQ:
                    QT = sb(LQ, "QT", [128, NJ, TO], BF16)
                    B_QT = {g: Buf(f"QT{g[0]}") for g in (G1, G2)}
                    with ExitStack() as LCS:
                        cs = sb(LCS, "cs_sb", [128, 2, T])
                        B_cs = Buf("cs")
                        S.dma(SP, cs[:].rearrange("p a t -> p (a t)"), cs_d[:, :], B_cs, writes=[B_cs])
                        pr = prot([0, 1, 2, 3])
                        pr2 = prot([4, 5])

                        def rope_evac(P, Bp, n, c0, c1, bias_ap, dst_ap, dstB, acc=True):
                            raw, Braw = fscr.get()
                            S.op(ACT, lambda e: e.activation(out=raw[:, 0:n], in_=P[:, 0:n], func=AF.Identity, bias=bias_ap),
                                 reads=[Bp, B_cf], writes=[Braw])
                            P2, Bp2 = pr2.get()
                            S.op(PE, lambda e: e.matmul(P2[:, 0:n], lhsT=pswap, rhs=raw[:, 0:n], start=True, stop=True),
                                 reads=[Braw, B_cf], writes=[Bp2])
                            t1, Bt1 = fscr.get()
                            S.op(DVE, lambda e: e.tensor_tensor(out=t1[:, 0:n], in0=raw[:, 0:n], in1=cs[:, 0, c0:c1], op=ALU.mult),
                                 reads=[Braw, B_cs], writes=[Bt1])
                            t2, Bt2 = fscr.get()
                            S.op(DVE, lambda e: e.tensor_tensor(out=t2[:, 0:n], in0=P2[:, 0:n], in1=cs[:, 1, c0:c1], op=ALU.mult),
                                 reads=[Bp2, B_cs], writes=[Bt2])
                            S.op(DVE, lambda e: e.tensor_tensor(out=dst_ap, in0=t1[:, 0:n], in1=t2[:, 0:n], op=ALU.add),
                                 reads=[Bt1, Bt2], accw=[dstB])

                        units = [0, 1]
                        load = stream_weights(units, lambda g: wk_d[g], 2048)
                        load(0)
                        for gp in units:
                            load(gp + 1)
                            w = wsl[gp % 2][:, 0:2048].rearrange("p (k m) -> p k m", k=NJ)
                            for (c0, c1) in [(0, 16), (48, 176), G1, G2]:
                                n = c1 - c0
                                P, Bp = pr.get()
                                S.group(PE, [lambda e, kc=kc: e.matmul(P[:, 0:n], lhsT=w[:, kc, :], rhs=hbcol(kc, c0, c1), start=(kc == 0), stop=(kc == NJ - 1)) for kc in range(NJ)],
                                        reads=[wslB[gp % 2], HB[tg_of(c0)]], writes=[Bp])
                                a, b = CF['bk']
                                rope_evac(P, Bp, n, c0, c1, cf[:, a + gp:a + gp + 1], KT[:, gp, c0:c1], B_KT)
                        S.dma(POOL, wsl[0][:, 0:4096], wv_d[:, :], wslB[0], writes=[wslB[0]])
                        wv = wsl[0][:, 0:4096].rearrange("p (k m) -> p k m", k=NJ)
                        vts = [(0, 16)] + [(48 + 128 * m, 48 + 128 * (m + 1)) for m in range(9)]
                        for vi, (c0, c1) in enumerate(vts):
                            n = c1 - c0
                            P, Bp = pr.get()
                            S.group(PE, [lambda e, kc=kc: e.matmul(P[0:n, 0:256], lhsT=hbcol(kc, c0, c1), rhs=wv[:, kc, :], start=(kc == 0), stop=(kc == NJ - 1)) for kc in range(NJ)],
                                    reads=[wslB[0], HB[tg_of(c0)]], writes=[Bp])
                            S.op(DVE, lambda e: e.tensor_tensor(out=V[0:n, vi, :], in0=P[0:n, 0:256], in1=cfv('bvb')[0:n, :], op=ALU.add),
                                 reads=[Bp, B_cf], accw=[B_V])
                        units = list(range(NJ))
                        load = stream_weights(units, lambda j: wq_d[j], 2048)
                        load(0)
                        for j in units:
                            load(j + 1)
                            w = wsl[j % 2][:, 0:2048].rearrange("p (k m) -> p k m", k=NJ)
                            for tg in (G1, G2):
                                c0, c1 = tg
                                n = c1 - c0
                                P, Bp = pr.get()
                                S.group(PE, [lambda e, kc=kc: e.matmul(P[:, 0:n], lhsT=w[:, kc, :], rhs=hbcol(kc, c0, c1), start=(kc == 0), stop=(kc == NJ - 1)) for kc in range(NJ)],
                                        reads=[wslB[j % 2], HB[tg]], writes=[Bp])
                                rope_evac(P, Bp, n, c0, c1, cfv('bq', j), QT[:, j, c0 - TE:c1 - TE], B_QT[tg])
                        S.barrier()
                    if 'qkv' in dbg:
                        dump("KT", KT[:].rearrange("p g t -> p (g t)"), [], [128, 2 * T], BF16)
                        dump("V", V[:].rearrange("p g t -> p (g t)"), [], [128, 10 * 256], BF16)
                        dump("QT", QT[:].rearrange("p g t -> p (g t)"), [], [128, NJ * TO], BF16)

                    Eo = Rot([(sb(LQ, f"Eo{i}", [128, 512], BF16), Buf(f"Eo{i}")) for i in range(3)])
                    Ep = Rot([(sb(LQ, f"Ep{i}", [128, 512], BF16), Buf(f"Ep{i}")) for i in range(3)])
                    Em = Rot([(sb(LQ, f"Em{i}", [16, 512], BF16), Buf(f"Em{i}")) for i in range(3)])
                    pS = prot([0, 1, 2, 3, 4, 5])
                    pO = prot([6])
                    pDn = prot([7])
                    ascr = Rot([(sb(LQ, f"as{i}", [128, 512]), Buf(f"as{i}")) for i in range(6)])
                    its = [(gp, half, n_, quad) for gp in range(2) for half in range(2) for n_ in range(8) for quad in range(2)]
                    stS = {}
                    stE = {}

                    def geom(it):
                        gp, half, n_, quad = it
                        r0, r1 = half * 64, half * 64 + 64
                        own = (TE + 128 * n_, TE + 128 * n_ + 128)
                        prv = (48 + 128 * n_, 48 + 128 * n_ + 128)
                        qtg = G1 if n_ < 4 else G2
                        cj = gp * 8 + quad * 4
                        return gp, half, n_, quad, r0, r1, own, prv, qtg, cj

                    def stage_S(k):
                        gp, half, n_, quad, r0, r1, own, prv, qtg, cj = geom(its[k])
                        rhsQ = QT[r0:r1, cj:cj + 4, n_ * 128:(n_ + 1) * 128]
                        PSo, Bso = pS.get()
                        PSp, Bsp = pS.get()
                        PSm, Bsm = pS.get()
                        S.op(PE, lambda e: e.matmul(PSo[:, :], lhsT=KT[r0:r1, gp, own[0]:own[1]], rhs=rhsQ, start=True, stop=True),
                             reads=[B_KT, B_QT[qtg]], writes=[Bso])
                        S.op(PE, lambda e: e.matmul(PSp[:, :], lhsT=KT[r0:r1, gp, prv[0]:prv[1]], rhs=rhsQ, start=True, stop=True),
                             reads=[B_KT, B_QT[qtg]], writes=[Bsp])
                        S.op(PE, lambda e: e.matmul(PSm[0:16, :], lhsT=KT[r0:r1, gp, 0:16], rhs=rhsQ, start=True, stop=True),
                             reads=[B_KT, B_QT[qtg]], writes=[Bsm])
                        stS[k] = (PSo, Bso, PSp, Bsp, PSm, Bsm)

                    def stage_X(k):
                        gp, half, n_, quad, r0, r1, own, prv, qtg, cj = geom(its[k])
                        PSo, Bso, PSp, Bsp, PSm, Bsm = stS.pop(k)
                        eo, Beo = Eo.get()
                        ep, Bep = Ep.get()
                        em, Bem = Em.get()
                        S.op(ACT, lambda e: e.activation(out=eo[:, :], in_=PSo[:, :], func=AF.Exp, scale=0.125), reads=[Bso], writes=[Beo])
                        S.op(ACT, lambda e: e.activation(out=ep[:, :], in_=PSp[:, :], func=AF.Exp, scale=0.125), reads=[Bsp], writes=[Bep])
                        S.op(ACT, lambda e: e.activation(out=em[:, :], in_=PSm[0:16, :], func=AF.Exp, scale=0.125), reads=[Bsm], writes=[Bem])
                        eo3 = eo[:, :].rearrange("p (a b) -> p a b", a=4)
                        ep3 = ep[:, :].rearrange("p (a b) -> p a b", a=4)
                        S.op(DVE, lambda e: e.tensor_tensor(out=eo3, in0=eo3, in1=cbv('m_own').unsqueeze(1).broadcast_to([128, 4, 128]), op=ALU.mult),
                             reads=[B_cb], writes=[Beo])
                        mp = cbv('m_prev0') if n_ == 0 else cbv('m_prev')
                        S.op(DVE, lambda e: e.tensor_tensor(out=ep3, in0=ep3, in1=mp.unsqueeze(1).broadcast_to([128, 4, 128]), op=ALU.mult),
                             reads=[B_cb], writes=[Bep])
                        stE[k] = (eo, Beo, ep, Bep, em, Bem)

                    def stage_V(k):
                        gp, half, n_, quad, r0, r1, own, prv, qtg, cj = geom(its[k])
                        eo, Beo, ep, Bep, em, Bem = stE.pop(k)
                        PO, Bpo = pO.get()
                        PDn, Bpdn = pDn.get()
                        vo, vp = n_ + 2, n_ + 1
                        S.group(PE, [
                            lambda e: e.matmul(PO[:, :], lhsT=V[:, vo, gp * 128:(gp + 1) * 128], rhs=eo[:, :], start=True, stop=False),
                            lambda e: e.matmul(PO[:, :], lhsT=V[:, vp, gp * 128:(gp + 1) * 128], rhs=ep[:, :], start=False, stop=False),
                            lambda e: e.matmul(PO[:, :], lhsT=V[0:16, 0, gp * 128:(gp + 1) * 128], rhs=em[:, :], start=False, stop=True),
                        ], reads=[B_V, Beo, Bep, Bem], writes=[Bpo])
                        S.group(PE, [
                            lambda e: e.matmul(PDn[:, :], lhsT=ones, rhs=eo[:, :], start=True, stop=False),
                            lambda e: e.matmul(PDn[:, :], lhsT=ones, rhs=ep[:, :], start=False, stop=False),
                            lambda e: e.matmul(PDn[:, :], lhsT=ones[0:16, :], rhs=em[:, :], start=False, stop=True),
                        ], reads=[B_cb, Beo, Bep, Bem], writes=[Bpdn])
                        d1, Bd1 = ascr.get()
                        d13 = d1[r0:r1, :].rearrange("p (a b) -> p a b", a=4)
                        S.op(DVE, lambda e: e.tensor_tensor(out=d13, in0=PDn[r0:r1, :].rearrange("p (a b) -> p a b", a=4),
                                                            in1=esnk[r0:r1, cj:cj + 4].unsqueeze(2).broadcast_to([64, 4, 128]), op=ALU.add),
                             reads=[Bpdn, B_esnk], writes=[Bd1])
                        po, Bpos = ascr.get()
                        S.op(ACT, lambda e: e.copy(po[r0:r1, :], PO[r0:r1, :]), reads=[Bpo], writes=[Bpos])
                        S.op(ACT, lambda e: e.activation(out=d1[r0:r1, :], in_=d1[r0:r1, :], func=AF.Ln), reads=[Bd1], writes=[Bd1])
                        S.op(ACT, lambda e: e.activation(out=d1[r0:r1, :], in_=d1[r0:r1, :], func=AF.Exp, scale=-1.0), reads=[Bd1], writes=[Bd1])
                        S.op(DVE, lambda e: e.tensor_tensor(out=hbO[r0:r1, cj:cj + 4, n_ * 128:(n_ + 1) * 128],
                                                            in0=po[r0:r1, :].rearrange("p (a b) -> p a b", a=4),
                                                            in1=d1[r0:r1, :].rearrange("p (a b) -> p a b", a=4), op=ALU.mult),
                             reads=[Bpos, Bd1], accw=[HB[qtg]])

                    stage_S(0)
                    stage_X(0)
                    stage_S(1)
                    stage_X(1)
                    for k in range(len(its)):
                        if k + 2 < len(its):
                            stage_S(k + 2)
                            stage_X(k + 2)
                        stage_V(k)
                    S.barrier()
            if 'att' in dbg:
                dump("attT", hbO[:].rearrange("p j t -> p (j t)"), [], [128, NJ * TO], BF16)
            proj_residual((G1, G2), wo_d, 'bo', hcol, hbcol)
            layernorm((G1, G2), hcol, hbcol, 'mixg1', 'mixb1')
            if 'h3' in dbg:
                S.barrier()
                dump("h3O", hO[:].rearrange("p j t -> p (j t)"), [], [128, NJ * TO])
            if 'stop3' not in dbg:
                with ExitStack() as PM_:
                    moe(1, (G1, G2), TT_O, hcol, hbcol, PM_)
                    S.barrier()
                layernorm((G1, G2), hcol, hbcol, 'ffng1', 'ffnb1', write_hb=False)
            S.barrier()

        if not _CACHE_OUT_DONE:
            with ExitStack() as LO:
                ost = [sb(LO, f"ost{i}", [128, D]) for i in range(2)]
                Bost = [Buf("ost0"), Buf("ost1")]
                pr = prot([0, 1, 2, 3])
                for k in range(8):
                    o = ost[k % 2]
                    tg = G1 if k < 4 else G2
                    for jq in range(4):
                        P, Bp = pr.get()
                        S.group(PE, [lambda e, i=i: e.transpose(P[:, i * 128:(i + 1) * 128], hO[:, jq * 4 + i, k * 128:(k + 1) * 128], ident) for i in range(4)],
                                reads=[B_cf] + [H[(jq * 4 + i, tg)] for i in range(4)], writes=[Bp])
                        eng = ACT if jq % 2 == 0 else DVE
                        if eng is ACT:
                            S.op(ACT, lambda e: e.copy(o[:, jq * 512:(jq + 1) * 512], P[:, :]), reads=[Bp], writes=[Bost[k % 2]] if jq == 0 else [], accw=[] if jq == 0 else [Bost[k % 2]])
                        else:
                            S.op(DVE, lambda e: e.tensor_copy(out=o[:, jq * 512:(jq + 1) * 512], in_=P[:, :]), reads=[Bp], accw=[Bost[k % 2]])
                    S.dma(SP, out_d[k * 128:(k + 1) * 128, :], o[:, :], Bost[k % 2], reads=[Bost[k % 2]])
                S.wait_all(SP, Bost)
                S.barrier()

    return nc, dbg_d


def _fm_vec(v):
    return np.ascontiguousarray(np.asarray(v, np.float32).reshape(NJ, 128).T)


def _head_perm():
    idx = []
    for jp in range(NJ):
        gp, i = jp // 8, jp % 8
        for hd in (16 * gp + i, 16 * gp + 8 + i):
            idx.extend(range(hd * 64, hd * 64 + 64))
    return np.array(idx)


def _w_fm(w, ncols_chunk):
    K, C = w.shape
    nch = C // ncols_chunk
    a = w.reshape(NJ, 128, nch, ncols_chunk).transpose(2, 1, 0, 3)
    return np.ascontiguousarray(a).reshape(nch, 128, NJ * ncols_chunk)


def prepare(inputs):
    f = lambda k: np.asarray(inputs[k], np.float32)
    x = f('x')[0]
    meta = f('meta_tokens')
    h0 = np.concatenate([meta, x], 0)
    shared = {}
    w_in = f('conv_w_in')[0]
    wv_ = w_in[:, :D].reshape(NJ, 128, NJ, 128)
    wg_ = w_in[:, D:].reshape(NJ, 128, NJ, 128)
    win = np.stack([wv_, wg_], 3)
    shared['win'] = np.ascontiguousarray(win.transpose(2, 1, 0, 3, 4)).reshape(NJ, 128, 4096)
    shared['wout'] = _w_fm(f('conv_w_out')[0], 128)
    w1 = f('expert_w1')
    w3 = f('expert_w3')
    a1 = w1.reshape(2, 32, NJ, 128, 2, 128)
    a3 = w3.reshape(2, 32, NJ, 128, 2, 128)
    w13 = np.stack([a1, a3], 5)
    shared['w13'] = np.ascontiguousarray(w13.transpose(0, 1, 4, 3, 2, 5, 6)).reshape(2, 32, 2, 128, 4096)
    del w13, a1, a3
    w2 = f('expert_w2').reshape(2, 32, 2, 128, D)
    shared['w2'] = np.ascontiguousarray(w2.transpose(0, 1, 3, 2, 4)).reshape(2, 32, 128, 4096)
    wr = np.concatenate([f('router_group_w'), f('router_expert_w')], -1)
    shared['wr'] = np.ascontiguousarray(wr.reshape(2, NJ, 128, 36).transpose(0, 2, 1, 3)).reshape(2, 128, NJ * 36)
    shared['wk'] = _w_fm(f('w_k'), 128)
    shared['wv'] = _w_fm(f('w_v'), 256)[0]
    perm = _head_perm()
    shared['wq'] = _w_fm(f('w_q')[0][:, perm], 128)
    shared['wo'] = _w_fm(f('w_o')[0][perm, :], 128)
    cfp = np.zeros((128, NCF), np.float32)

    def put(name, arr):
        a, b = CF[name]
        cfp[:, a:b] = arr
    put('ident', np.eye(128, dtype=np.float32))
    put('onesf', np.ones((128, 128), np.float32))
    ps = np.zeros((128, 128), np.float32)
    for m in range(128):
        d = m % 64
        if d < 8:
            ps[m + 8, m] = 1.0
        elif d < 16:
            ps[m - 8, m] = 1.0
    put('pswap', ps)
    b_in = f('conv_b_in')[0]
    put('b1', _fm_vec(b_in[:D]))
    put('b2', _fm_vec(b_in[D:]))
    put('bdw', _fm_vec(f('conv_b_dw')[0]))
    put('clng', _fm_vec(f('conv_ln_g')[0]))
    put('clnb', _fm_vec(f('conv_ln_b')[0]))
    put('bout', _fm_vec(f('conv_b_out')[0]))
    for l in range(2):
        put('mixg%d' % l, _fm_vec(f('ln_mix_g')[l]))
        put('mixb%d' % l, _fm_vec(f('ln_mix_b')[l]))
        put('ffng%d' % l, _fm_vec(f('ln_ffn_g')[l]))
        put('ffnb%d' % l, _fm_vec(f('ln_ffn_b')[l]))
    put('bq', _fm_vec(f('b_q')[0][perm]))
    put('bo', _fm_vec(f('b_o')[0]))
    put('snk', _fm_vec(np.repeat(f('sinks')[0], 64)[perm]))
    put('bk', np.ascontiguousarray(f('b_k').reshape(2, 128).T))
    wdw = f('conv_w_dw')[0]
    put('wdw', np.ascontiguousarray(wdw.reshape(31, NJ, 128).transpose(2, 1, 0)).reshape(128, NJ * 31))
    for l in range(2):
        br = np.concatenate([f('router_group_b')[l], f('router_expert_b')[l]])
        put('brb%d' % l, np.broadcast_to(br[None, :], (128, 36)))
    put('bvb', np.broadcast_to(f('b_v')[None, :], (128, 256)))
    kk = np.arange(128)[:, None]
    qq = np.arange(128)[None, :]
    m_own = (kk <= qq).astype(np.float32)
    m_prev = (kk > qq).astype(np.float32)
    sel = np.zeros((64, 32, 128), np.float32)
    for e in range(32):
        sel[e, e, :] = 1.0
        sel[32 + e, e, :] = 1.0
    shared['sel'] = sel.reshape(64, 32 * 128)
    inv_freq = (np.float32(500000.0) ** (-np.arange(0, 16, 2, dtype=np.float32) / np.float32(16))).astype(np.float32)
    in_maps = []
    for c in range(NCORES):
        own0 = 16 + 1024 * c
        pos = np.concatenate([np.arange(16), np.arange(own0 - 160, own0 + 1024)])
        valid = pos >= 0
        xe = np.zeros((T, D), np.float32)
        xe[valid] = h0[pos[valid]]
        cfc = cfp.copy()
        a, b = CF['padmask']
        cfc[:, a:b] = valid[16:176].astype(np.float32)[None, :]
        cbc = np.zeros((128, NCB), np.float32)
        cbc[:, 0:128] = 1.0
        cbc[:, 128:256] = np.eye(128, dtype=np.float32)
        cbc[:, 256:384] = m_own
        cbc[:, 384:512] = m_prev
        cbc[:, 512:640] = m_prev if c > 0 else 0.0
        ang = np.clip(pos, 0, None).astype(np.float32)[:, None] * inv_freq[None, :]
        cosv = np.cos(ang).astype(np.float32)
        sinv = np.sin(ang).astype(np.float32)
        cst = np.zeros((128, 2, T), np.float32)
        cst[:, 0, :] = 1.0
        for p in range(128):
            d = p % 64
            if d < 16:
                cst[p, 0, :] = cosv[:, d % 8]
                cst[p, 1, :] = -sinv[:, d % 8] if d < 8 else sinv[:, d % 8]
        m = dict(shared)
        m['xe'] = xe
        m['cf'] = cfc
        m['cb'] = cbc
        m['cs'] = cst.reshape(128, 2 * T)
        in_maps.append(m)
    return in_maps


_CACHE = {}


def kernel(**inputs):
    in_maps = prepare(inputs)
    if 'nc' not in _CACHE:
        _CACHE['nc'] = build()[0]
    nc = _CACHE['nc']
    res = run_bass_kernel_spmd(nc, in_maps, core_ids=list(range(NCORES)))
    out = np.concatenate([np.asarray(r["out"], np.float32) for r in res.results], 0)
    return out.reshape(1, NCORES * TO, D)
```

```python
import numpy as np
from contextlib import ExitStack
import concourse.bass as bass
import concourse.mybir as mybir
from concourse.bass_utils import run_bass_kernel_spmd

F32 = mybir.dt.float32
BF16 = mybir.dt.bfloat16
AF = mybir.ActivationFunctionType
ALU = mybir.AluOpType
AX = mybir.AxisListType

NCORES = 8
D = 2048
NJ = 16
T = 1200
TE = 176
TO = 1024
ALPHA = float((2.0 * 2) ** 0.25)
EPS = 1e-5
G0, G1, G2 = (0, 176), (176, 688), (688, 1200)
TT_E = [(0, 128), (128, 176)]
TT_O = [(176 + 128 * k, 176 + 128 * (k + 1)) for k in range(8)]
BIG = 1.0e9


class Sem:
    _n = 0

    def __init__(self, h):
        self.h = h
        self.id = Sem._n
        Sem._n += 1


class Buf:
    def __init__(self, name):
        self.name = name
        self.w = {}
        self.wf = {}
        self.rd = {}
        self.dsem = None
        self.dcnt = 0
        self.psum = name.startswith('ps')


class Eng:
    def __init__(self, name, h, sem):
        self.name = name
        self.h = h
        self.sem = sem
        self.cnt = 0
        self.seen = {}


class Sched:
    def __init__(self, nc, es):
        self.nc = nc
        self.es = es
        mk = lambda n: Sem(es.enter_context(nc.semaphore(n)))
        self.pe = Eng('pe', nc.tensor, mk('s_pe'))
        self.act = Eng('act', nc.scalar, mk('s_act'))
        self.dve = Eng('dve', nc.vector, mk('s_dve'))
        self.pool = Eng('pool', nc.gpsimd, mk('s_pool'))
        self.sp = Eng('sp', nc.sync, mk('s_sp'))
        self.engs = [self.pe, self.act, self.dve, self.pool, self.sp]
        self.dsems = []

    def _deps(self, reads, writes, accw):
        deps = []
        for b in reads:
            deps.extend(b.w.values())
            if b.psum:
                deps.extend(b.rd.values())
        for b in writes:
            deps.extend(b.w.values())
            deps.extend(b.rd.values())
        for b in accw:
            deps.extend(b.rd.values())
            deps.extend(b.wf.values())
        return deps

    def _wait(self, eng, deps):
        best = {}
        for (s, v) in deps:
            if best.get(s.id, (None, 0))[1] < v:
                best[s.id] = (s, v)
        for sid, (s, v) in best.items():
            if eng is self.pe and s is self.pe.sem:
                continue
            if eng.seen.get(sid, 0) < v:
                eng.h.wait_ge(s.h, v)
                eng.seen[sid] = v

    def _mark(self, tag, reads, writes, accw):
        for b in reads:
            b.rd[tag[0].id] = tag
        for b in writes:
            b.w = {tag[0].id: tag}
            b.wf = {tag[0].id: tag}
        for b in accw:
            b.w[tag[0].id] = tag

    def op(self, eng, fn, reads=(), writes=(), accw=()):
        self._wait(eng, self._deps(reads, writes, accw))
        ins = fn(eng.h)
        ins.then_inc(eng.sem.h, 1)
        eng.cnt += 1
        self._mark((eng.sem, eng.cnt), reads, writes, accw)

    def group(self, eng, fns, reads=(), writes=(), accw=()):
        self._wait(eng, self._deps(reads, writes, accw))
        ins = None
        for fn in fns:
            ins = fn(eng.h)
        ins.then_inc(eng.sem.h, 1)
        eng.cnt += 1
        self._mark((eng.sem, eng.cnt), reads, writes, accw)

    def dma(self, eng, out, in_, owner, reads=(), writes=(), accw=()):
        if owner.dsem is None:
            owner.dsem = Sem(self.es.enter_context(self.nc.semaphore('d_' + owner.name)))
            self.dsems.append(owner)
        self._wait(eng, self._deps(reads, writes, accw))
        eng.h.dma_start(out=out, in_=in_).then_inc(owner.dsem.h, 16)
        owner.dcnt += 16
        self._mark((owner.dsem, owner.dcnt), reads, writes, accw)

    def barrier(self):
        for e in self.engs:
            for e2 in self.engs:
                if e2 is e or e2.cnt == 0:
                    continue
                if e.seen.get(e2.sem.id, 0) < e2.cnt:
                    e.h.wait_ge(e2.sem.h, e2.cnt)
                    e.seen[e2.sem.id] = e2.cnt
            for o in self.dsems:
                if o.dcnt and e.seen.get(o.dsem.id, 0) < o.dcnt:
                    e.h.wait_ge(o.dsem.h, o.dcnt)
                    e.seen[o.dsem.id] = o.dcnt

    def wait_all(self, eng, bufs):
        deps = []
        for b in bufs:
            deps.extend(b.w.values())
            deps.extend(b.rd.values())
        self._wait(eng, deps)


class Rot:
    def __init__(self, items):
        self.items = items
        self.i = 0

    def get(self):
        it = self.items[self.i % len(self.items)]
        self.i += 1
        return it


VEC_NAMES = ['b1', 'b2', 'bdw', 'clng', 'clnb', 'bout', 'mixg0', 'mixb0', 'ffng0', 'ffnb0',
             'mixg1', 'mixb1', 'ffng1', 'ffnb1', 'bq', 'bo', 'snk']
CF = {}
_o = 0
for _n, _w in [('ident', 128), ('pswap', 128), ('onesf', 128)] + [(v, 16) for v in VEC_NAMES] + \
        [('bk', 2), ('wdw', 16 * 31), ('padmask', 160), ('brb0', 36), ('brb1', 36), ('bvb', 256)]:
    CF[_n] = (_o, _o + _w)
    _o += _w
NCF = _o
CB = {'ones': (0, 128), 'identb': (128, 256), 'm_own': (256, 384), 'm_prev': (384, 512),
      'm_prev0': (512, 640)}
NCB = 640


class _Stop(Exception):
    pass


def build(dbg=()):
    dbg = set(dbg)
    try:
        return _build(dbg)
    except _Stop as st:
        return st.args


def _build(dbg):
    _CACHE_OUT_DONE = []
    nc = bass.Bass("TRN2", target_bir_lowering=False)

    def din(name, shape):
        return nc.dram_tensor(name, list(shape), F32, kind="ExternalInput").ap()

    xe_d = din("xe", [T, D])
    cf_d = din("cf", [128, NCF])
    cb_d = din("cb", [128, NCB])
    cs_d = din("cs", [128, 2 * T])
    sel_d = din("sel", [64, 32 * 128])
    win_d = din("win", [NJ, 128, 4096])
    wout_d = din("wout", [NJ, 128, 2048])
    w13_d = din("w13", [2, 32, 2, 128, 4096])
    w2_d = din("w2", [2, 32, 128, 4096])
    wr_d = din("wr", [2, 128, 16 * 36])
    wk_d = din("wk", [2, 128, 2048])
    wv_d = din("wv", [128, 4096])
    wq_d = din("wq", [NJ, 128, 2048])
    wo_d = din("wo", [NJ, 128, 2048])
    out_d = nc.dram_tensor("out", [TO, D], F32, kind="ExternalOutput").ap()
    dbg_d = {}

    with ExitStack() as es:
        S = Sched(nc, es)
        PE, ACT, DVE, POOL, SP = S.pe, S.act, S.dve, S.pool, S.sp

        def sb(scope, name, shape, dt=F32):
            return scope.enter_context(nc.sbuf_tensor(name, list(shape), dt))

        hO = sb(es, "hO", [128, NJ, TO])
        hbO = sb(es, "hbO", [128, NJ, TO], BF16)
        hbE = sb(es, "hbE", [128, NJ, TE], BF16)
        wsl = [sb(es, f"wsl{i}", [128, 4096], BF16) for i in range(2)]
        wslB = [Buf(f"wsl{i}") for i in range(2)]
        cf = sb(es, "cf_sb", [128, NCF])
        cb = sb(es, "cb_sb", [128, NCB], BF16)
        st_mean = sb(es, "st_mean", [128, 512])
        st_rstd = sb(es, "st_rstd", [128, 512])
        B_mean, B_rstd = Buf("st_mean"), Buf("st_rstd")
        _st0 = (st_mean, B_mean, st_rstd, B_rstd)
        fscr = Rot([(sb(es, f"fs{i}", [128, 512]), Buf(f"fs{i}")) for i in range(5)])
        bscr = Rot([(sb(es, f"bs{i}", [128, 512], BF16), Buf(f"bs{i}")) for i in range(3)])
        esnk = sb(es, "esnk", [128, 16])
        B_esnk = Buf("esnk")
        B_cf, B_cb = Buf("cf"), Buf("cb")

        PS = [es.enter_context(nc.psum_tensor(f"ps{i}", [128, 512], F32)) for i in range(8)]
        PB = [Buf(f"ps{i}") for i in range(8)]

        def prot(idx):
            return Rot([(PS[i], PB[i]) for i in idx])

        HB = {G0: Buf("hb0"), G1: Buf("hb1"), G2: Buf("hb2")}
        H = {(j, g): Buf(f"h{j}_{g[0]}") for j in range(NJ) for g in (G0, G1, G2)}

        def tg_of(c0):
            return G0 if c0 < 176 else (G1 if c0 < 688 else G2)

        def cfv(name, j=None):
            a, b = CF[name]
            if j is None:
                return cf[:, a:b]
            return cf[:, a + j:a + j + 1]

        def cbv(name):
            a, b = CB[name]
            return cb[:, a:b]

        S.dma(SP, cf[:], cf_d[:, :], B_cf, writes=[B_cf])
        S.dma(POOL, cb[:], cb_d[:, :], B_cb, writes=[B_cb])
        S.op(ACT, lambda e: e.activation(out=esnk[:], in_=cfv('snk'), func=AF.Exp),
             reads=[B_cf], writes=[B_esnk])
        ident = cfv('ident')
        pswap = cfv('pswap')
        ones = cbv('ones')

        if 'stop0' in dbg:
            dump_esnk = True
        def dump(name, ap_sb, bufs, shape, dt=F32):
            d = nc.dram_tensor("o_" + name, list(shape), dt, kind="ExternalOutput").ap()
            dbg_d[name] = d
            ob = Buf("dbg_" + name)
            S.dma(SP, d, ap_sb, ob, reads=bufs)
            S.wait_all(SP, [ob])
            S._wait(SP, [(ob.dsem, ob.dcnt)])
            S.barrier()

        if 'stop0' in dbg:
            dump('esnk', esnk[:], [B_esnk], [128, 16])
            dump('cbd', cb[:], [B_cb], [128, NCB], BF16)
            S.barrier()
            raise _Stop(nc, dbg_d)
        _loaders = {}

        def stream_weights(units, src_fn, width, key=None):
            if key is not None and key in _loaders:
                return _loaders[key]
            issued = set()

            def load(i):
                if i in issued or i >= len(units):
                    return
                issued.add(i)
                k = i % 2
                S.dma(POOL, wsl[k][:, 0:width], src_fn(units[i]), wslB[k], writes=[wslB[k]])
            if key is not None:
                _loaders[key] = load
            return load

        def moe_loader(layer):
            units = [(e, fc) for e in range(32) for fc in range(2)]
            return stream_weights(units, lambda u: w13_d[layer, u[0], u[1]], 4096, key=('w13', layer))

        def proj_loader(wsrc, key):
            return stream_weights(list(range(NJ)), lambda j: wsrc[j], 2048, key=key)

        def prefetch2(load):
            load(0)
            load(1)

        def layernorm(tgs, hcol, hbcol, gname, bname, write_hb=True, after=None):
            with ExitStack() as sc:
                uid = len(S.dsems) * 1000 + S.act.cnt
                sts = [(st_mean, B_mean, st_rstd, B_rstd),
                       (sb(sc, f"lnm{uid}", [128, 512]), Buf("lnm"), sb(sc, f"lnr{uid}", [128, 512]), Buf("lnr"))]
                vsc = Rot([(sb(sc, f"lnv{uid}_{i}", [128, 512], BF16), Buf(f"lnv{i}")) for i in range(4)])
                p12 = prot([6, 7, 4, 5])

                def stats(ti):
                    tg = tgs[ti]
                    c0, c1 = tg
                    n = c1 - c0
                    P1, B1 = p12.get()
                    P2, B2 = p12.get()
                    for j in range(NJ):
                        if True:
                            vb, Bvb = vsc.get()
                            S.op(ACT, lambda e: e.activation(out=vb[:, 0:n], in_=hcol(j, c0, c1), func=AF.Copy),
                                 reads=[H[(j, tg)]], writes=[Bvb])
                            S.op(PE, lambda e: e.matmul(P1[:, 0:n], lhsT=ones, rhs=vb[:, 0:n], start=(j == 0), stop=(j == NJ - 1)),
                                 reads=[Bvb, B_cb], writes=[B1] if j == 0 else [], accw=[] if j == 0 else [B1])
                        else:
                            S.op(PE, lambda e: e.matmul(P1[:, 0:n], lhsT=cfv('onesf'), rhs=hcol(j, c0, c1), start=(j == 0), stop=(j == NJ - 1)),
                                 reads=[H[(j, tg)], B_cf], writes=[B1] if j == 0 else [], accw=[] if j == 0 else [B1])
                        vq, Bvq = vsc.get()
                        S.op(ACT, lambda e: e.activation(out=vq[:, 0:n], in_=hcol(j, c0, c1), func=AF.Square),
                             reads=[H[(j, tg)]], writes=[Bvq])
                        S.op(PE, lambda e: e.matmul(P2[:, 0:n], lhsT=ones, rhs=vq[:, 0:n], start=(j == 0), stop=(j == NJ - 1)),
                             reads=[Bvq, B_cb], writes=[B2] if j == 0 else [], accw=[] if j == 0 else [B2])
                    ln_stats(P1, B1, P2, B2, n, sts[ti % 2])

                nsc = Rot([(sb(sc, f"lns{uid}_{i}", [128, 512]), Buf(f"lns{i}")) for i in range(8)])

                def norm(ti):
                    tg = tgs[ti]
                    c0, c1 = tg
                    n = c1 - c0
                    mean, Bm, rstd, Br = sts[ti % 2]
                    for jb in range(0, NJ, 4):
                        js = list(range(jb, jb + 4))
                        t1s = {}
                        t2s = {}
                        for j in js:
                            t1s[j] = nsc.get()
                            t1, Bt1 = t1s[j]
                            S.op(DVE, lambda e: e.tensor_tensor(out=t1[:, 0:n], in0=hcol(j, c0, c1), in1=mean[:, 0:n], op=ALU.subtract),
                                 reads=[H[(j, tg)], Bm], writes=[Bt1])
                        for j in js:
                            t1, Bt1 = t1s[j]
                            t2s[j] = nsc.get()
                            t2, Bt2 = t2s[j]
                            S.op(DVE, lambda e: e.scalar_tensor_tensor(out=t2[:, 0:n], in0=t1[:, 0:n], scalar=cfv(gname, j), in1=rstd[:, 0:n], op0=ALU.mult, op1=ALU.mult),
                                 reads=[Bt1, Br, B_cf], writes=[Bt2])
                        for j in js:
                            t2, Bt2 = t2s[j]
                            S.op(ACT, lambda e: e.activation(out=hcol(j, c0, c1), in_=t2[:, 0:n], func=AF.Identity, bias=cfv(bname, j)),
                                 reads=[Bt2, B_cf], writes=[H[(j, tg)]])
                        if write_hb:
                            for j in js:
                                t2, Bt2 = t2s[j]
                                if j % 8 < 5:
                                    S.op(ACT, lambda e: e.activation(out=hbcol(j, c0, c1), in_=t2[:, 0:n], func=AF.Identity, bias=cfv(bname, j)),
                                         reads=[Bt2, B_cf], accw=[HB[tg]])
                                else:
                                    S.op(DVE, lambda e: e.tensor_scalar(out=hbcol(j, c0, c1), in0=t2[:, 0:n], scalar1=cfv(bname, j), scalar2=None, op0=ALU.add),
                                         reads=[Bt2, B_cf], accw=[HB[tg]])
                    if after is not None:
                        after(ti)

                stats(0)
                for ti in range(len(tgs)):
                    if ti + 1 < len(tgs):
                        stats(ti + 1)
                    norm(ti)
                S.barrier()

        def ln_stats(P1, B1, P2, B2, n, st=None):
            st_mean, B_mean, st_rstd, B_rstd = st if st is not None else _st0
            S.op(DVE, lambda e: e.tensor_scalar(out=st_mean[:, 0:n], in0=P1[:, 0:n], scalar1=1.0 / D, scalar2=None, op0=ALU.mult),
                 reads=[B1], writes=[B_mean])
            t1, Bt1 = fscr.get()
            S.op(DVE, lambda e: e.tensor_tensor(out=t1[:, 0:n], in0=st_mean[:, 0:n], in1=st_mean[:, 0:n], op=ALU.mult),
                 reads=[B_mean], writes=[Bt1])
            t2, Bt2 = fscr.get()
            S.op(DVE, lambda e: e.scalar_tensor_tensor(out=t2[:, 0:n], in0=P2[:, 0:n], scalar=1.0 / D, in1=t1[:, 0:n], op0=ALU.mult, op1=ALU.subtract),
                 reads=[B2, Bt1], writes=[Bt2])
            t3, Bt3 = fscr.get()
            S.op(ACT, lambda e: e.activation(out=t3[:, 0:n], in_=t2[:, 0:n], func=AF.Sqrt, bias=EPS, scale=1.0),
                 reads=[Bt2], writes=[Bt3])
            S.op(DVE, lambda e: e.reciprocal(out=st_rstd[:, 0:n], in_=t3[:, 0:n]),
                 reads=[Bt3], writes=[B_rstd])

        def proj_residual(tgs, wsrc, bias_name, hcol, hbcol):
            units = list(range(NJ))
            load = proj_loader(wsrc, ('proj', bias_name))
            pr = prot([0, 1, 2, 3])
            load(0)
            for j in units:
                load(j + 1)
                w = wsl[j % 2][:, 0:2048].rearrange("p (k m) -> p k m", k=NJ)
                for tg in tgs:
                    c0, c1 = tg
                    n = c1 - c0
                    P, Bp = pr.get()
                    S.group(PE, [lambda e, kc=kc: e.matmul(P[:, 0:n], lhsT=w[:, kc, :], rhs=hbcol(kc, c0, c1), start=(kc == 0), stop=(kc == NJ - 1)) for kc in range(NJ)],
                            reads=[wslB[j % 2], HB[tg]], writes=[Bp])
                    t, Bt = fscr.get()
                    S.op(ACT, lambda e, t=t, P=P: e.activation(out=t[:, 0:n], in_=P[:, 0:n], func=AF.Identity, bias=cfv(bias_name, j)),
                         reads=[Bp, B_cf], writes=[Bt])
                    S.op(DVE, lambda e, t=t: e.scalar_tensor_tensor(out=hcol(j, c0, c1), in0=hcol(j, c0, c1), scalar=ALPHA, in1=t[:, 0:n], op0=ALU.mult, op1=ALU.add),
                         reads=[Bt], writes=[H[(j, tg)]])

        def moe(layer, tgs, tts, hcol, hbcol, scope):
            wrt = sb(scope, f"wrt{layer}", [128, NJ, 36])
            B_wrt = Buf(f"wrt{layer}")
            sel = sb(scope, f"sel_sb{layer}", [64, 32, 128], BF16)
            B_sel = Buf(f"sel{layer}")
            gT = sb(scope, f"gT{layer}", [64, T], BF16)
            B_gT = {tg: Buf(f"gT{layer}_{tg[0]}") for tg in tgs}
            gball = sb(scope, f"gball{layer}", [128, T])
            B_gb = {tg: Buf(f"gb{layer}_{tg[0]}") for tg in tgs}
            hid = sb(scope, f"hid{layer}", [128, 2, 2, T], BF16)
            B_hid = [Buf(f"hid{layer}_0"), Buf(f"hid{layer}_1")]
            w2s = [sb(scope, f"w2s{layer}_{i}", [128, 2, D], BF16) for i in range(2)]
            B_w2 = [Buf(f"w2s{layer}_0"), Buf(f"w2s{layer}_1")]
            lg = sb(scope, f"lg{layer}", [128, 36])
            B_lg = Buf(f"lg{layer}")
            S.dma(SP, wrt[:].rearrange("p k m -> p (k m)"), wr_d[layer], B_wrt, writes=[B_wrt])
            S.dma(POOL, sel[:].rearrange("p e m -> p (e m)"), sel_d[:, :], B_sel, writes=[B_sel])
            brb = cfv('brb%d' % layer)
            units = [(e, fc) for e in range(32) for fc in range(2)]
            load = moe_loader(layer)
            load(0)
            NT = len(tts)
            pr = prot([6, 7])

            def wt(name, shape, dt=F32):
                return sb(scope, f"g{layer}_{name}", shape, dt), Buf(f"g{layer}_{name}")
            lgall, B_lga = wt("lgall", [128, NT, 36])
            S.op(POOL, lambda e: e.memset(lgall[:].rearrange("p t m -> p (t m)"), 0.0), writes=[B_lga])
            for ti, (c0, c1) in enumerate(tts):
                n = c1 - c0
                tg = tg_of(c0)
                P, Bp = pr.get()
                S.group(PE, [lambda e, kc=kc: e.matmul(P[0:n, 0:36], lhsT=hcol(kc, c0, c1), rhs=wrt[:, kc, :], start=(kc == 0), stop=(kc == NJ - 1)) for kc in range(NJ)],
                        reads=[B_wrt] + [H[(kc, tg)] for kc in range(NJ)], writes=[Bp])
                S.op(DVE, lambda e: e.tensor_tensor(out=lgall[0:n, ti, :], in0=P[0:n, 0:36], in1=brb[0:n, :], op=ALU.add),
                     reads=[Bp, B_cf], accw=[B_lga])
            lg4 = lgall[:, :, 0:4]
            le = lgall[:, :, 4:36]
            gmax, Bgmax = wt("gmax", [128, NT])
            S.op(DVE, lambda e: e.tensor_reduce(out=gmax[:], in_=lg4, axis=AX.X, op=ALU.max), reads=[B_lga], writes=[Bgmax])
            gd, Bgd = wt("gd", [128, NT, 4])
            S.op(DVE, lambda e: e.tensor_tensor(out=gd[:], in0=lg4, in1=gmax[:].unsqueeze(2).broadcast_to([128, NT, 4]), op=ALU.subtract),
                 reads=[B_lga, Bgmax], writes=[Bgd])
            gexp, Bgexp = wt("gexp", [128, NT, 4])
            S.op(ACT, lambda e: e.activation(out=gexp[:], in_=gd[:], func=AF.Exp), reads=[Bgd], writes=[Bgexp])
            gsum, Bgsum = wt("gsum", [128, NT])
            S.op(DVE, lambda e: e.tensor_reduce(out=gsum[:], in_=gexp[:], axis=AX.X, op=ALU.add), reads=[Bgexp], writes=[Bgsum])
            gw, Bgw = wt("gw", [128, NT])
            S.op(DVE, lambda e: e.reciprocal(out=gw[:], in_=gsum[:]), reads=[Bgsum], writes=[Bgw])
            pen, Bpen = wt("pen", [128, NT, 4])
            S.op(DVE, lambda e: e.tensor_scalar(out=pen[:], in0=gd[:], scalar1=0.0, scalar2=None, op0=ALU.is_equal), reads=[Bgd], writes=[Bpen])
            S.op(DVE, lambda e: e.tensor_scalar(out=pen[:], in0=pen[:], scalar1=BIG, scalar2=-BIG, op0=ALU.mult, op1=ALU.add), reads=[Bpen], writes=[Bpen])
            lem, Blem = wt("lem", [128, NT, 32])
            S.op(DVE, lambda e: e.tensor_tensor(out=lem[:].rearrange("p t (g k) -> p t g k", g=4), in0=le.rearrange("p t (g k) -> p t g k", g=4),
                                                in1=pen[:].unsqueeze(3).broadcast_to([128, NT, 4, 8]), op=ALU.add),
                 reads=[B_lga, Bpen], writes=[Blem])
            m1, Bm1 = wt("m1", [128, NT])
            S.op(DVE, lambda e: e.tensor_reduce(out=m1[:], in_=lem[:], axis=AX.X, op=ALU.max), reads=[Blem], writes=[Bm1])
            eq1, Beq1 = wt("eq1", [128, NT, 32])
            S.op(DVE, lambda e: e.tensor_tensor(out=eq1[:], in0=lem[:], in1=m1[:].unsqueeze(2).broadcast_to([128, NT, 32]), op=ALU.is_equal),
                 reads=[Blem, Bm1], writes=[Beq1])
            lem2, Blem2 = wt("lem2", [128, NT, 32])
            S.op(DVE, lambda e: e.scalar_tensor_tensor(out=lem2[:], in0=eq1[:], scalar=-BIG, in1=lem[:], op0=ALU.mult, op1=ALU.add),
                 reads=[Beq1, Blem], writes=[Blem2])
            m2, Bm2 = wt("m2", [128, NT])
            S.op(DVE, lambda e: e.tensor_reduce(out=m2[:], in_=lem2[:], axis=AX.X, op=ALU.max), reads=[Blem2], writes=[Bm2])
            eq2, Beq2 = wt("eq2", [128, NT, 32])
            S.op(DVE, lambda e: e.tensor_tensor(out=eq2[:], in0=lem2[:], in1=m2[:].unsqueeze(2).broadcast_to([128, NT, 32]), op=ALU.is_equal),
                 reads=[Blem2, Bm2], writes=[Beq2])
            dm, Bdm = wt("dm", [128, NT])
            S.op(DVE, lambda e: e.tensor_tensor(out=dm[:], in0=m1[:], in1=m2[:], op=ALU.subtract), reads=[Bm1, Bm2], writes=[Bdm])
            sg, Bsg = wt("sg", [128, NT])
            S.op(ACT, lambda e: e.activation(out=sg[:], in_=dm[:], func=AF.Sigmoid), reads=[Bdm], writes=[Bsg])
            wa, Bwa = wt("wa", [128, NT])
            S.op(DVE, lambda e: e.tensor_tensor(out=wa[:], in0=gw[:], in1=sg[:], op=ALU.mult), reads=[Bgw, Bsg], writes=[Bwa])
            wb, Bwb = wt("wb", [128, NT])
            S.op(DVE, lambda e: e.tensor_tensor(out=wb[:], in0=gw[:], in1=wa[:], op=ALU.subtract), reads=[Bgw, Bwa], writes=[Bwb])
            S.op(DVE, lambda e: e.tensor_tensor(out=eq1[:], in0=eq1[:], in1=wa[:].unsqueeze(2).broadcast_to([128, NT, 32]), op=ALU.mult),
                 reads=[Bwa], writes=[Beq1])
            S.op(DVE, lambda e: e.tensor_tensor(out=eq2[:], in0=eq2[:], in1=wb[:].unsqueeze(2).broadcast_to([128, NT, 32]), op=ALU.mult),
                 reads=[Bwb], writes=[Beq2])
            gate, Bgate = lem, Blem
            S.op(DVE, lambda e: e.tensor_tensor(out=gate[:], in0=eq1[:], in1=eq2[:], op=ALU.add), reads=[Beq1, Beq2], writes=[Bgate])
            if ('gate%d' % layer) in dbg:
                dump(f"gate{layer}", gate[:].rearrange("p t m -> p (t m)"), [Bgate], [128, NT * 32])
            hib, Bhib = wt("hib", [128, NT, 32], BF16)
            S.op(DVE, lambda e: e.tensor_copy(out=hib[:], in_=gate[:]), reads=[Bgate], writes=[Bhib])
            ghl, Bghl = wt("ghl", [128, NT, 64])
            S.op(DVE, lambda e: e.tensor_copy(out=ghl[:, :, 0:32], in_=hib[:]), reads=[Bhib], writes=[Bghl])
            S.op(DVE, lambda e: e.tensor_tensor(out=ghl[:, :, 32:64], in0=gate[:], in1=hib[:], op=ALU.subtract),
                 reads=[Bgate, Bhib], accw=[Bghl])
            for ti, (c0, c1) in enumerate(tts):
                n = c1 - c0
                tg = tg_of(c0)
                PT, Bpt = pr.get()
                S.op(PE, lambda e: e.transpose(PT[0:64, 0:n], ghl[0:n, ti, :], ident[0:n, 0:n]),
                     reads=[Bghl, B_cf], writes=[Bpt])
                S.op(ACT, lambda e: e.copy(gT[:, c0:c1], PT[0:64, 0:n]), reads=[Bpt], accw=[B_gT[tg]])

            pA = prot([0, 1])
            pB = prot([2, 3])
            pG = prot([4])
            pD = prot([5, 6, 7])
            for ui, (e_, fc) in enumerate(units):
                load(ui + 1)
                es_ = e_ % 2
                w13 = wsl[ui % 2][:, 0:4096].rearrange("p (k h m) -> p k h m", k=NJ, h=2)
                if fc == 0:
                    S.dma(POOL, w2s[es_][:].rearrange("p f d -> p (f d)"), w2_d[layer, e_], B_w2[es_], writes=[B_w2[es_]])
                for tg in tgs:
                    c0, c1 = tg
                    n = c1 - c0
                    if fc == 0:
                        PG, Bpg = pG.get()
                        S.op(PE, lambda e: e.matmul(PG[:, 0:n], lhsT=sel[:, e_, :], rhs=gT[:, c0:c1], start=True, stop=True),
                             reads=[B_sel, B_gT[tg]], writes=[Bpg])
                        S.op(ACT, lambda e: e.copy(gball[:, c0:c1], PG[:, 0:n]), reads=[Bpg], writes=[B_gb[tg]])
                    PA, Bpa = pA.get()
                    S.group(PE, [lambda e, kc=kc: e.matmul(PA[:, 0:n], lhsT=w13[:, kc, 0, :], rhs=hbcol(kc, c0, c1), start=(kc == 0), stop=(kc == NJ - 1)) for kc in range(NJ)],
                            reads=[wslB[ui % 2], HB[tg]], writes=[Bpa])
                    PBk, Bpb = pB.get()
                    S.group(PE, [lambda e, kc=kc: e.matmul(PBk[:, 0:n], lhsT=w13[:, kc, 1, :], rhs=hbcol(kc, c0, c1), start=(kc == 0), stop=(kc == NJ - 1)) for kc in range(NJ)],
                            reads=[wslB[ui % 2], HB[tg]], writes=[Bpb])
                    sa, Bsa = fscr.get()
                    S.op(ACT, lambda e: e.activation(out=sa[:, 0:n], in_=PA[:, 0:n], func=AF.Silu), reads=[Bpa], writes=[Bsa])
                    t, Bt = fscr.get()
                    S.op(DVE, lambda e: e.tensor_tensor(out=t[:, 0:n], in0=sa[:, 0:n], in1=gball[:, c0:c1], op=ALU.mult),
                         reads=[Bsa, B_gb[tg]], writes=[Bt])
                    S.op(DVE, lambda e: e.tensor_tensor(out=hid[:, es_, fc, c0:c1], in0=PBk[:, 0:n], in1=t[:, 0:n], op=ALU.mult),
                         reads=[Bpb, Bt], accw=[B_hid[es_]])
                if fc == 1 and es_ == 1:
                    first = (e_ == 1)
                    for j in range(NJ):
                        for tg in tgs:
                            c0, c1 = tg
                            n = c1 - c0
                            PD, Bpd = pD.get()
                            fns = []
                            for k, (ee, ff) in enumerate([(0, 0), (0, 1), (1, 0), (1, 1)]):
                                fns.append(lambda e, ee=ee, ff=ff, k=k: e.matmul(PD[:, 0:n], lhsT=w2s[ee][:, ff, j * 128:(j + 1) * 128], rhs=hid[:, ee, ff, c0:c1], start=(k == 0), stop=(k == 3)))
                            S.group(PE, fns, reads=[B_w2[0], B_w2[1], B_hid[0], B_hid[1]], writes=[Bpd])
                            if first:
                                S.op(DVE, lambda e: e.scalar_tensor_tensor(out=hcol(j, c0, c1), in0=hcol(j, c0, c1), scalar=ALPHA, in1=PD[:, 0:n], op0=ALU.mult, op1=ALU.add),
                                     reads=[Bpd], writes=[H[(j, tg)]])
                            else:
                                S.op(DVE, lambda e: e.tensor_tensor(out=hcol(j, c0, c1), in0=hcol(j, c0, c1), in1=PD[:, 0:n], op=ALU.add),
                                     reads=[Bpd], writes=[H[(j, tg)]])

        with ExitStack() as L0:
            hE = sb(L0, "hE", [128, NJ, TE])

            def hcol(j, c0, c1):
                return hE[:, j, c0:c1] if c1 <= TE else hO[:, j, c0 - TE:c1 - TE]

            def hbcol(j, c0, c1):
                return hbE[:, j, c0:c1] if c1 <= TE else hbO[:, j, c0 - TE:c1 - TE]

            def hcol4(j0, c0, c1):
                return hE[:, j0:j0 + 4, c0:c1] if c1 <= TE else hO[:, j0:j0 + 4, c0 - TE:c1 - TE]

            def hbcol4(j0, c0, c1):
                return hbE[:, j0:j0 + 4, c0:c1] if c1 <= TE else hbO[:, j0:j0 + 4, c0 - TE:c1 - TE]

            with ExitStack() as PA_:
                xs = [sb(PA_, f"xs{i}", [128, D]) for i in range(2)]
                xsB = [Buf(f"xs{i}") for i in range(2)]
                pr = prot([0, 1, 2, 3])
                tts = TT_E + TT_O
                S.dma(SP, xs[0][0:128, :], xe_d[0:128, :], xsB[0], writes=[xsB[0]])
                for ti, (c0, c1) in enumerate(tts):
                    n = c1 - c0
                    if ti + 1 < len(tts):
                        a0, a1 = tts[ti + 1]
                        S.dma(SP, xs[(ti + 1) % 2][0:a1 - a0, :], xe_d[a0:a1, :], xsB[(ti + 1) % 2], writes=[xsB[(ti + 1) % 2]])
                    x_t = xs[ti % 2]
                    tg = tg_of(c0)
                    for jq in range(4):
                        P, Bp = pr.get()
                        Pv = P[:, :].rearrange("p (a b) -> p a b", a=4)
                        S.group(PE, [lambda e, i=i: e.transpose(Pv[:, i, 0:n], x_t[0:n, (jq * 4 + i) * 128:(jq * 4 + i + 1) * 128], ident[0:n, 0:n]) for i in range(4)],
                                reads=[xsB[ti % 2], B_cf], writes=[Bp])
                        S.op(ACT, lambda e: e.copy(hcol4(jq * 4, c0, c1), Pv[:, :, 0:n]),
                             reads=[Bp], writes=[H[(jq * 4 + i, tg)] for i in range(4)] if False else [], accw=[H[(jq * 4 + i, tg)] for i in range(4)])
                        S.op(DVE, lambda e: e.tensor_copy(out=hbcol4(jq * 4, c0, c1), in_=hcol4(jq * 4, c0, c1)),
                             reads=[H[(jq * 4 + i, tg)] for i in range(4)], accw=[HB[tg]])
                S.barrier()
            def stop(flag):
                if flag in dbg:
                    S.barrier()
                    raise _Stop(nc, dbg_d)
            if 'h0' in dbg:
                dump("h0E", hE[:].rearrange("p j t -> p (j t)"), [], [128, NJ * TE])
                dump("h0O", hO[:].rearrange("p j t -> p (j t)"), [], [128, NJ * TO])

            stop('stopA')
            with ExitStack() as PB_:
                z = sb(PB_, "z", [128, NJ, T], BF16)
                Bz = {(j, g): Buf(f"z{j}_{g[0]}") for j in range(NJ) for g in (G0, G1, G2)}
                yb = [sb(PB_, f"yb{i}", [128, T + 30], BF16) for i in range(2)]
                Byb = [Buf("yb0"), Buf("yb1")]
                diag = sb(PB_, "diag", [128, 31, 128], BF16)
                Bdiag = Buf("diag")
                for i in range(2):
                    S.op(DVE, lambda e, i=i: e.memset(yb[i][:, 0:30], 0.0), writes=[Byb[i]])
                units = list(range(NJ))
                load = stream_weights(units, lambda j: win_d[j], 4096)
                pA = prot([0, 1])
                pB = prot([2, 3])
                pC = prot([4, 5])
                load(0)
                pending = []

                def conv(j, tg):
                    c0, c1 = tg
                    n = c1 - c0
                    y = yb[j % 2]
                    PC, Bpc = pC.get()
                    S.group(PE, [lambda e, k=k: e.matmul(PC[:, 0:n], lhsT=diag[:, k, :], rhs=y[:, c0 + k:c0 + k + n], start=(k == 0), stop=(k == 30)) for k in range(31)],
                            reads=[Bdiag, Byb[j % 2]], writes=[Bpc])
                    S.op(ACT, lambda e: e.activation(out=z[:, j, c0:c1], in_=PC[:, 0:n], func=AF.Identity, bias=cfv('bdw', j)),
                         reads=[Bpc, B_cf], writes=[Bz[(j, tg)]])

                for j in units:
                    load(j + 1)
                    w = wsl[j % 2][:, 0:4096].rearrange("p (k h m) -> p k h m", k=NJ, h=2)
                    y = yb[j % 2]
                    for tg in (G0, G1, G2):
                        c0, c1 = tg
                        n = c1 - c0
                        PA, Bpa = pA.get()
                        S.group(PE, [lambda e, kc=kc: e.matmul(PA[:, 0:n], lhsT=w[:, kc, 0, :], rhs=hbcol(kc, c0, c1), start=(kc == 0), stop=(kc == NJ - 1)) for kc in range(NJ)],
                                reads=[wslB[j % 2], HB[tg]], writes=[Bpa])
                        PBk, Bpb = pB.get()
                        S.group(PE, [lambda e, kc=kc: e.matmul(PBk[:, 0:n], lhsT=w[:, kc, 1, :], rhs=hbcol(kc, c0, c1), start=(kc == 0), stop=(kc == NJ - 1)) for kc in range(NJ)],
                                reads=[wslB[j % 2], HB[tg]], writes=[Bpb])
                        if pending:
                            pj, ptg = pending.pop(0)
                            conv(pj, ptg)
                        if tg == G0:
                            a, b = CF['wdw']
                            wd = cf[:, a + j * 31:a + (j + 1) * 31]
                            S.op(DVE, lambda e: e.tensor_tensor(out=diag[:], in0=cbv('identb').unsqueeze(1).broadcast_to([128, 31, 128]),
                                                                in1=wd.unsqueeze(2).broadcast_to([128, 31, 128]), op=ALU.mult),
                                 reads=[B_cb, B_cf], writes=[Bdiag])
                        sg, Bsg = fscr.get()
                        S.op(ACT, lambda e: e.activation(out=sg[:, 0:n], in_=PBk[:, 0:n], func=AF.Sigmoid, bias=cfv('b2', j)),
                             reads=[Bpb, B_cf], writes=[Bsg])
                        if tg == G0:
                            S.op(DVE, lambda e: e.scalar_tensor_tensor(out=y[:, 30 + c0:30 + c1], in0=PA[:, 0:n], scalar=cfv('b1', j), in1=sg[:, 0:n], op0=ALU.add, op1=ALU.mult),
                                 reads=[Bpa, Bsg, B_cf], writes=[Byb[j % 2]])
                            S.op(DVE, lambda e: e.tensor_tensor(out=y[:, 30 + 16:30 + 176], in0=y[:, 30 + 16:30 + 176], in1=cfv('padmask'), op=ALU.mult),
                                 reads=[B_cf], writes=[Byb[j % 2]])
                        else:
                            S.op(DVE, lambda e: e.scalar_tensor_tensor(out=y[:, 30 + c0:30 + c1], in0=PA[:, 0:n], scalar=cfv('b1', j), in1=sg[:, 0:n], op0=ALU.add, op1=ALU.mult),
                                 reads=[Bpa, Bsg, B_cf], accw=[Byb[j % 2]])
                        pending.append((j, tg))
                while pending:
                    pj, ptg = pending.pop(0)
                    conv(pj, ptg)
                if 'z' in dbg:
                    dump("z", z[:].rearrange("p j t -> p (j t)"), list(Bz.values()), [128, NJ * T], BF16)

                prefetch2(proj_loader(wout_d, ('proj', 'bout')))
                p12 = prot([6, 7, 0, 1])
                for tg in (G0, G1, G2):
                    c0, c1 = tg
                    n = c1 - c0
                    P1, B1 = p12.get()
                    P2, B2 = p12.get()
                    S.group(PE, [lambda e, j=j: e.matmul(P1[:, 0:n], lhsT=ones, rhs=z[:, j, c0:c1], start=(j == 0), stop=(j == NJ - 1)) for j in range(NJ)],
                            reads=[B_cb] + [Bz[(j, tg)] for j in range(NJ)], writes=[B1])
                    for j in range(NJ):
                        vq, Bvq = bscr.get()
                        S.op(ACT, lambda e, vq=vq, j=j: e.activation(out=vq[:, 0:n], in_=z[:, j, c0:c1], func=AF.Square),
                             reads=[Bz[(j, tg)]], writes=[Bvq])
                        S.op(PE, lambda e, vq=vq, j=j: e.matmul(P2[:, 0:n], lhsT=ones, rhs=vq[:, 0:n], start=(j == 0), stop=(j == NJ - 1)),
                             reads=[Bvq, B_cb], writes=[B2] if j == 0 else [], accw=[] if j == 0 else [B2])
                    ln_stats(P1, B1, P2, B2, n)
                    for j in range(NJ):
                        t1, Bt1 = fscr.get()
                        S.op(DVE, lambda e, t1=t1, j=j: e.tensor_tensor(out=t1[:, 0:n], in0=z[:, j, c0:c1], in1=st_mean[:, 0:n], op=ALU.subtract),
                             reads=[Bz[(j, tg)], B_mean], writes=[Bt1])
                        t2, Bt2 = fscr.get()
                        S.op(DVE, lambda e, t1=t1, t2=t2, j=j: e.scalar_tensor_tensor(out=t2[:, 0:n], in0=t1[:, 0:n], scalar=cfv('clng', j), in1=st_rstd[:, 0:n], op0=ALU.mult, op1=ALU.mult),
                             reads=[Bt1, B_rstd, B_cf], writes=[Bt2])
                        S.op(ACT, lambda e, t2=t2, j=j: e.activation(out=hbcol(j, c0, c1), in_=t2[:, 0:n], func=AF.Silu, bias=cfv('clnb', j)),
                             reads=[Bt2, B_cf], writes=[HB[tg]] if j == 0 else [], accw=[] if j == 0 else [HB[tg]])
                S.barrier()
            if 'a' in dbg:
                dump("aE", hbE[:].rearrange("p j t -> p (j t)"), list(HB.values()), [128, NJ * TE], BF16)
                dump("aO", hbO[:].rearrange("p j t -> p (j t)"), list(HB.values()), [128, NJ * TO], BF16)

            stop('stopC')
            proj_residual((G0, G1, G2), wout_d, 'bout', hcol, hbcol)
            prefetch2(moe_loader(0))
            layernorm((G0, G1, G2), hcol, hbcol, 'mixg0', 'mixb0')
            if 'h1' in dbg:
                S.barrier()
                dump("h1E", hE[:].rearrange("p j t -> p (j t)"), [], [128, NJ * TE])
                dump("h1O", hO[:].rearrange("p j t -> p (j t)"), [], [128, NJ * TO])

            stop('stopD')
            if 'stop1' not in dbg:
                with ExitStack() as PE_:
                    moe(0, (G0, G1, G2), TT_E + TT_O, hcol, hbcol, PE_)
                    S.barrier()
                    if 'pre2' in dbg:
                        dump("pre2E", hE[:].rearrange("p j t -> p (j t)"), [], [128, NJ * TE])
                        dump("pre2O", hO[:].rearrange("p j t -> p (j t)"), [], [128, NJ * TO])
                layernorm((G0, G1, G2), hcol, hbcol, 'ffng0', 'ffnb0')
            S.barrier()
            if 'h2' in dbg:
                dump("h2E", hE[:].rearrange("p j t -> p (j t)"), [], [128, NJ * TE])
                dump("h2O", hO[:].rearrange("p j t -> p (j t)"), [], [128, NJ * TO])

        def hcol(j, c0, c1):
            return hO[:, j, c0 - TE:c1 - TE]

        def hbcol(j, c0, c1):
            return hbE[:, j, c0:c1] if c1 <= TE else hbO[:, j, c0 - TE:c1 - TE]

        if 'stop2' not in dbg:
            with ExitStack() as L1:
                KT = sb(L1, "KT", [128, 2, T], BF16)
                B_KT = Buf("KT")
                V = sb(L1, "V", [128, 10, 256], BF16)
                B_V = Buf("V")
                with ExitStack() as LQ:
                    QT = sb(LQ, "QT", [128, NJ, TO], BF16)
                    B_QT = {g: Buf(f"QT{g[0]}") for g in (G1, G2)}
                    with ExitStack() as LCS:
                        cs = sb(LCS, "cs_sb", [128, 2, T])
                        B_cs = Buf("cs")
                        S.dma(SP, cs[:].rearrange("p a t -> p (a t)"), cs_d[:, :], B_cs, writes=[B_cs])
                        pr = prot([0, 1, 2, 3])
                        pr2 = prot([4, 5])

                        def rope_evac(P, Bp, n, c0, c1, bias_ap, dst_ap, dstB, acc=True):
                            raw, Braw = fscr.get()
                            S.op(ACT, lambda e: e.activation(out=raw[:, 0:n], in_=P[:, 0:n], func=AF.Identity, bias=bias_ap),
                                 reads=[Bp, B_cf], writes=[Braw])
                            P2, Bp2 = pr2.get()
                            S.op(PE, lambda e: e.matmul(P2[:, 0:n], lhsT=pswap, rhs=raw[:, 0:n], start=True, stop=True),
                                 reads=[Braw, B_cf], writes=[Bp2])
                            t1, Bt1 = fscr.get()
                            S.op(DVE, lambda e: e.tensor_tensor(out=t1[:, 0:n], in0=raw[:, 0:n], in1=cs[:, 0, c0:c1], op=ALU.mult),
                                 reads=[Braw, B_cs], writes=[Bt1])
                            t2, Bt2 = fscr.get()
                            S.op(DVE, lambda e: e.tensor_tensor(out=t2[:, 0:n], in0=P2[:, 0:n], in1=cs[:, 1, c0:c1], op=ALU.mult),
                                 reads=[Bp2, B_cs], writes=[Bt2])
                            S.op(DVE, lambda e: e.tensor_tensor(out=dst_ap, in0=t1[:, 0:n], in1=t2[:, 0:n], op=ALU.add),
                                 reads=[Bt1, Bt2], accw=[dstB])

                        units = [0, 1]
                        load = stream_weights(units, lambda g: wk_d[g], 2048)
                        load(0)
                        for gp in units:
                            load(gp + 1)
                            w = wsl[gp % 2][:, 0:2048].rearrange("p (k m) -> p k m", k=NJ)
                            for (c0, c1) in [(0, 16), (48, 176), G1, G2]:
                                n = c1 - c0
                                P, Bp = pr.get()
                                S.group(PE, [lambda e, kc=kc: e.matmul(P[:, 0:n], lhsT=w[:, kc, :], rhs=hbcol(kc, c0, c1), start=(kc == 0), stop=(kc == NJ - 1)) for kc in range(NJ)],
                                        reads=[wslB[gp % 2], HB[tg_of(c0)]], writes=[Bp])
                                a, b = CF['bk']
                                rope_evac(P, Bp, n, c0, c1, cf[:, a + gp:a + gp + 1], KT[:, gp, c0:c1], B_KT)
                        S.dma(POOL, wsl[0][:, 0:4096], wv_d[:, :], wslB[0], writes=[wslB[0]])
                        wv = wsl[0][:, 0:4096].rearrange("p (k m) -> p k m", k=NJ)
                        vts = [(0, 16)] + [(48 + 128 * m, 48 + 128 * (m + 1)) for m in range(9)]
                        for vi, (c0, c1) in enumerate(vts):
                            n = c1 - c0
                            P, Bp = pr.get()
                            S.group(PE, [lambda e, kc=kc: e.matmul(P[0:n, 0:256], lhsT=hbcol(kc, c0, c1), rhs=wv[:, kc, :], start=(kc == 0), stop=(kc == NJ - 1)) for kc in range(NJ)],
                                    reads=[wslB[0], HB[tg_of(c0)]], writes=[Bp])
                            S.op(DVE, lambda e: e.tensor_tensor(out=V[0:n, vi, :], in0=P[0:n, 0:256], in1=cfv('bvb')[0:n, :], op=ALU.add),
                                 reads=[Bp, B_cf], accw=[B_V])
                        units = list(range(NJ))
                        load = stream_weights(units, lambda j: wq_d[j], 2048)
                        load(0)
                        for j in units:
                            load(j + 1)
                            w = wsl[j % 2][:, 0:2048].rearrange("p (k m) -> p k m", k=NJ)
                            for tg in (G1, G2):
                                c0, c1 = tg
                                n = c1 - c0
                                P, Bp = pr.get()
                                S.group(PE, [lambda e, kc=kc: e.matmul(P[:, 0:n], lhsT=w[:, kc, :], rhs=hbcol(kc, c0, c1), start=(kc == 0), stop=(kc == NJ - 1)) for kc in range(NJ)],
                                        reads=[wslB[j % 2], HB[tg]], writes=[Bp])
                                rope_evac(P, Bp, n, c0, c1, cfv('bq', j), QT[:, j, c0 - TE:c1 - TE], B_QT[tg])
                        S.barrier()
                    if 'qkv' in dbg:
                        dump("KT", KT[:].rearrange("p g t -> p (g t)"), [], [128, 2 * T], BF16)
                        dump("V", V[:].rearrange("p g t -> p (g t)"), [], [128, 10 * 256], BF16)
                        dump("QT", QT[:].rearrange("p g t -> p (g t)"), [], [128, NJ * TO], BF16)

                    prefetch2(proj_loader(wo_d, ('proj', 'bo')))
                    Eo = Rot([(sb(LQ, f"Eo{i}", [128, 512], BF16), Buf(f"Eo{i}")) for i in range(3)])
                    Ep = Rot([(sb(LQ, f"Ep{i}", [128, 512], BF16), Buf(f"Ep{i}")) for i in range(3)])
                    Em = Rot([(sb(LQ, f"Em{i}", [16, 512], BF16), Buf(f"Em{i}")) for i in range(3)])
                    pS = prot([0, 1, 2, 3, 4, 5])
                    pO = prot([6])
                    pDn = prot([7])
                    ascr = Rot([(sb(LQ, f"as{i}", [128, 512]), Buf(f"as{i}")) for i in range(6)])
                    its = [(gp, half, n_, quad) for gp in range(2) for half in range(2) for n_ in range(8) for quad in range(2)]
                    stS = {}
                    stE = {}

                    def geom(it):
                        gp, half, n_, quad = it
                        r0, r1 = half * 64, half * 64 + 64
                        own = (TE + 128 * n_, TE + 128 * n_ + 128)
                        prv = (48 + 128 * n_, 48 + 128 * n_ + 128)
                        qtg = G1 if n_ < 4 else G2
                        cj = gp * 8 + quad * 4
                        return gp, half, n_, quad, r0, r1, own, prv, qtg, cj

                    def stage_S(k):
                        gp, half, n_, quad, r0, r1, own, prv, qtg, cj = geom(its[k])
                        rhsQ = QT[r0:r1, cj:cj + 4, n_ * 128:(n_ + 1) * 128]
                        PSo, Bso = pS.get()
                        PSp, Bsp = pS.get()
                        PSm, Bsm = pS.get()
                        S.op(PE, lambda e: e.matmul(PSo[:, :], lhsT=KT[r0:r1, gp, own[0]:own[1]], rhs=rhsQ, start=True, stop=True),
                             reads=[B_KT, B_QT[qtg]], writes=[Bso])
                        S.op(PE, lambda e: e.matmul(PSp[:, :], lhsT=KT[r0:r1, gp, prv[0]:prv[1]], rhs=rhsQ, start=True, stop=True),
                             reads=[B_KT, B_QT[qtg]], writes=[Bsp])
                        S.op(PE, lambda e: e.matmul(PSm[0:16, :], lhsT=KT[r0:r1, gp, 0:16], rhs=rhsQ, start=True, stop=True),
                             reads=[B_KT, B_QT[qtg]], writes=[Bsm])
                        stS[k] = (PSo, Bso, PSp, Bsp, PSm, Bsm)

                    def stage_X(k):
                        gp, half, n_, quad, r0, r1, own, prv, qtg, cj = geom(its[k])
                        PSo, Bso, PSp, Bsp, PSm, Bsm = stS.pop(k)
                        eo, Beo = Eo.get()
                        ep, Bep = Ep.get()
                        em, Bem = Em.get()
                        S.op(ACT, lambda e: e.activation(out=eo[:, :], in_=PSo[:, :], func=AF.Exp, scale=0.125), reads=[Bso], writes=[Beo])
                        S.op(ACT, lambda e: e.activation(out=ep[:, :], in_=PSp[:, :], func=AF.Exp, scale=0.125), reads=[Bsp], writes=[Bep])
                        S.op(ACT, lambda e: e.activation(out=em[:, :], in_=PSm[0:16, :], func=AF.Exp, scale=0.125), reads=[Bsm], writes=[Bem])
                        eo3 = eo[:, :].rearrange("p (a b) -> p a b", a=4)
                        ep3 = ep[:, :].rearrange("p (a b) -> p a b", a=4)
                        S.op(DVE, lambda e: e.tensor_tensor(out=eo3, in0=eo3, in1=cbv('m_own').unsqueeze(1).broadcast_to([128, 4, 128]), op=ALU.mult),
                             reads=[B_cb], writes=[Beo])
                        mp = cbv('m_prev0') if n_ == 0 else cbv('m_prev')
                        S.op(DVE, lambda e: e.tensor_tensor(out=ep3, in0=ep3, in1=mp.unsqueeze(1).broadcast_to([128, 4, 128]), op=ALU.mult),
                             reads=[B_cb], writes=[Bep])
                        stE[k] = (eo, Beo, ep, Bep, em, Bem)

                    def stage_V(k):
                        gp, half, n_, quad, r0, r1, own, prv, qtg, cj = geom(its[k])
                        eo, Beo, ep, Bep, em, Bem = stE.pop(k)
                        PO, Bpo = pO.get()
                        PDn, Bpdn = pDn.get()
                        vo, vp = n_ + 2, n_ + 1
                        S.group(PE, [
                            lambda e: e.matmul(PO[:, :], lhsT=V[:, vo, gp * 128:(gp + 1) * 128], rhs=eo[:, :], start=True, stop=False),
                            lambda e: e.matmul(PO[:, :], lhsT=V[:, vp, gp * 128:(gp + 1) * 128], rhs=ep[:, :], start=False, stop=False),
                            lambda e: e.matmul(PO[:, :], lhsT=V[0:16, 0, gp * 128:(gp + 1) * 128], rhs=em[:, :], start=False, stop=True),
                        ], reads=[B_V, Beo, Bep, Bem], writes=[Bpo])
                        S.group(PE, [
                            lambda e: e.matmul(PDn[:, :], lhsT=ones, rhs=eo[:, :], start=True, stop=False),
                            lambda e: e.matmul(PDn[:, :], lhsT=ones, rhs=ep[:, :], start=False, stop=False),
                            lambda e: e.matmul(PDn[:, :], lhsT=ones[0:16, :], rhs=em[:, :], start=False, stop=True),
                        ], reads=[B_cb, Beo, Bep, Bem], writes=[Bpdn])
                        d1, Bd1 = ascr.get()
                        d13 = d1[r0:r1, :].rearrange("p (a b) -> p a b", a=4)
                        S.op(DVE, lambda e: e.tensor_tensor(out=d13, in0=PDn[r0:r1, :].rearrange("p (a b) -> p a b", a=4),
                                                            in1=esnk[r0:r1, cj:cj + 4].unsqueeze(2).broadcast_to([64, 4, 128]), op=ALU.add),
                             reads=[Bpdn, B_esnk], writes=[Bd1])
                        po, Bpos = ascr.get()
                        S.op(ACT, lambda e: e.copy(po[r0:r1, :], PO[r0:r1, :]), reads=[Bpo], writes=[Bpos])
                        S.op(ACT, lambda e: e.activation(out=d1[r0:r1, :], in_=d1[r0:r1, :], func=AF.Ln), reads=[Bd1], writes=[Bd1])
                        S.op(ACT, lambda e: e.activation(out=d1[r0:r1, :], in_=d1[r0:r1, :], func=AF.Exp, scale=-1.0), reads=[Bd1], writes=[Bd1])
                        S.op(DVE, lambda e: e.tensor_tensor(out=hbO[r0:r1, cj:cj + 4, n_ * 128:(n_ + 1) * 128],
                                                            in0=po[r0:r1, :].rearrange("p (a b) -> p a b", a=4),
                                                            in1=d1[r0:r1, :].rearrange("p (a b) -> p a b", a=4), op=ALU.mult),
                             reads=[Bpos, Bd1], accw=[HB[qtg]])

                    stage_S(0)
                    stage_X(0)
                    stage_S(1)
                    stage_X(1)
                    for k in range(len(its)):
                        if k + 2 < len(its):
                            stage_S(k + 2)
                            stage_X(k + 2)
                        stage_V(k)
                    S.barrier()
            if 'att' in dbg:
                dump("attT", hbO[:].rearrange("p j t -> p (j t)"), [], [128, NJ * TO], BF16)
            proj_residual((G1, G2), wo_d, 'bo', hcol, hbcol)
            prefetch2(moe_loader(1))
            layernorm((G1, G2), hcol, hbcol, 'mixg1', 'mixb1')
            if 'h3' in dbg:
                S.barrier()
                dump("h3O", hO[:].rearrange("p j t -> p (j t)"), [], [128, NJ * TO])
            if 'stop3' not in dbg:
                with ExitStack() as PM_:
                    moe(1, (G1, G2), TT_O, hcol, hbcol, PM_)
                    S.barrier()
                layernorm((G1, G2), hcol, hbcol, 'ffng1', 'ffnb1', write_hb=False)
            S.barrier()

        if not _CACHE_OUT_DONE:
            with ExitStack() as LO:
                ost = [sb(LO, f"ost{i}", [128, D]) for i in range(2)]
                Bost = [Buf("ost0"), Buf("ost1")]
                pr = prot([0, 1, 2, 3])
                for k in range(8):
                    o = ost[k % 2]
                    tg = G1 if k < 4 else G2
                    for jq in range(4):
                        P, Bp = pr.get()
                        S.group(PE, [lambda e, i=i: e.transpose(P[:, i * 128:(i + 1) * 128], hO[:, jq * 4 + i, k * 128:(k + 1) * 128], ident) for i in range(4)],
                                reads=[B_cf] + [H[(jq * 4 + i, tg)] for i in range(4)], writes=[Bp])
                        eng = ACT if jq % 2 == 0 else DVE
                        if eng is ACT:
                            S.op(ACT, lambda e: e.copy(o[:, jq * 512:(jq + 1) * 512], P[:, :]), reads=[Bp], writes=[Bost[k % 2]] if jq == 0 else [], accw=[] if jq == 0 else [Bost[k % 2]])
                        else:
                            S.op(DVE, lambda e: e.tensor_copy(out=o[:, jq * 512:(jq + 1) * 512], in_=P[:, :]), reads=[Bp], accw=[Bost[k % 2]])
                    S.dma(SP, out_d[k * 128:(k + 1) * 128, :], o[:, :], Bost[k % 2], reads=[Bost[k % 2]])
                S.wait_all(SP, Bost)
                S.barrier()

    return nc, dbg_d


def _fm_vec(v):
    return np.ascontiguousarray(np.asarray(v, np.float32).reshape(NJ, 128).T)


def _head_perm():
    idx = []
    for jp in range(NJ):
        gp, i = jp // 8, jp % 8
        for hd in (16 * gp + i, 16 * gp + 8 + i):
            idx.extend(range(hd * 64, hd * 64 + 64))
    return np.array(idx)


def _w_fm(w, ncols_chunk):
    K, C = w.shape
    nch = C // ncols_chunk
    a = w.reshape(NJ, 128, nch, ncols_chunk).transpose(2, 1, 0, 3)
    return np.ascontiguousarray(a).reshape(nch, 128, NJ * ncols_chunk)


def prepare(inputs):
    f = lambda k: np.asarray(inputs[k], np.float32)
    x = f('x')[0]
    meta = f('meta_tokens')
    h0 = np.concatenate([meta, x], 0)
    shared = {}
    w_in = f('conv_w_in')[0]
    wv_ = w_in[:, :D].reshape(NJ, 128, NJ, 128)
    wg_ = w_in[:, D:].reshape(NJ, 128, NJ, 128)
    win = np.stack([wv_, wg_], 3)
    shared['win'] = np.ascontiguousarray(win.transpose(2, 1, 0, 3, 4)).reshape(NJ, 128, 4096)
    shared['wout'] = _w_fm(f('conv_w_out')[0], 128)
    w1 = f('expert_w1')
    w3 = f('expert_w3')
    a1 = w1.reshape(2, 32, NJ, 128, 2, 128)
    a3 = w3.reshape(2, 32, NJ, 128, 2, 128)
    w13 = np.stack([a1, a3], 5)
    shared['w13'] = np.ascontiguousarray(w13.transpose(0, 1, 4, 3, 2, 5, 6)).reshape(2, 32, 2, 128, 4096)
    del w13, a1, a3
    w2 = f('expert_w2').reshape(2, 32, 2, 128, D)
    shared['w2'] = np.ascontiguousarray(w2.transpose(0, 1, 3, 2, 4)).reshape(2, 32, 128, 4096)
    wr = np.concatenate([f('router_group_w'), f('router_expert_w')], -1)
    shared['wr'] = np.ascontiguousarray(wr.reshape(2, NJ, 128, 36).transpose(0, 2, 1, 3)).reshape(2, 128, NJ * 36)
    shared['wk'] = _w_fm(f('w_k'), 128)
    shared['wv'] = _w_fm(f('w_v'), 256)[0]
    perm = _head_perm()
    shared['wq'] = _w_fm(f('w_q')[0][:, perm], 128)
    shared['wo'] = _w_fm(f('w_o')[0][perm, :], 128)
    cfp = np.zeros((128, NCF), np.float32)

    def put(name, arr):
        a, b = CF[name]
        cfp[:, a:b] = arr
    put('ident', np.eye(128, dtype=np.float32))
    put('onesf', np.ones((128, 128), np.float32))
    ps = np.zeros((128, 128), np.float32)
    for m in range(128):
        d = m % 64
        if d < 8:
            ps[m + 8, m] = 1.0
        elif d < 16:
            ps[m - 8, m] = 1.0
    put('pswap', ps)
    b_in = f('conv_b_in')[0]
    put('b1', _fm_vec(b_in[:D]))
    put('b2', _fm_vec(b_in[D:]))
    put('bdw', _fm_vec(f('conv_b_dw')[0]))
    put('clng', _fm_vec(f('conv_ln_g')[0]))
    put('clnb', _fm_vec(f('conv_ln_b')[0]))
    put('bout', _fm_vec(f('conv_b_out')[0]))
    for l in range(2):
        put('mixg%d' % l, _fm_vec(f('ln_mix_g')[l]))
        put('mixb%d' % l, _fm_vec(f('ln_mix_b')[l]))
        put('ffng%d' % l, _fm_vec(f('ln_ffn_g')[l]))
        put('ffnb%d' % l, _fm_vec(f('ln_ffn_b')[l]))
    put('bq', _fm_vec(f('b_q')[0][perm]))
    put('bo', _fm_vec(f('b_o')[0]))
    put('snk', _fm_vec(np.repeat(f('sinks')[0], 64)[perm]))
    put('bk', np.ascontiguousarray(f('b_k').reshape(2, 128).T))
    wdw = f('conv_w_dw')[0]
    put('wdw', np.ascontiguousarray(wdw.reshape(31, NJ, 128).transpose(2, 1, 0)).reshape(128, NJ * 31))
    for l in range(2):
        br = np.concatenate([f('router_group_b')[l], f('router_expert_b')[l]])
        put('brb%d' % l, np.broadcast_to(br[None, :], (128, 36)))
    put('bvb', np.broadcast_to(f('b_v')[None, :], (128, 256)))
    kk = np.arange(128)[:, None]
    qq = np.arange(128)[None, :]
    m_own = (kk <= qq).astype(np.float32)
    m_prev = (kk > qq).astype(np.float32)
    sel = np.zeros((64, 32, 128), np.float32)
    for e in range(32):
        sel[e, e, :] = 1.0
        sel[32 + e, e, :] = 1.0
    shared['sel'] = sel.reshape(64, 32 * 128)
    inv_freq = (np.float32(500000.0) ** (-np.arange(0, 16, 2, dtype=np.float32) / np.float32(16))).astype(np.float32)
    in_maps = []
    for c in range(NCORES):
        own0 = 16 + 1024 * c
        pos = np.concatenate([np.arange(16), np.arange(own0 - 160, own0 + 1024)])
        valid = pos >= 0
        xe = np.zeros((T, D), np.float32)
        xe[valid] = h0[pos[valid]]
        cfc = cfp.copy()
        a, b = CF['padmask']
        cfc[:, a:b] = valid[16:176].astype(np.float32)[None, :]
        cbc = np.zeros((128, NCB), np.float32)
        cbc[:, 0:128] = 1.0
        cbc[:, 128:256] = np.eye(128, dtype=np.float32)
        cbc[:, 256:384] = m_own
        cbc[:, 384:512] = m_prev
        cbc[:, 512:640] = m_prev if c > 0 else 0.0
        ang = np.clip(pos, 0, None).astype(np.float32)[:, None] * inv_freq[None, :]
        cosv = np.cos(ang).astype(np.float32)
        sinv = np.sin(ang).astype(np.float32)
        cst = np.zeros((128, 2, T), np.float32)
        cst[:, 0, :] = 1.0
        for p in range(128):
            d = p % 64
            if d < 16:
                cst[p, 0, :] = cosv[:, d % 8]
                cst[p, 1, :] = -sinv[:, d % 8] if d < 8 else sinv[:, d % 8]
        m = dict(shared)
        m['xe'] = xe
        m['cf'] = cfc
        m['cb'] = cbc
        m['cs'] = cst.reshape(128, 2 * T)
        in_maps.append(m)
    return in_maps


_CACHE = {}


def kernel(**inputs):
    in_maps = prepare(inputs)
    if 'nc' not in _CACHE:
        _CACHE['nc'] = build()[0]
    nc = _CACHE['nc']
    res = run_bass_kernel_spmd(nc, in_maps, core_ids=list(range(NCORES)))
    out = np.concatenate([np.asarray(r["out"], np.float32) for r in res.results], 0)
    return out.reshape(1, NCORES * TO, D)
```

```python
import numpy as np
from contextlib import ExitStack
import concourse.bass as bass
import concourse.mybir as mybir
from concourse.bass_utils import run_bass_kernel_spmd

F32 = mybir.dt.float32
BF16 = mybir.dt.bfloat16
AF = mybir.ActivationFunctionType
ALU = mybir.AluOpType
AX = mybir.AxisListType

NCORES = 8
D = 2048
NJ = 16
T = 1200
TE = 176
TO = 1024
ALPHA = float((2.0 * 2) ** 0.25)
EPS = 1e-5
G0, G1, G2 = (0, 176), (176, 688), (688, 1200)
TT_E = [(0, 128), (128, 176)]
TT_O = [(176 + 128 * k, 176 + 128 * (k + 1)) for k in range(8)]
BIG = 1.0e9


class Sem:
    _n = 0

    def __init__(self, h):
        self.h = h
        self.id = Sem._n
        Sem._n += 1


class Buf:
    def __init__(self, name):
        self.name = name
        self.w = {}
        self.wf = {}
        self.rd = {}
        self.dsem = None
        self.dcnt = 0
        self.psum = name.startswith('ps')


class Eng:
    def __init__(self, name, h, sem):
        self.name = name
        self.h = h
        self.sem = sem
        self.cnt = 0
        self.seen = {}


class Sched:
    def __init__(self, nc, es):
        self.nc = nc
        self.es = es
        mk = lambda n: Sem(es.enter_context(nc.semaphore(n)))
        self.pe = Eng('pe', nc.tensor, mk('s_pe'))
        self.act = Eng('act', nc.scalar, mk('s_act'))
        self.dve = Eng('dve', nc.vector, mk('s_dve'))
        self.pool = Eng('pool', nc.gpsimd, mk('s_pool'))
        self.sp = Eng('sp', nc.sync, mk('s_sp'))
        self.engs = [self.pe, self.act, self.dve, self.pool, self.sp]
        self.dsems = []

    def _deps(self, reads, writes, accw):
        deps = []
        for b in reads:
            deps.extend(b.w.values())
            if b.psum:
                deps.extend(b.rd.values())
        for b in writes:
            deps.extend(b.w.values())
            deps.extend(b.rd.values())
        for b in accw:
            deps.extend(b.rd.values())
            deps.extend(b.wf.values())
        return deps

    def _wait(self, eng, deps):
        best = {}
        for (s, v) in deps:
            if best.get(s.id, (None, 0))[1] < v:
                best[s.id] = (s, v)
        for sid, (s, v) in best.items():
            if eng is self.pe and s is self.pe.sem:
                continue
            if eng.seen.get(sid, 0) < v:
                eng.h.wait_ge(s.h, v)
                eng.seen[sid] = v

    def _mark(self, tag, reads, writes, accw):
        for b in reads:
            b.rd[tag[0].id] = tag
        for b in writes:
            b.w = {tag[0].id: tag}
            b.wf = {tag[0].id: tag}
        for b in accw:
            b.w[tag[0].id] = tag

    def op(self, eng, fn, reads=(), writes=(), accw=()):
        self._wait(eng, self._deps(reads, writes, accw))
        ins = fn(eng.h)
        ins.then_inc(eng.sem.h, 1)
        eng.cnt += 1
        self._mark((eng.sem, eng.cnt), reads, writes, accw)

    def group(self, eng, fns, reads=(), writes=(), accw=()):
        self._wait(eng, self._deps(reads, writes, accw))
        ins = None
        for fn in fns:
            ins = fn(eng.h)
        ins.then_inc(eng.sem.h, 1)
        eng.cnt += 1
        self._mark((eng.sem, eng.cnt), reads, writes, accw)

    def dma(self, eng, out, in_, owner, reads=(), writes=(), accw=()):
        if owner.dsem is None:
            owner.dsem = Sem(self.es.enter_context(self.nc.semaphore('d_' + owner.name)))
            self.dsems.append(owner)
        self._wait(eng, self._deps(reads, writes, accw))
        eng.h.dma_start(out=out, in_=in_).then_inc(owner.dsem.h, 16)
        owner.dcnt += 16
        self._mark((owner.dsem, owner.dcnt), reads, writes, accw)

    def barrier(self):
        for e in self.engs:
            for e2 in self.engs:
                if e2 is e or e2.cnt == 0:
                    continue
                if e.seen.get(e2.sem.id, 0) < e2.cnt:
                    e.h.wait_ge(e2.sem.h, e2.cnt)
                    e.seen[e2.sem.id] = e2.cnt
            for o in self.dsems:
                if o.dcnt and e.seen.get(o.dsem.id, 0) < o.dcnt:
                    e.h.wait_ge(o.dsem.h, o.dcnt)
                    e.seen[o.dsem.id] = o.dcnt

    def wait_all(self, eng, bufs):
        deps = []
        for b in bufs:
            deps.extend(b.w.values())
            deps.extend(b.rd.values())
        self._wait(eng, deps)


class Rot:
    def __init__(self, items):
        self.items = items
        self.i = 0

    def get(self):
        it = self.items[self.i % len(self.items)]
        self.i += 1
        return it


VEC_NAMES = ['b1', 'b2', 'bdw', 'clng', 'clnb', 'bout', 'mixg0', 'mixb0', 'ffng0', 'ffnb0',
             'mixg1', 'mixb1', 'ffng1', 'ffnb1', 'bq', 'bo', 'snk']
CF = {}
_o = 0
for _n, _w in [('ident', 128), ('pswap', 128), ('onesf', 128)] + [(v, 16) for v in VEC_NAMES] + \
        [('bk', 2), ('wdw', 16 * 31), ('padmask', 160), ('brb0', 36), ('brb1', 36), ('bvb', 256)]:
    CF[_n] = (_o, _o + _w)
    _o += _w
NCF = _o
CB = {'ones': (0, 128), 'identb': (128, 256), 'm_own': (256, 384), 'm_prev': (384, 512),
      'm_prev0': (512, 640)}
NCB = 640


class _Stop(Exception):
    pass


def build(dbg=()):
    dbg = set(dbg)
    try:
        return _build(dbg)
    except _Stop as st:
        return st.args


def _build(dbg):
    _CACHE_OUT_DONE = []
    nc = bass.Bass("TRN2", target_bir_lowering=False)

    def din(name, shape):
        return nc.dram_tensor(name, list(shape), F32, kind="ExternalInput").ap()

    xe_d = din("xe", [T, D])
    cf_d = din("cf", [128, NCF])
    cb_d = din("cb", [128, NCB])
    cs_d = din("cs", [128, 2 * T])
    sel_d = din("sel", [64, 32 * 128])
    win_d = din("win", [NJ, 128, 4096])
    wout_d = din("wout", [NJ, 128, 2048])
    w13_d = din("w13", [2, 32, 2, 128, 4096])
    w2_d = din("w2", [2, 32, 128, 4096])
    wr_d = din("wr", [2, 128, 16 * 36])
    wk_d = din("wk", [2, 128, 2048])
    wv_d = din("wv", [128, 4096])
    wq_d = din("wq", [NJ, 128, 2048])
    wo_d = din("wo", [NJ, 128, 2048])
    out_d = nc.dram_tensor("out", [TO, D], F32, kind="ExternalOutput").ap()
    dbg_d = {}

    with ExitStack() as es:
        S = Sched(nc, es)
        PE, ACT, DVE, POOL, SP = S.pe, S.act, S.dve, S.pool, S.sp

        def sb(scope, name, shape, dt=F32):
            return scope.enter_context(nc.sbuf_tensor(name, list(shape), dt))

        hO = sb(es, "hO", [128, NJ, TO])
        hbO = sb(es, "hbO", [128, NJ, TO], BF16)
        hbE = sb(es, "hbE", [128, NJ, TE], BF16)
        wsl = [sb(es, f"wsl{i}", [128, 4096], BF16) for i in range(2)]
        wslB = [Buf(f"wsl{i}") for i in range(2)]
        cf = sb(es, "cf_sb", [128, NCF])
        cb = sb(es, "cb_sb", [128, NCB], BF16)
        st_mean = sb(es, "st_mean", [128, 512])
        st_rstd = sb(es, "st_rstd", [128, 512])
        B_mean, B_rstd = Buf("st_mean"), Buf("st_rstd")
        _st0 = (st_mean, B_mean, st_rstd, B_rstd)
        fscr = Rot([(sb(es, f"fs{i}", [128, 512]), Buf(f"fs{i}")) for i in range(5)])
        bscr = Rot([(sb(es, f"bs{i}", [128, 512], BF16), Buf(f"bs{i}")) for i in range(3)])
        esnk = sb(es, "esnk", [128, 16])
        B_esnk = Buf("esnk")
        B_cf, B_cb = Buf("cf"), Buf("cb")

        PS = [es.enter_context(nc.psum_tensor(f"ps{i}", [128, 512], F32)) for i in range(8)]
        PB = [Buf(f"ps{i}") for i in range(8)]

        def prot(idx):
            return Rot([(PS[i], PB[i]) for i in idx])

        HB = {G0: Buf("hb0"), G1: Buf("hb1"), G2: Buf("hb2")}
        H = {(j, g): Buf(f"h{j}_{g[0]}") for j in range(NJ) for g in (G0, G1, G2)}

        def tg_of(c0):
            return G0 if c0 < 176 else (G1 if c0 < 688 else G2)

        def cfv(name, j=None):
            a, b = CF[name]
            if j is None:
                return cf[:, a:b]
            return cf[:, a + j:a + j + 1]

        def cbv(name):
            a, b = CB[name]
            return cb[:, a:b]

        S.dma(SP, cf[:], cf_d[:, :], B_cf, writes=[B_cf])
        S.dma(POOL, cb[:], cb_d[:, :], B_cb, writes=[B_cb])
        S.op(ACT, lambda e: e.activation(out=esnk[:], in_=cfv('snk'), func=AF.Exp),
             reads=[B_cf], writes=[B_esnk])
        ident = cfv('ident')
        pswap = cfv('pswap')
        ones = cbv('ones')

        if 'stop0' in dbg:
            dump_esnk = True
        def dump(name, ap_sb, bufs, shape, dt=F32):
            d = nc.dram_tensor("o_" + name, list(shape), dt, kind="ExternalOutput").ap()
            dbg_d[name] = d
            ob = Buf("dbg_" + name)
            S.dma(SP, d, ap_sb, ob, reads=bufs)
            S.wait_all(SP, [ob])
            S._wait(SP, [(ob.dsem, ob.dcnt)])
            S.barrier()

        if 'stop0' in dbg:
            dump('esnk', esnk[:], [B_esnk], [128, 16])
            dump('cbd', cb[:], [B_cb], [128, NCB], BF16)
            S.barrier()
            raise _Stop(nc, dbg_d)
        _loaders = {}

        def stream_weights(units, src_fn, width, key=None):
            if key is not None and key in _loaders:
                return _loaders[key]
            issued = set()

            def load(i):
                if i in issued or i >= len(units):
                    return
                issued.add(i)
                k = i % 2
                S.dma(POOL, wsl[k][:, 0:width], src_fn(units[i]), wslB[k], writes=[wslB[k]])
            if key is not None:
                _loaders[key] = load
            return load

        def moe_loader(layer):
            units = [(e, fc) for e in range(32) for fc in range(2)]
            return stream_weights(units, lambda u: w13_d[layer, u[0], u[1]], 4096, key=('w13', layer))

        def proj_loader(wsrc, key):
            return stream_weights(list(range(NJ)), lambda j: wsrc[j], 2048, key=key)

        def prefetch2(load):
            load(0)
            load(1)

        def layernorm(tgs, hcol, hbcol, gname, bname, write_hb=True, after=None):
            with ExitStack() as sc:
                uid = len(S.dsems) * 1000 + S.act.cnt
                sts = [(st_mean, B_mean, st_rstd, B_rstd),
                       (sb(sc, f"lnm{uid}", [128, 512]), Buf("lnm"), sb(sc, f"lnr{uid}", [128, 512]), Buf("lnr"))]
                vsc = Rot([(sb(sc, f"lnv{uid}_{i}", [128, 512], BF16), Buf(f"lnv{i}")) for i in range(4)])
                p12 = prot([6, 7, 4, 5])

                def stats(ti):
                    tg = tgs[ti]
                    c0, c1 = tg
                    n = c1 - c0
                    P1, B1 = p12.get()
                    P2, B2 = p12.get()
                    for j in range(NJ):
                        if True:
                            vb, Bvb = vsc.get()
                            S.op(ACT, lambda e: e.activation(out=vb[:, 0:n], in_=hcol(j, c0, c1), func=AF.Copy),
                                 reads=[H[(j, tg)]], writes=[Bvb])
                            S.op(PE, lambda e: e.matmul(P1[:, 0:n], lhsT=ones, rhs=vb[:, 0:n], start=(j == 0), stop=(j == NJ - 1)),
                                 reads=[Bvb, B_cb], writes=[B1] if j == 0 else [], accw=[] if j == 0 else [B1])
                        else:
                            S.op(PE, lambda e: e.matmul(P1[:, 0:n], lhsT=cfv('onesf'), rhs=hcol(j, c0, c1), start=(j == 0), stop=(j == NJ - 1)),
                                 reads=[H[(j, tg)], B_cf], writes=[B1] if j == 0 else [], accw=[] if j == 0 else [B1])
                        vq, Bvq = vsc.get()
                        S.op(ACT, lambda e: e.activation(out=vq[:, 0:n], in_=hcol(j, c0, c1), func=AF.Square),
                             reads=[H[(j, tg)]], writes=[Bvq])
                        S.op(PE, lambda e: e.matmul(P2[:, 0:n], lhsT=ones, rhs=vq[:, 0:n], start=(j == 0), stop=(j == NJ - 1)),
                             reads=[Bvq, B_cb], writes=[B2] if j == 0 else [], accw=[] if j == 0 else [B2])
                    ln_stats(P1, B1, P2, B2, n, sts[ti % 2])

                nsc = Rot([(sb(sc, f"lns{uid}_{i}", [128, 512]), Buf(f"lns{i}")) for i in range(8)])

                def norm(ti):
                    tg = tgs[ti]
                    c0, c1 = tg
                    n = c1 - c0
                    mean, Bm, rstd, Br = sts[ti % 2]
                    for jb in range(0, NJ, 4):
                        js = list(range(jb, jb + 4))
                        t1s = {}
                        t2s = {}
                        for j in js:
                            t1s[j] = nsc.get()
                            t1, Bt1 = t1s[j]
                            S.op(DVE, lambda e: e.tensor_tensor(out=t1[:, 0:n], in0=hcol(j, c0, c1), in1=mean[:, 0:n], op=ALU.subtract),
                                 reads=[H[(j, tg)], Bm], writes=[Bt1])
                        for j in js:
                            t1, Bt1 = t1s[j]
                            t2s[j] = nsc.get()
                            t2, Bt2 = t2s[j]
                            S.op(DVE, lambda e: e.scalar_tensor_tensor(out=t2[:, 0:n], in0=t1[:, 0:n], scalar=cfv(gname, j), in1=rstd[:, 0:n], op0=ALU.mult, op1=ALU.mult),
                                 reads=[Bt1, Br, B_cf], writes=[Bt2])
                        for j in js:
                            t2, Bt2 = t2s[j]
                            S.op(ACT, lambda e: e.activation(out=hcol(j, c0, c1), in_=t2[:, 0:n], func=AF.Identity, bias=cfv(bname, j)),
                                 reads=[Bt2, B_cf], writes=[H[(j, tg)]])
                        if write_hb:
                            for j in js:
                                t2, Bt2 = t2s[j]
                                if j % 8 < 5:
                                    S.op(ACT, lambda e: e.activation(out=hbcol(j, c0, c1), in_=t2[:, 0:n], func=AF.Identity, bias=cfv(bname, j)),
                                         reads=[Bt2, B_cf], accw=[HB[tg]])
                                else:
                                    S.op(DVE, lambda e: e.tensor_scalar(out=hbcol(j, c0, c1), in0=t2[:, 0:n], scalar1=cfv(bname, j), scalar2=None, op0=ALU.add),
                                         reads=[Bt2, B_cf], accw=[HB[tg]])
                    if after is not None:
                        after(ti)

                stats(0)
                for ti in range(len(tgs)):
                    if ti + 1 < len(tgs):
                        stats(ti + 1)
                    norm(ti)
                S.barrier()

        def ln_stats(P1, B1, P2, B2, n, st=None):
            st_mean, B_mean, st_rstd, B_rstd = st if st is not None else _st0
            S.op(DVE, lambda e: e.tensor_scalar(out=st_mean[:, 0:n], in0=P1[:, 0:n], scalar1=1.0 / D, scalar2=None, op0=ALU.mult),
                 reads=[B1], writes=[B_mean])
            t1, Bt1 = fscr.get()
            S.op(DVE, lambda e: e.tensor_tensor(out=t1[:, 0:n], in0=st_mean[:, 0:n], in1=st_mean[:, 0:n], op=ALU.mult),
                 reads=[B_mean], writes=[Bt1])
            t2, Bt2 = fscr.get()
            S.op(DVE, lambda e: e.scalar_tensor_tensor(out=t2[:, 0:n], in0=P2[:, 0:n], scalar=1.0 / D, in1=t1[:, 0:n], op0=ALU.mult, op1=ALU.subtract),
                 reads=[B2, Bt1], writes=[Bt2])
            t3, Bt3 = fscr.get()
            S.op(ACT, lambda e: e.activation(out=t3[:, 0:n], in_=t2[:, 0:n], func=AF.Sqrt, bias=EPS, scale=1.0),
                 reads=[Bt2], writes=[Bt3])
            S.op(DVE, lambda e: e.reciprocal(out=st_rstd[:, 0:n], in_=t3[:, 0:n]),
                 reads=[Bt3], writes=[B_rstd])

        def proj_residual(tgs, wsrc, bias_name, hcol, hbcol):
            units = list(range(NJ))
            load = proj_loader(wsrc, ('proj', bias_name))
            pr = prot([0, 1, 2, 3])
            load(0)
            for j in units:
                load(j + 1)
                w = wsl[j % 2][:, 0:2048].rearrange("p (k m) -> p k m", k=NJ)
                for tg in tgs:
                    c0, c1 = tg
                    n = c1 - c0
                    P, Bp = pr.get()
                    S.group(PE, [lambda e, kc=kc: e.matmul(P[:, 0:n], lhsT=w[:, kc, :], rhs=hbcol(kc, c0, c1), start=(kc == 0), stop=(kc == NJ - 1)) for kc in range(NJ)],
                            reads=[wslB[j % 2], HB[tg]], writes=[Bp])
                    t, Bt = fscr.get()
                    S.op(ACT, lambda e, t=t, P=P: e.activation(out=t[:, 0:n], in_=P[:, 0:n], func=AF.Identity, bias=cfv(bias_name, j)),
                         reads=[Bp, B_cf], writes=[Bt])
                    S.op(DVE, lambda e, t=t: e.scalar_tensor_tensor(out=hcol(j, c0, c1), in0=hcol(j, c0, c1), scalar=ALPHA, in1=t[:, 0:n], op0=ALU.mult, op1=ALU.add),
                         reads=[Bt], writes=[H[(j, tg)]])

        def moe(layer, tgs, tts, hcol, hbcol, scope):
            wrt = sb(scope, f"wrt{layer}", [128, NJ, 36])
            B_wrt = Buf(f"wrt{layer}")
            sel = sb(scope, f"sel_sb{layer}", [64, 32, 128], BF16)
            B_sel = Buf(f"sel{layer}")
            gT = sb(scope, f"gT{layer}", [64, T], BF16)
            B_gT = {tg: Buf(f"gT{layer}_{tg[0]}") for tg in tgs}
            gball = sb(scope, f"gball{layer}", [128, T])
            B_gb = {tg: Buf(f"gb{layer}_{tg[0]}") for tg in tgs}
            hid = sb(scope, f"hid{layer}", [128, 2, 2, T], BF16)
            B_hid = [Buf(f"hid{layer}_0"), Buf(f"hid{layer}_1")]
            w2s = [sb(scope, f"w2s{layer}_{i}", [128, 2, D], BF16) for i in range(2)]
            B_w2 = [Buf(f"w2s{layer}_0"), Buf(f"w2s{layer}_1")]
            lg = sb(scope, f"lg{layer}", [128, 36])
            B_lg = Buf(f"lg{layer}")
            S.dma(SP, wrt[:].rearrange("p k m -> p (k m)"), wr_d[layer], B_wrt, writes=[B_wrt])
            S.dma(POOL, sel[:].rearrange("p e m -> p (e m)"), sel_d[:, :], B_sel, writes=[B_sel])
            brb = cfv('brb%d' % layer)
            units = [(e, fc) for e in range(32) for fc in range(2)]
            load = moe_loader(layer)
            load(0)
            NT = len(tts)
            pr = prot([6, 7])

            def wt(name, shape, dt=F32):
                return sb(scope, f"g{layer}_{name}", shape, dt), Buf(f"g{layer}_{name}")
            lgall, B_lga = wt("lgall", [128, NT, 36])
            S.op(POOL, lambda e: e.memset(lgall[:].rearrange("p t m -> p (t m)"), 0.0), writes=[B_lga])
            for ti, (c0, c1) in enumerate(tts):
                n = c1 - c0
                tg = tg_of(c0)
                P, Bp = pr.get()
                S.group(PE, [lambda e, kc=kc: e.matmul(P[0:n, 0:36], lhsT=hcol(kc, c0, c1), rhs=wrt[:, kc, :], start=(kc == 0), stop=(kc == NJ - 1)) for kc in range(NJ)],
                        reads=[B_wrt] + [H[(kc, tg)] for kc in range(NJ)], writes=[Bp])
                S.op(DVE, lambda e: e.tensor_tensor(out=lgall[0:n, ti, :], in0=P[0:n, 0:36], in1=brb[0:n, :], op=ALU.add),
                     reads=[Bp, B_cf], accw=[B_lga])
            lg4 = lgall[:, :, 0:4]
            le = lgall[:, :, 4:36]
            gmax, Bgmax = wt("gmax", [128, NT])
            S.op(DVE, lambda e: e.tensor_reduce(out=gmax[:], in_=lg4, axis=AX.X, op=ALU.max), reads=[B_lga], writes=[Bgmax])
            gd, Bgd = wt("gd", [128, NT, 4])
            S.op(DVE, lambda e: e.tensor_tensor(out=gd[:], in0=lg4, in1=gmax[:].unsqueeze(2).broadcast_to([128, NT, 4]), op=ALU.subtract),
                 reads=[B_lga, Bgmax], writes=[Bgd])
            gexp, Bgexp = wt("gexp", [128, NT, 4])
            S.op(ACT, lambda e: e.activation(out=gexp[:], in_=gd[:], func=AF.Exp), reads=[Bgd], writes=[Bgexp])
            gsum, Bgsum = wt("gsum", [128, NT])
            S.op(DVE, lambda e: e.tensor_reduce(out=gsum[:], in_=gexp[:], axis=AX.X, op=ALU.add), reads=[Bgexp], writes=[Bgsum])
            gw, Bgw = wt("gw", [128, NT])
            S.op(DVE, lambda e: e.reciprocal(out=gw[:], in_=gsum[:]), reads=[Bgsum], writes=[Bgw])
            pen, Bpen = wt("pen", [128, NT, 4])
            S.op(DVE, lambda e: e.tensor_scalar(out=pen[:], in0=gd[:], scalar1=0.0, scalar2=None, op0=ALU.is_equal), reads=[Bgd], writes=[Bpen])
            S.op(DVE, lambda e: e.tensor_scalar(out=pen[:], in0=pen[:], scalar1=BIG, scalar2=-BIG, op0=ALU.mult, op1=ALU.add), reads=[Bpen], writes=[Bpen])
            lem, Blem = wt("lem", [128, NT, 32])
            S.op(DVE, lambda e: e.tensor_tensor(out=lem[:].rearrange("p t (g k) -> p t g k", g=4), in0=le.rearrange("p t (g k) -> p t g k", g=4),
                                                in1=pen[:].unsqueeze(3).broadcast_to([128, NT, 4, 8]), op=ALU.add),
                 reads=[B_lga, Bpen], writes=[Blem])
            m1, Bm1 = wt("m1", [128, NT])
            S.op(DVE, lambda e: e.tensor_reduce(out=m1[:], in_=lem[:], axis=AX.X, op=ALU.max), reads=[Blem], writes=[Bm1])
            eq1, Beq1 = wt("eq1", [128, NT, 32])
            S.op(DVE, lambda e: e.tensor_tensor(out=eq1[:], in0=lem[:], in1=m1[:].unsqueeze(2).broadcast_to([128, NT, 32]), op=ALU.is_equal),
                 reads=[Blem, Bm1], writes=[Beq1])
            lem2, Blem2 = wt("lem2", [128, NT, 32])
            S.op(DVE, lambda e: e.scalar_tensor_tensor(out=lem2[:], in0=eq1[:], scalar=-BIG, in1=lem[:], op0=ALU.mult, op1=ALU.add),
                 reads=[Beq1, Blem], writes=[Blem2])
            m2, Bm2 = wt("m2", [128, NT])
            S.op(DVE, lambda e: e.tensor_reduce(out=m2[:], in_=lem2[:], axis=AX.X, op=ALU.max), reads=[Blem2], writes=[Bm2])
            eq2, Beq2 = wt("eq2", [128, NT, 32])
            S.op(DVE, lambda e: e.tensor_tensor(out=eq2[:], in0=lem2[:], in1=m2[:].unsqueeze(2).broadcast_to([128, NT, 32]), op=ALU.is_equal),
                 reads=[Blem2, Bm2], writes=[Beq2])
            dm, Bdm = wt("dm", [128, NT])
            S.op(DVE, lambda e: e.tensor_tensor(out=dm[:], in0=m1[:], in1=m2[:], op=ALU.subtract), reads=[Bm1, Bm2], writes=[Bdm])
            sg, Bsg = wt("sg", [128, NT])
            S.op(ACT, lambda e: e.activation(out=sg[:], in_=dm[:], func=AF.Sigmoid), reads=[Bdm], writes=[Bsg])
            wa, Bwa = wt("wa", [128, NT])
            S.op(DVE, lambda e: e.tensor_tensor(out=wa[:], in0=gw[:], in1=sg[:], op=ALU.mult), reads=[Bgw, Bsg], writes=[Bwa])
            wb, Bwb = wt("wb", [128, NT])
            S.op(DVE, lambda e: e.tensor_tensor(out=wb[:], in0=gw[:], in1=wa[:], op=ALU.subtract), reads=[Bgw, Bwa], writes=[Bwb])
            S.op(DVE, lambda e: e.tensor_tensor(out=eq1[:], in0=eq1[:], in1=wa[:].unsqueeze(2).broadcast_to([128, NT, 32]), op=ALU.mult),
                 reads=[Bwa], writes=[Beq1])
            S.op(DVE, lambda e: e.tensor_tensor(out=eq2[:], in0=eq2[:], in1=wb[:].unsqueeze(2).broadcast_to([128, NT, 32]), op=ALU.mult),
                 reads=[Bwb], writes=[Beq2])
            gate, Bgate = lem, Blem
            S.op(DVE, lambda e: e.tensor_tensor(out=gate[:], in0=eq1[:], in1=eq2[:], op=ALU.add), reads=[Beq1, Beq2], writes=[Bgate])
            if ('gate%d' % layer) in dbg:
                dump(f"gate{layer}", gate[:].rearrange("p t m -> p (t m)"), [Bgate], [128, NT * 32])
            hib, Bhib = wt("hib", [128, NT, 32], BF16)
            S.op(DVE, lambda e: e.tensor_copy(out=hib[:], in_=gate[:]), reads=[Bgate], writes=[Bhib])
            ghl, Bghl = wt("ghl", [128, NT, 64])
            S.op(DVE, lambda e: e.tensor_copy(out=ghl[:, :, 0:32], in_=hib[:]), reads=[Bhib], writes=[Bghl])
            S.op(DVE, lambda e: e.tensor_tensor(out=ghl[:, :, 32:64], in0=gate[:], in1=hib[:], op=ALU.subtract),
                 reads=[Bgate, Bhib], accw=[Bghl])
            for ti, (c0, c1) in enumerate(tts):
                n = c1 - c0
                tg = tg_of(c0)
                PT, Bpt = pr.get()
                S.op(PE, lambda e: e.transpose(PT[0:64, 0:n], ghl[0:n, ti, :], ident[0:n, 0:n]),
                     reads=[Bghl, B_cf], writes=[Bpt])
                S.op(ACT, lambda e: e.copy(gT[:, c0:c1], PT[0:64, 0:n]), reads=[Bpt], accw=[B_gT[tg]])

            pA = prot([0, 1])
            pB = prot([2, 3])
            pG = prot([4])
            pD = prot([5, 6, 7])
            for ui, (e_, fc) in enumerate(units):
                load(ui + 1)
                es_ = e_ % 2
                w13 = wsl[ui % 2][:, 0:4096].rearrange("p (k h m) -> p k h m", k=NJ, h=2)
                if fc == 0:
                    S.dma(POOL, w2s[es_][:].rearrange("p f d -> p (f d)"), w2_d[layer, e_], B_w2[es_], writes=[B_w2[es_]])
                for tg in tgs:
                    c0, c1 = tg
                    n = c1 - c0
                    if fc == 0:
                        PG, Bpg = pG.get()
                        S.op(PE, lambda e: e.matmul(PG[:, 0:n], lhsT=sel[:, e_, :], rhs=gT[:, c0:c1], start=True, stop=True),
                             reads=[B_sel, B_gT[tg]], writes=[Bpg])
                        S.op(ACT, lambda e: e.copy(gball[:, c0:c1], PG[:, 0:n]), reads=[Bpg], writes=[B_gb[tg]])
                    PA, Bpa = pA.get()
                    S.group(PE, [lambda e, kc=kc: e.matmul(PA[:, 0:n], lhsT=w13[:, kc, 0, :], rhs=hbcol(kc, c0, c1), start=(kc == 0), stop=(kc == NJ - 1)) for kc in range(NJ)],
                            reads=[wslB[ui % 2], HB[tg]], writes=[Bpa])
                    PBk, Bpb = pB.get()
                    S.group(PE, [lambda e, kc=kc: e.matmul(PBk[:, 0:n], lhsT=w13[:, kc, 1, :], rhs=hbcol(kc, c0, c1), start=(kc == 0), stop=(kc == NJ - 1)) for kc in range(NJ)],
                            reads=[wslB[ui % 2], HB[tg]], writes=[Bpb])
                    sa, Bsa = fscr.get()
                    S.op(ACT, lambda e: e.activation(out=sa[:, 0:n], in_=PA[:, 0:n], func=AF.Silu), reads=[Bpa], writes=[Bsa])
                    t, Bt = fscr.get()
                    S.op(DVE, lambda e: e.tensor_tensor(out=t[:, 0:n], in0=sa[:, 0:n], in1=gball[:, c0:c1], op=ALU.mult),
                         reads=[Bsa, B_gb[tg]], writes=[Bt])
                    S.op(DVE, lambda e: e.tensor_tensor(out=hid[:, es_, fc, c0:c1], in0=PBk[:, 0:n], in1=t[:, 0:n], op=ALU.mult),
                         reads=[Bpb, Bt], accw=[B_hid[es_]])
                if fc == 1 and es_ == 1:
                    first = (e_ == 1)
                    for j in range(NJ):
                        for tg in tgs:
                            c0, c1 = tg
                            n = c1 - c0
                            PD, Bpd = pD.get()
                            fns = []
                            for k, (ee, ff) in enumerate([(0, 0), (0, 1), (1, 0), (1, 1)]):
                                fns.append(lambda e, ee=ee, ff=ff, k=k: e.matmul(PD[:, 0:n], lhsT=w2s[ee][:, ff, j * 128:(j + 1) * 128], rhs=hid[:, ee, ff, c0:c1], start=(k == 0), stop=(k == 3)))
                            S.group(PE, fns, reads=[B_w2[0], B_w2[1], B_hid[0], B_hid[1]], writes=[Bpd])
                            if first:
                                S.op(DVE, lambda e: e.scalar_tensor_tensor(out=hcol(j, c0, c1), in0=hcol(j, c0, c1), scalar=ALPHA, in1=PD[:, 0:n], op0=ALU.mult, op1=ALU.add),
                                     reads=[Bpd], writes=[H[(j, tg)]])
                            else:
                                S.op(DVE, lambda e: e.tensor_tensor(out=hcol(j, c0, c1), in0=hcol(j, c0, c1), in1=PD[:, 0:n], op=ALU.add),
                                     reads=[Bpd], writes=[H[(j, tg)]])

        with ExitStack() as L0:
            hE = sb(L0, "hE", [128, NJ, TE])

            def hcol(j, c0, c1):
                return hE[:, j, c0:c1] if c1 <= TE else hO[:, j, c0 - TE:c1 - TE]

            def hbcol(j, c0, c1):
                return hbE[:, j, c0:c1] if c1 <= TE else hbO[:, j, c0 - TE:c1 - TE]

            def hcol4(j0, c0, c1):
                return hE[:, j0:j0 + 4, c0:c1] if c1 <= TE else hO[:, j0:j0 + 4, c0 - TE:c1 - TE]

            def hbcol4(j0, c0, c1):
                return hbE[:, j0:j0 + 4, c0:c1] if c1 <= TE else hbO[:, j0:j0 + 4, c0 - TE:c1 - TE]

            prefetch2(stream_weights(list(range(NJ)), lambda j: win_d[j], 4096, key='win'))
            with ExitStack() as PA_:
                xs = [sb(PA_, f"xs{i}", [128, D]) for i in range(2)]
                xsB = [Buf(f"xs{i}") for i in range(2)]
                pr = prot([0, 1, 2, 3])
                tts = TT_E + TT_O
                S.dma(SP, xs[0][0:128, :], xe_d[0:128, :], xsB[0], writes=[xsB[0]])
                for ti, (c0, c1) in enumerate(tts):
                    n = c1 - c0
                    if ti + 1 < len(tts):
                        a0, a1 = tts[ti + 1]
                        S.dma(SP, xs[(ti + 1) % 2][0:a1 - a0, :], xe_d[a0:a1, :], xsB[(ti + 1) % 2], writes=[xsB[(ti + 1) % 2]])
                    x_t = xs[ti % 2]
                    tg = tg_of(c0)
                    for jq in range(4):
                        P, Bp = pr.get()
                        Pv = P[:, :].rearrange("p (a b) -> p a b", a=4)
                        S.group(PE, [lambda e, i=i: e.transpose(Pv[:, i, 0:n], x_t[0:n, (jq * 4 + i) * 128:(jq * 4 + i + 1) * 128], ident[0:n, 0:n]) for i in range(4)],
                                reads=[xsB[ti % 2], B_cf], writes=[Bp])
                        S.op(ACT, lambda e: e.copy(hcol4(jq * 4, c0, c1), Pv[:, :, 0:n]),
                             reads=[Bp], writes=[H[(jq * 4 + i, tg)] for i in range(4)] if False else [], accw=[H[(jq * 4 + i, tg)] for i in range(4)])
                        S.op(DVE, lambda e: e.tensor_copy(out=hbcol4(jq * 4, c0, c1), in_=hcol4(jq * 4, c0, c1)),
                             reads=[H[(jq * 4 + i, tg)] for i in range(4)], accw=[HB[tg]])
                S.barrier()
            def stop(flag):
                if flag in dbg:
                    S.barrier()
                    raise _Stop(nc, dbg_d)
            if 'h0' in dbg:
                dump("h0E", hE[:].rearrange("p j t -> p (j t)"), [], [128, NJ * TE])
                dump("h0O", hO[:].rearrange("p j t -> p (j t)"), [], [128, NJ * TO])

            stop('stopA')
            with ExitStack() as PB_:
                z = sb(PB_, "z", [128, NJ, T], BF16)
                Bz = {(j, g): Buf(f"z{j}_{g[0]}") for j in range(NJ) for g in (G0, G1, G2)}
                yb = [sb(PB_, f"yb{i}", [128, T + 30], BF16) for i in range(2)]
                Byb = [Buf("yb0"), Buf("yb1")]
                diag = sb(PB_, "diag", [128, 31, 128], BF16)
                Bdiag = Buf("diag")
                for i in range(2):
                    S.op(DVE, lambda e, i=i: e.memset(yb[i][:, 0:30], 0.0), writes=[Byb[i]])
                units = list(range(NJ))
                load = stream_weights(units, lambda j: win_d[j], 4096, key='win')
                pA = prot([0, 1])
                pB = prot([2, 3])
                pC = prot([4, 5])
                load(0)
                pending = []

                def conv(j, tg):
                    c0, c1 = tg
                    n = c1 - c0
                    y = yb[j % 2]
                    PC, Bpc = pC.get()
                    S.group(PE, [lambda e, k=k: e.matmul(PC[:, 0:n], lhsT=diag[:, k, :], rhs=y[:, c0 + k:c0 + k + n], start=(k == 0), stop=(k == 30)) for k in range(31)],
                            reads=[Bdiag, Byb[j % 2]], writes=[Bpc])
                    S.op(ACT, lambda e: e.activation(out=z[:, j, c0:c1], in_=PC[:, 0:n], func=AF.Identity, bias=cfv('bdw', j)),
                         reads=[Bpc, B_cf], writes=[Bz[(j, tg)]])

                for j in units:
                    load(j + 1)
                    w = wsl[j % 2][:, 0:4096].rearrange("p (k h m) -> p k h m", k=NJ, h=2)
                    y = yb[j % 2]
                    for tg in (G0, G1, G2):
                        c0, c1 = tg
                        n = c1 - c0
                        PA, Bpa = pA.get()
                        S.group(PE, [lambda e, kc=kc: e.matmul(PA[:, 0:n], lhsT=w[:, kc, 0, :], rhs=hbcol(kc, c0, c1), start=(kc == 0), stop=(kc == NJ - 1)) for kc in range(NJ)],
                                reads=[wslB[j % 2], HB[tg]], writes=[Bpa])
                        PBk, Bpb = pB.get()
                        S.group(PE, [lambda e, kc=kc: e.matmul(PBk[:, 0:n], lhsT=w[:, kc, 1, :], rhs=hbcol(kc, c0, c1), start=(kc == 0), stop=(kc == NJ - 1)) for kc in range(NJ)],
                                reads=[wslB[j % 2], HB[tg]], writes=[Bpb])
                        if pending:
                            pj, ptg = pending.pop(0)
                            conv(pj, ptg)
                        if tg == G0:
                            a, b = CF['wdw']
                            wd = cf[:, a + j * 31:a + (j + 1) * 31]
                            S.op(DVE, lambda e: e.tensor_tensor(out=diag[:], in0=cbv('identb').unsqueeze(1).broadcast_to([128, 31, 128]),
                                                                in1=wd.unsqueeze(2).broadcast_to([128, 31, 128]), op=ALU.mult),
                                 reads=[B_cb, B_cf], writes=[Bdiag])
                        sg, Bsg = fscr.get()
                        S.op(ACT, lambda e: e.activation(out=sg[:, 0:n], in_=PBk[:, 0:n], func=AF.Sigmoid, bias=cfv('b2', j)),
                             reads=[Bpb, B_cf], writes=[Bsg])
                        if tg == G0:
                            S.op(DVE, lambda e: e.scalar_tensor_tensor(out=y[:, 30 + c0:30 + c1], in0=PA[:, 0:n], scalar=cfv('b1', j), in1=sg[:, 0:n], op0=ALU.add, op1=ALU.mult),
                                 reads=[Bpa, Bsg, B_cf], writes=[Byb[j % 2]])
                            S.op(DVE, lambda e: e.tensor_tensor(out=y[:, 30 + 16:30 + 176], in0=y[:, 30 + 16:30 + 176], in1=cfv('padmask'), op=ALU.mult),
                                 reads=[B_cf], writes=[Byb[j % 2]])
                        else:
                            S.op(DVE, lambda e: e.scalar_tensor_tensor(out=y[:, 30 + c0:30 + c1], in0=PA[:, 0:n], scalar=cfv('b1', j), in1=sg[:, 0:n], op0=ALU.add, op1=ALU.mult),
                                 reads=[Bpa, Bsg, B_cf], accw=[Byb[j % 2]])
                        pending.append((j, tg))
                while pending:
                    pj, ptg = pending.pop(0)
                    conv(pj, ptg)
                if 'z' in dbg:
                    dump("z", z[:].rearrange("p j t -> p (j t)"), list(Bz.values()), [128, NJ * T], BF16)

                prefetch2(proj_loader(wout_d, ('proj', 'bout')))
                p12 = prot([6, 7, 0, 1])
                for tg in (G0, G1, G2):
                    c0, c1 = tg
                    n = c1 - c0
                    P1, B1 = p12.get()
                    P2, B2 = p12.get()
                    S.group(PE, [lambda e, j=j: e.matmul(P1[:, 0:n], lhsT=ones, rhs=z[:, j, c0:c1], start=(j == 0), stop=(j == NJ - 1)) for j in range(NJ)],
                            reads=[B_cb] + [Bz[(j, tg)] for j in range(NJ)], writes=[B1])
                    for j in range(NJ):
                        vq, Bvq = bscr.get()
                        S.op(ACT, lambda e, vq=vq, j=j: e.activation(out=vq[:, 0:n], in_=z[:, j, c0:c1], func=AF.Square),
                             reads=[Bz[(j, tg)]], writes=[Bvq])
                        S.op(PE, lambda e, vq=vq, j=j: e.matmul(P2[:, 0:n], lhsT=ones, rhs=vq[:, 0:n], start=(j == 0), stop=(j == NJ - 1)),
                             reads=[Bvq, B_cb], writes=[B2] if j == 0 else [], accw=[] if j == 0 else [B2])
                    ln_stats(P1, B1, P2, B2, n)
                    for j in range(NJ):
                        t1, Bt1 = fscr.get()
                        S.op(DVE, lambda e, t1=t1, j=j: e.tensor_tensor(out=t1[:, 0:n], in0=z[:, j, c0:c1], in1=st_mean[:, 0:n], op=ALU.subtract),
                             reads=[Bz[(j, tg)], B_mean], writes=[Bt1])
                        t2, Bt2 = fscr.get()
                        S.op(DVE, lambda e, t1=t1, t2=t2, j=j: e.scalar_tensor_tensor(out=t2[:, 0:n], in0=t1[:, 0:n], scalar=cfv('clng', j), in1=st_rstd[:, 0:n], op0=ALU.mult, op1=ALU.mult),
                             reads=[Bt1, B_rstd, B_cf], writes=[Bt2])
                        S.op(ACT, lambda e, t2=t2, j=j: e.activation(out=hbcol(j, c0, c1), in_=t2[:, 0:n], func=AF.Silu, bias=cfv('clnb', j)),
                             reads=[Bt2, B_cf], writes=[HB[tg]] if j == 0 else [], accw=[] if j == 0 else [HB[tg]])
                S.barrier()
            if 'a' in dbg:
                dump("aE", hbE[:].rearrange("p j t -> p (j t)"), list(HB.values()), [128, NJ * TE], BF16)
                dump("aO", hbO[:].rearrange("p j t -> p (j t)"), list(HB.values()), [128, NJ * TO], BF16)

            stop('stopC')
            proj_residual((G0, G1, G2), wout_d, 'bout', hcol, hbcol)
            prefetch2(moe_loader(0))
            layernorm((G0, G1, G2), hcol, hbcol, 'mixg0', 'mixb0')
            if 'h1' in dbg:
                S.barrier()
                dump("h1E", hE[:].rearrange("p j t -> p (j t)"), [], [128, NJ * TE])
                dump("h1O", hO[:].rearrange("p j t -> p (j t)"), [], [128, NJ * TO])

            stop('stopD')
            if 'stop1' not in dbg:
                with ExitStack() as PE_:
                    moe(0, (G0, G1, G2), TT_E + TT_O, hcol, hbcol, PE_)
                    S.barrier()
                    if 'pre2' in dbg:
                        dump("pre2E", hE[:].rearrange("p j t -> p (j t)"), [], [128, NJ * TE])
                        dump("pre2O", hO[:].rearrange("p j t -> p (j t)"), [], [128, NJ * TO])
                prefetch2(stream_weights([0, 1], lambda g: wk_d[g], 2048, key='wk'))
                layernorm((G0, G1, G2), hcol, hbcol, 'ffng0', 'ffnb0')
            S.barrier()
            if 'h2' in dbg:
                dump("h2E", hE[:].rearrange("p j t -> p (j t)"), [], [128, NJ * TE])
                dump("h2O", hO[:].rearrange("p j t -> p (j t)"), [], [128, NJ * TO])

        def hcol(j, c0, c1):
            return hO[:, j, c0 - TE:c1 - TE]

        def hbcol(j, c0, c1):
            return hbE[:, j, c0:c1] if c1 <= TE else hbO[:, j, c0 - TE:c1 - TE]

        if 'stop2' not in dbg:
            with ExitStack() as L1:
                KT = sb(L1, "KT", [128, 2, T], BF16)
                B_KT = Buf("KT")
                V = sb(L1, "V", [128, 10, 256], BF16)
                B_V = Buf("V")
                with ExitStack() as LQ:
                    QT = sb(LQ, "QT", [128, NJ, TO], BF16)
                    B_QT = {g: Buf(f"QT{g[0]}") for g in (G1, G2)}
                    with ExitStack() as LCS:
                        cs = sb(LCS, "cs_sb", [128, 2, T])
                        B_cs = Buf("cs")
                        S.dma(SP, cs[:].rearrange("p a t -> p (a t)"), cs_d[:, :], B_cs, writes=[B_cs])
                        pr = prot([0, 1, 2, 3])
                        pr2 = prot([4, 5])

                        def rope_evac(P, Bp, n, c0, c1, bias_ap, dst_ap, dstB, acc=True):
                            raw, Braw = fscr.get()
                            S.op(ACT, lambda e: e.activation(out=raw[:, 0:n], in_=P[:, 0:n], func=AF.Identity, bias=bias_ap),
                                 reads=[Bp, B_cf], writes=[Braw])
                            P2, Bp2 = pr2.get()
                            S.op(PE, lambda e: e.matmul(P2[:, 0:n], lhsT=pswap, rhs=raw[:, 0:n], start=True, stop=True),
                                 reads=[Braw, B_cf], writes=[Bp2])
                            t1, Bt1 = fscr.get()
                            S.op(DVE, lambda e: e.tensor_tensor(out=t1[:, 0:n], in0=raw[:, 0:n], in1=cs[:, 0, c0:c1], op=ALU.mult),
                                 reads=[Braw, B_cs], writes=[Bt1])
                            t2, Bt2 = fscr.get()
                            S.op(DVE, lambda e: e.tensor_tensor(out=t2[:, 0:n], in0=P2[:, 0:n], in1=cs[:, 1, c0:c1], op=ALU.mult),
                                 reads=[Bp2, B_cs], writes=[Bt2])
                            S.op(DVE, lambda e: e.tensor_tensor(out=dst_ap, in0=t1[:, 0:n], in1=t2[:, 0:n], op=ALU.add),
                                 reads=[Bt1, Bt2], accw=[dstB])

                        units = [0, 1]
                        load = stream_weights(units, lambda g: wk_d[g], 2048, key='wk')
                        load(0)
                        for gp in units:
                            load(gp + 1)
                            w = wsl[gp % 2][:, 0:2048].rearrange("p (k m) -> p k m", k=NJ)
                            for (c0, c1) in [(0, 16), (48, 176), G1, G2]:
                                n = c1 - c0
                                P, Bp = pr.get()
                                S.group(PE, [lambda e, kc=kc: e.matmul(P[:, 0:n], lhsT=w[:, kc, :], rhs=hbcol(kc, c0, c1), start=(kc == 0), stop=(kc == NJ - 1)) for kc in range(NJ)],
                                        reads=[wslB[gp % 2], HB[tg_of(c0)]], writes=[Bp])
                                a, b = CF['bk']
                                rope_evac(P, Bp, n, c0, c1, cf[:, a + gp:a + gp + 1], KT[:, gp, c0:c1], B_KT)
                        S.dma(POOL, wsl[0][:, 0:4096], wv_d[:, :], wslB[0], writes=[wslB[0]])
                        wv = wsl[0][:, 0:4096].rearrange("p (k m) -> p k m", k=NJ)
                        vts = [(0, 16)] + [(48 + 128 * m, 48 + 128 * (m + 1)) for m in range(9)]
                        for vi, (c0, c1) in enumerate(vts):
                            n = c1 - c0
                            P, Bp = pr.get()
                            S.group(PE, [lambda e, kc=kc: e.matmul(P[0:n, 0:256], lhsT=hbcol(kc, c0, c1), rhs=wv[:, kc, :], start=(kc == 0), stop=(kc == NJ - 1)) for kc in range(NJ)],
                                    reads=[wslB[0], HB[tg_of(c0)]], writes=[Bp])
                            S.op(DVE, lambda e: e.tensor_tensor(out=V[0:n, vi, :], in0=P[0:n, 0:256], in1=cfv('bvb')[0:n, :], op=ALU.add),
                                 reads=[Bp, B_cf], accw=[B_V])
                        units = list(range(NJ))
                        load = stream_weights(units, lambda j: wq_d[j], 2048)
                        load(0)
                        for j in units:
                            load(j + 1)
                            w = wsl[j % 2][:, 0:2048].rearrange("p (k m) -> p k m", k=NJ)
                            for tg in (G1, G2):
                                c0, c1 = tg
                                n = c1 - c0
                                P, Bp = pr.get()
                                S.group(PE, [lambda e, kc=kc: e.matmul(P[:, 0:n], lhsT=w[:, kc, :], rhs=hbcol(kc, c0, c1), start=(kc == 0), stop=(kc == NJ - 1)) for kc in range(NJ)],
                                        reads=[wslB[j % 2], HB[tg]], writes=[Bp])
                                rope_evac(P, Bp, n, c0, c1, cfv('bq', j), QT[:, j, c0 - TE:c1 - TE], B_QT[tg])
                        S.barrier()
                    if 'qkv' in dbg:
                        dump("KT", KT[:].rearrange("p g t -> p (g t)"), [], [128, 2 * T], BF16)
                        dump("V", V[:].rearrange("p g t -> p (g t)"), [], [128, 10 * 256], BF16)
                        dump("QT", QT[:].rearrange("p g t -> p (g t)"), [], [128, NJ * TO], BF16)

                    prefetch2(proj_loader(wo_d, ('proj', 'bo')))
                    Eo = Rot([(sb(LQ, f"Eo{i}", [128, 512], BF16), Buf(f"Eo{i}")) for i in range(3)])
                    Ep = Rot([(sb(LQ, f"Ep{i}", [128, 512], BF16), Buf(f"Ep{i}")) for i in range(3)])
                    Em = Rot([(sb(LQ, f"Em{i}", [16, 512], BF16), Buf(f"Em{i}")) for i in range(3)])
                    pS = prot([0, 1, 2, 3, 4, 5])
                    pO = prot([6])
                    pDn = prot([7])
                    ascr = Rot([(sb(LQ, f"as{i}", [128, 512]), Buf(f"as{i}")) for i in range(6)])
                    its = [(gp, half, n_, quad) for gp in range(2) for half in range(2) for n_ in range(8) for quad in range(2)]
                    stS = {}
                    stE = {}

                    def geom(it):
                        gp, half, n_, quad = it
                        r0, r1 = half * 64, half * 64 + 64
                        own = (TE + 128 * n_, TE + 128 * n_ + 128)
                        prv = (48 + 128 * n_, 48 + 128 * n_ + 128)
                        qtg = G1 if n_ < 4 else G2
                        cj = gp * 8 + quad * 4
                        return gp, half, n_, quad, r0, r1, own, prv, qtg, cj

                    def stage_S(k):
                        gp, half, n_, quad, r0, r1, own, prv, qtg, cj = geom(its[k])
                        rhsQ = QT[r0:r1, cj:cj + 4, n_ * 128:(n_ + 1) * 128]
                        PSo, Bso = pS.get()
                        PSp, Bsp = pS.get()
                        PSm, Bsm = pS.get()
                        S.op(PE, lambda e: e.matmul(PSo[:, :], lhsT=KT[r0:r1, gp, own[0]:own[1]], rhs=rhsQ, start=True, stop=True),
                             reads=[B_KT, B_QT[qtg]], writes=[Bso])
                        S.op(PE, lambda e: e.matmul(PSp[:, :], lhsT=KT[r0:r1, gp, prv[0]:prv[1]], rhs=rhsQ, start=True, stop=True),
                             reads=[B_KT, B_QT[qtg]], writes=[Bsp])
                        S.op(PE, lambda e: e.matmul(PSm[0:16, :], lhsT=KT[r0:r1, gp, 0:16], rhs=rhsQ, start=True, stop=True),
                             reads=[B_KT, B_QT[qtg]], writes=[Bsm])
                        stS[k] = (PSo, Bso, PSp, Bsp, PSm, Bsm)

                    def stage_X(k):
                        gp, half, n_, quad, r0, r1, own, prv, qtg, cj = geom(its[k])
                        PSo, Bso, PSp, Bsp, PSm, Bsm = stS.pop(k)
                        eo, Beo = Eo.get()
                        ep, Bep = Ep.get()
                        em, Bem = Em.get()
                        S.op(ACT, lambda e: e.activation(out=eo[:, :], in_=PSo[:, :], func=AF.Exp, scale=0.125), reads=[Bso], writes=[Beo])
                        S.op(ACT, lambda e: e.activation(out=ep[:, :], in_=PSp[:, :], func=AF.Exp, scale=0.125), reads=[Bsp], writes=[Bep])
                        S.op(ACT, lambda e: e.activation(out=em[:, :], in_=PSm[0:16, :], func=AF.Exp, scale=0.125), reads=[Bsm], writes=[Bem])
                        eo3 = eo[:, :].rearrange("p (a b) -> p a b", a=4)
                        ep3 = ep[:, :].rearrange("p (a b) -> p a b", a=4)
                        S.op(DVE, lambda e: e.tensor_tensor(out=eo3, in0=eo3, in1=cbv('m_own').unsqueeze(1).broadcast_to([128, 4, 128]), op=ALU.mult),
                             reads=[B_cb], writes=[Beo])
                        mp = cbv('m_prev0') if n_ == 0 else cbv('m_prev')
                        S.op(DVE, lambda e: e.tensor_tensor(out=ep3, in0=ep3, in1=mp.unsqueeze(1).broadcast_to([128, 4, 128]), op=ALU.mult),
                             reads=[B_cb], writes=[Bep])
                        stE[k] = (eo, Beo, ep, Bep, em, Bem)

                    def stage_V(k):
                        gp, half, n_, quad, r0, r1, own, prv, qtg, cj = geom(its[k])
                        eo, Beo, ep, Bep, em, Bem = stE.pop(k)
                        PO, Bpo = pO.get()
                        PDn, Bpdn = pDn.get()
                        vo, vp = n_ + 2, n_ + 1
                        S.group(PE, [
                            lambda e: e.matmul(PO[:, :], lhsT=V[:, vo, gp * 128:(gp + 1) * 128], rhs=eo[:, :], start=True, stop=False),
                            lambda e: e.matmul(PO[:, :], lhsT=V[:, vp, gp * 128:(gp + 1) * 128], rhs=ep[:, :], start=False, stop=False),
                            lambda e: e.matmul(PO[:, :], lhsT=V[0:16, 0, gp * 128:(gp + 1) * 128], rhs=em[:, :], start=False, stop=True),
                        ], reads=[B_V, Beo, Bep, Bem], writes=[Bpo])
                        S.group(PE, [
                            lambda e: e.matmul(PDn[:, :], lhsT=ones, rhs=eo[:, :], start=True, stop=False),
                            lambda e: e.matmul(PDn[:, :], lhsT=ones, rhs=ep[:, :], start=False, stop=False),
                            lambda e: e.matmul(PDn[:, :], lhsT=ones[0:16, :], rhs=em[:, :], start=False, stop=True),
                        ], reads=[B_cb, Beo, Bep, Bem], writes=[Bpdn])
                        d1, Bd1 = ascr.get()
                        d13 = d1[r0:r1, :].rearrange("p (a b) -> p a b", a=4)
                        S.op(DVE, lambda e: e.tensor_tensor(out=d13, in0=PDn[r0:r1, :].rearrange("p (a b) -> p a b", a=4),
                                                            in1=esnk[r0:r1, cj:cj + 4].unsqueeze(2).broadcast_to([64, 4, 128]), op=ALU.add),
                             reads=[Bpdn, B_esnk], writes=[Bd1])
                        po, Bpos = ascr.get()
                        S.op(ACT, lambda e: e.copy(po[r0:r1, :], PO[r0:r1, :]), reads=[Bpo], writes=[Bpos])
                        S.op(ACT, lambda e: e.activation(out=d1[r0:r1, :], in_=d1[r0:r1, :], func=AF.Ln), reads=[Bd1], writes=[Bd1])
                        S.op(ACT, lambda e: e.activation(out=d1[r0:r1, :], in_=d1[r0:r1, :], func=AF.Exp, scale=-1.0), reads=[Bd1], writes=[Bd1])
                        S.op(DVE, lambda e: e.tensor_tensor(out=hbO[r0:r1, cj:cj + 4, n_ * 128:(n_ + 1) * 128],
                                                            in0=po[r0:r1, :].rearrange("p (a b) -> p a b", a=4),
                                                            in1=d1[r0:r1, :].rearrange("p (a b) -> p a b", a=4), op=ALU.mult),
                             reads=[Bpos, Bd1], accw=[HB[qtg]])

                    stage_S(0)
                    stage_X(0)
                    stage_S(1)
                    stage_X(1)
                    for k in range(len(its)):
                        if k + 2 < len(its):
                            stage_S(k + 2)
                            stage_X(k + 2)
                        stage_V(k)
                    S.barrier()
            if 'att' in dbg:
                dump("attT", hbO[:].rearrange("p j t -> p (j t)"), [], [128, NJ * TO], BF16)
            proj_residual((G1, G2), wo_d, 'bo', hcol, hbcol)
            prefetch2(moe_loader(1))
            layernorm((G1, G2), hcol, hbcol, 'mixg1', 'mixb1')
            if 'h3' in dbg:
                S.barrier()
                dump("h3O", hO[:].rearrange("p j t -> p (j t)"), [], [128, NJ * TO])
            if 'stop3' not in dbg:
                with ExitStack() as PM_:
                    moe(1, (G1, G2), TT_O, hcol, hbcol, PM_)
                    S.barrier()
                layernorm((G1, G2), hcol, hbcol, 'ffng1', 'ffnb1', write_hb=False)
            S.barrier()

        if not _CACHE_OUT_DONE:
            with ExitStack() as LO:
                ost = [sb(LO, f"ost{i}", [128, D]) for i in range(2)]
                Bost = [Buf("ost0"), Buf("ost1")]
                pr = prot([0, 1, 2, 3])
                for k in range(8):
                    o = ost[k % 2]
                    tg = G1 if k < 4 else G2
                    for jq in range(4):
                        P, Bp = pr.get()
                        S.group(PE, [lambda e, i=i: e.transpose(P[:, i * 128:(i + 1) * 128], hO[:, jq * 4 + i, k * 128:(k + 1) * 128], ident) for i in range(4)],
                                reads=[B_cf] + [H[(jq * 4 + i, tg)] for i in range(4)], writes=[Bp])
                        eng = ACT if jq % 2 == 0 else DVE
                        if eng is ACT:
                            S.op(ACT, lambda e: e.copy(o[:, jq * 512:(jq + 1) * 512], P[:, :]), reads=[Bp], writes=[Bost[k % 2]] if jq == 0 else [], accw=[] if jq == 0 else [Bost[k % 2]])
                        else:
                            S.op(DVE, lambda e: e.tensor_copy(out=o[:, jq * 512:(jq + 1) * 512], in_=P[:, :]), reads=[Bp], accw=[Bost[k % 2]])
                    S.dma(SP, out_d[k * 128:(k + 1) * 128, :], o[:, :], Bost[k % 2], reads=[Bost[k % 2]])
                S.wait_all(SP, Bost)
                S.barrier()

    return nc, dbg_d


def _fm_vec(v):
    return np.ascontiguousarray(np.asarray(v, np.float32).reshape(NJ, 128).T)


def _head_perm():
    idx = []
    for jp in range(NJ):
        gp, i = jp // 8, jp % 8
        for hd in (16 * gp + i, 16 * gp + 8 + i):
            idx.extend(range(hd * 64, hd * 64 + 64))
    return np.array(idx)


def _w_fm(w, ncols_chunk):
    K, C = w.shape
    nch = C // ncols_chunk
    a = w.reshape(NJ, 128, nch, ncols_chunk).transpose(2, 1, 0, 3)
    return np.ascontiguousarray(a).reshape(nch, 128, NJ * ncols_chunk)


def prepare(inputs):
    f = lambda k: np.asarray(inputs[k], np.float32)
    x = f('x')[0]
    meta = f('meta_tokens')
    h0 = np.concatenate([meta, x], 0)
    shared = {}
    w_in = f('conv_w_in')[0]
    wv_ = w_in[:, :D].reshape(NJ, 128, NJ, 128)
    wg_ = w_in[:, D:].reshape(NJ, 128, NJ, 128)
    win = np.stack([wv_, wg_], 3)
    shared['win'] = np.ascontiguousarray(win.transpose(2, 1, 0, 3, 4)).reshape(NJ, 128, 4096)
    shared['wout'] = _w_fm(f('conv_w_out')[0], 128)
    w1 = f('expert_w1')
    w3 = f('expert_w3')
    a1 = w1.reshape(2, 32, NJ, 128, 2, 128)
    a3 = w3.reshape(2, 32, NJ, 128, 2, 128)
    w13 = np.stack([a1, a3], 5)
    shared['w13'] = np.ascontiguousarray(w13.transpose(0, 1, 4, 3, 2, 5, 6)).reshape(2, 32, 2, 128, 4096)
    del w13, a1, a3
    w2 = f('expert_w2').reshape(2, 32, 2, 128, D)
    shared['w2'] = np.ascontiguousarray(w2.transpose(0, 1, 3, 2, 4)).reshape(2, 32, 128, 4096)
    wr = np.concatenate([f('router_group_w'), f('router_expert_w')], -1)
    shared['wr'] = np.ascontiguousarray(wr.reshape(2, NJ, 128, 36).transpose(0, 2, 1, 3)).reshape(2, 128, NJ * 36)
    shared['wk'] = _w_fm(f('w_k'), 128)
    shared['wv'] = _w_fm(f('w_v'), 256)[0]
    perm = _head_perm()
    shared['wq'] = _w_fm(f('w_q')[0][:, perm], 128)
    shared['wo'] = _w_fm(f('w_o')[0][perm, :], 128)
    cfp = np.zeros((128, NCF), np.float32)

    def put(name, arr):
        a, b = CF[name]
        cfp[:, a:b] = arr
    put('ident', np.eye(128, dtype=np.float32))
    put('onesf', np.ones((128, 128), np.float32))
    ps = np.zeros((128, 128), np.float32)
    for m in range(128):
        d = m % 64
        if d < 8:
            ps[m + 8, m] = 1.0
        elif d < 16:
            ps[m - 8, m] = 1.0
    put('pswap', ps)
    b_in = f('conv_b_in')[0]
    put('b1', _fm_vec(b_in[:D]))
    put('b2', _fm_vec(b_in[D:]))
    put('bdw', _fm_vec(f('conv_b_dw')[0]))
    put('clng', _fm_vec(f('conv_ln_g')[0]))
    put('clnb', _fm_vec(f('conv_ln_b')[0]))
    put('bout', _fm_vec(f('conv_b_out')[0]))
    for l in range(2):
        put('mixg%d' % l, _fm_vec(f('ln_mix_g')[l]))
        put('mixb%d' % l, _fm_vec(f('ln_mix_b')[l]))
        put('ffng%d' % l, _fm_vec(f('ln_ffn_g')[l]))
        put('ffnb%d' % l, _fm_vec(f('ln_ffn_b')[l]))
    put('bq', _fm_vec(f('b_q')[0][perm]))
    put('bo', _fm_vec(f('b_o')[0]))
    put('snk', _fm_vec(np.repeat(f('sinks')[0], 64)[perm]))
    put('bk', np.ascontiguousarray(f('b_k').reshape(2, 128).T))
    wdw = f('conv_w_dw')[0]
    put('wdw', np.ascontiguousarray(wdw.reshape(31, NJ, 128).transpose(2, 1, 0)).reshape(128, NJ * 31))
    for l in range(2):
        br = np.concatenate([f('router_group_b')[l], f('router_expert_b')[l]])
        put('brb%d' % l, np.broadcast_to(br[None, :], (128, 36)))
    put('bvb', np.broadcast_to(f('b_v')[None, :], (128, 256)))
    kk = np.arange(128)[:, None]
    qq = np.arange(128)[None, :]
    m_own = (kk <= qq).astype(np.float32)
    m_prev = (kk > qq).astype(np.float32)
    sel = np.zeros((64, 32, 128), np.float32)
    for e in range(32):
        sel[e, e, :] = 1.0
        sel[32 + e, e, :] = 1.0
    shared['sel'] = sel.reshape(64, 32 * 128)
    inv_freq = (np.float32(500000.0) ** (-np.arange(0, 16, 2, dtype=np.float32) / np.float32(16))).astype(np.float32)
    in_maps = []
    for c in range(NCORES):
        own0 = 16 + 1024 * c
        pos = np.concatenate([np.arange(16), np.arange(own0 - 160, own0 + 1024)])
        valid = pos >= 0
        xe = np.zeros((T, D), np.float32)
        xe[valid] = h0[pos[valid]]
        cfc = cfp.copy()
        a, b = CF['padmask']
        cfc[:, a:b] = valid[16:176].astype(np.float32)[None, :]
        cbc = np.zeros((128, NCB), np.float32)
        cbc[:, 0:128] = 1.0
        cbc[:, 128:256] = np.eye(128, dtype=np.float32)
        cbc[:, 256:384] = m_own
        cbc[:, 384:512] = m_prev
        cbc[:, 512:640] = m_prev if c > 0 else 0.0
        ang = np.clip(pos, 0, None).astype(np.float32)[:, None] * inv_freq[None, :]
        cosv = np.cos(ang).astype(np.float32)
        sinv = np.sin(ang).astype(np.float32)
        cst = np.zeros((128, 2, T), np.float32)
        cst[:, 0, :] = 1.0
        for p in range(128):
            d = p % 64
            if d < 16:
                cst[p, 0, :] = cosv[:, d % 8]
                cst[p, 1, :] = -sinv[:, d % 8] if d < 8 else sinv[:, d % 8]
        m = dict(shared)
        m['xe'] = xe
        m['cf'] = cfc
        m['cb'] = cbc
        m['cs'] = cst.reshape(128, 2 * T)
        in_maps.append(m)
    return in_maps


_CACHE = {}


def kernel(**inputs):
    in_maps = prepare(inputs)
    if 'nc' not in _CACHE:
        _CACHE['nc'] = build()[0]
    nc = _CACHE['nc']
    res = run_bass_kernel_spmd(nc, in_maps, core_ids=list(range(NCORES)))
    out = np.concatenate([np.asarray(r["out"], np.float32) for r in res.results], 0)
    return out.reshape(1, NCORES * TO, D)
```
